# Optimizing a Trainium2 kernel written in Bass

```python
import numpy as np
import jax
import jax.numpy as jnp
from jax import lax

D_MODEL = 2048
BATCH = 2
SEQ = 4096
DEPTH = 1

PLE_DIM = 256
NSA_HEADS = 16
NSA_KV_GROUPS = 4
NSA_HPG = NSA_HEADS // NSA_KV_GROUPS
HEAD_DIM = 64
NSA_Q_WIDTH = NSA_HEADS * HEAD_DIM
NSA_KV_WIDTH = NSA_KV_GROUPS * HEAD_DIM
CMP_BLOCK = 32
CMP_STRIDE = 16
CMP_HIDDEN = 2 * HEAD_DIM
SLC_BLOCK = 64
SLC_TOPN = 16
WINDOW = 512
Q_BLOCK = 128
ATTN_SCALE = HEAD_DIM ** -0.5
NEG_INF = -1e30
FORCE_SCORE = 1e4
LRU_WIDTH = 1024
LRU_BLOCKS = 16
LRU_BW = LRU_WIDTH // LRU_BLOCKS
CONV_WIDTH = 4
LRU_C = 8.0
N_GROUPS = 4
EXPERTS_PER_GROUP = 8
N_EXPERTS = N_GROUPS * EXPERTS_PER_GROUP
EXPERT_TOPK = 2
D_EXPERT = 512
EPS = 1e-6
IN_SPLITS = (NSA_Q_WIDTH, NSA_KV_WIDTH, NSA_KV_WIDTH, NSA_KV_WIDTH, NSA_KV_WIDTH, NSA_KV_WIDTH, NSA_KV_WIDTH, 3 * NSA_HEADS, LRU_WIDTH, LRU_WIDTH, D_MODEL, D_MODEL)
IN_WIDTH = sum(IN_SPLITS)

kernel_name = 'hybrid_nsa_rglru_hmoe_block'


def _rms_norm(x, g):
    x32 = x.astype(jnp.float32)
    y = x32 * lax.rsqrt(jnp.mean(x32 * x32, axis=-1, keepdims=True) + EPS)
    return (y * g.astype(jnp.float32)).astype(x.dtype)


def _masked_softmax(s, mask):
    s = jnp.where(mask, s.astype(jnp.float32), NEG_INF)
    return jnp.where(mask, jax.nn.softmax(s, axis=-1), 0.0)


def _compress(kv, pos, w1, w2):
    b, s, g, hd = kv.shape
    nc = (s - CMP_BLOCK) // CMP_STRIDE + 1
    idx = np.arange(nc)[:, None] * CMP_STRIDE + np.arange(CMP_BLOCK)[None, :]
    blk = kv[:, idx] + pos[None, None, :, None, :]
    blk = jnp.moveaxis(blk, 3, 2).reshape(b, nc, g, CMP_BLOCK * hd)
    return jax.nn.gelu(blk @ w1) @ w2


def _overlap_matrix(nc, nsb):
    c0 = np.arange(nc) * CMP_STRIDE
    s0 = np.arange(nsb) * SLC_BLOCK
    ov = np.minimum(c0[:, None] + CMP_BLOCK, s0[None, :] + SLC_BLOCK) - np.maximum(c0[:, None], s0[None, :])
    return jnp.asarray(np.clip(ov, 0, None) / CMP_BLOCK, dtype=jnp.float32)


def _nsa(q, k_cmp, v_cmp, k_slc, v_slc, k_win, v_win, gates, ck_pos, ck_w1, ck_w2, cv_pos, cv_w1, cv_w2):
    b, s, _ = q.shape
    dt = q.dtype
    g, hpg, hd = NSA_KV_GROUPS, NSA_HPG, HEAD_DIM
    q = q.reshape(b, s, g, hpg, hd)
    kv_shape = (b, s, g, hd)
    k_cmp, v_cmp, k_slc, v_slc, k_win, v_win = [a.reshape(kv_shape) for a in (k_cmp, v_cmp, k_slc, v_slc, k_win, v_win)]
    t = jnp.arange(s)
    kc = _compress(k_cmp, ck_pos, ck_w1, ck_w2)
    vc = _compress(v_cmp, cv_pos, cv_w1, cv_w2)
    nc = kc.shape[1]
    cmp_end = jnp.arange(nc) * CMP_STRIDE + (CMP_BLOCK - 1)
    cmp_mask = cmp_end[None, :] <= t[:, None]
    sc = jnp.einsum('bsghd,bcgd->bghsc', q, kc) * ATTN_SCALE
    p_cmp = _masked_softmax(sc, cmp_mask)
    o_cmp = jnp.einsum('bghsc,bcgd->bsghd', p_cmp.astype(dt), vc)
    nsb = s // SLC_BLOCK
    imp = jnp.einsum('bghsc,cj->bgsj', p_cmp, _overlap_matrix(nc, nsb))
    blk_id = (t // SLC_BLOCK)[:, None]
    j = jnp.arange(nsb)[None, :]
    forced = (j == 0) | (j == blk_id) | (j == blk_id - 1)
    score = jnp.where(forced, FORCE_SCORE, jnp.where(j <= blk_id, imp, -1.0))
    top_n = min(SLC_TOPN, nsb)
    _, sel = lax.top_k(score, top_n)
    ks_blk = k_slc.reshape(b, nsb, SLC_BLOCK, g, hd).transpose(0, 3, 1, 2, 4)
    vs_blk = v_slc.reshape(b, nsb, SLC_BLOCK, g, hd).transpose(0, 3, 1, 2, 4)
    kw_pad = jnp.pad(k_win, ((0, 0), (WINDOW, 0), (0, 0), (0, 0)))
    vw_pad = jnp.pad(v_win, ((0, 0), (WINDOW, 0), (0, 0), (0, 0)))
    nqb = s // Q_BLOCK
    q_blocks = jnp.moveaxis(q.reshape(b, nqb, Q_BLOCK, g, hpg, hd), 1, 0)
    sel_blocks = jnp.moveaxis(sel.reshape(b, g, nqb, Q_BLOCK, top_n), 2, 0)
    bi_idx = jnp.arange(b)[:, None, None, None]
    gi_idx = jnp.arange(g)[None, :, None, None]
    offs_q = jnp.arange(Q_BLOCK)
    offs_slc = jnp.arange(SLC_BLOCK)
    offs_win = jnp.arange(WINDOW + Q_BLOCK)

    def block(args):
        qb, sb, blk = args
        tq = blk * Q_BLOCK + offs_q
        ks = ks_blk[bi_idx, gi_idx, sb]
        vs = vs_blk[bi_idx, gi_idx, sb]
        kpos = sb[..., None] * SLC_BLOCK + offs_slc
        m_s = (kpos <= tq[None, None, :, None, None]).reshape(b, g, 1, Q_BLOCK, top_n * SLC_BLOCK)
        ss = jnp.einsum('bqghd,bgqnkd->bghqnk', qb, ks).reshape(b, g, hpg, Q_BLOCK, top_n * SLC_BLOCK) * ATTN_SCALE
        ps = _masked_softmax(ss, m_s).astype(dt).reshape(b, g, hpg, Q_BLOCK, top_n, SLC_BLOCK)
        o_s = jnp.einsum('bghqnk,bgqnkd->bqghd', ps, vs)
        kw = lax.dynamic_slice_in_dim(kw_pad, blk * Q_BLOCK, WINDOW + Q_BLOCK, axis=1)
        vw = lax.dynamic_slice_in_dim(vw_pad, blk * Q_BLOCK, WINDOW + Q_BLOCK, axis=1)
        spos = blk * Q_BLOCK - WINDOW + offs_win
        diff = tq[:, None] - spos[None, :]
        m_w = (diff >= 0) & (diff < WINDOW) & (spos[None, :] >= 0)
        sw = jnp.einsum('bqghd,bkgd->bghqk', qb, kw) * ATTN_SCALE
        pw = _masked_softmax(sw, m_w).astype(dt)
        o_w = jnp.einsum('bghqk,bkgd->bqghd', pw, vw)
        return o_s, o_w

    o_slc, o_win = lax.map(block, (q_blocks, sel_blocks, jnp.arange(nqb)))
    o_slc = jnp.moveaxis(o_slc, 0, 1).reshape(b, s, g, hpg, hd)
    o_win = jnp.moveaxis(o_win, 0, 1).reshape(b, s, g, hpg, hd)
    gt = jax.nn.sigmoid(gates.astype(jnp.float32)).reshape(b, s, g, hpg, 3, 1).astype(dt)
    o = gt[..., 0, :] * o_cmp + gt[..., 1, :] * o_slc + gt[..., 2, :] * o_win
    return o.reshape(b, s, NSA_Q_WIDTH)


def _linear_combine(left, right):
    a_l, b_l = left
    a_r, b_r = right
    return a_l * a_r, a_r * b_l + b_r


def _rg_lru_branch(xb, yb, conv_w, conv_b, wa, ba, wx, bx, lam):
    b, s, _ = xb.shape
    dt = xb.dtype
    xc = lax.conv_general_dilated(xb, conv_w[:, None, :], window_strides=(1,), padding=[(CONV_WIDTH - 1, 0)], dimension_numbers=('NWC', 'WIO', 'NWC'), feature_group_count=LRU_WIDTH) + conv_b
    xg = xc.reshape(b, s, LRU_BLOCKS, LRU_BW)
    r = jax.nn.sigmoid((jnp.einsum('bsni,nij->bsnj', xg, wa) + ba).astype(jnp.float32))
    i = jax.nn.sigmoid((jnp.einsum('bsni,nij->bsnj', xg, wx) + bx).astype(jnp.float32))
    log_a = -LRU_C * jax.nn.softplus(-lam.astype(jnp.float32)).reshape(LRU_BLOCKS, LRU_BW) * r
    a = jnp.exp(log_a)
    u = jnp.sqrt(-jnp.expm1(2.0 * log_a)) * (i * xg.astype(jnp.float32))
    _, h = lax.associative_scan(_linear_combine, (a, u), axis=1)
    return h.reshape(b, s, LRU_WIDTH).astype(dt) * jax.nn.gelu(yb)


def _hier_moe(xn, w_grp, b_grp, w_exp, b_exp, w_gate, w_up, w_down):
    b, s, d = xn.shape
    xt = xn.reshape(b * s, d)
    n = xt.shape[0]
    g_prob = jax.nn.softmax((xt @ w_grp + b_grp).astype(jnp.float32), axis=-1)
    g_val, g_idx = lax.top_k(g_prob, 1)
    e_logits = (xt @ w_exp + b_exp).astype(jnp.float32).reshape(n, N_GROUPS, EXPERTS_PER_GROUP)
    e_in = jnp.take_along_axis(e_logits, jnp.broadcast_to(g_idx[:, :, None], (n, 1, EXPERTS_PER_GROUP)), axis=1)[:, 0]
    e_prob = jax.nn.softmax(e_in, axis=-1)
    e_val, e_idx = lax.top_k(e_prob, EXPERT_TOPK)
    w = g_val * e_val / jnp.sum(e_val, axis=-1, keepdims=True)
    eid = g_idx * EXPERTS_PER_GROUP + e_idx
    comb = jnp.sum(jax.nn.one_hot(eid, N_EXPERTS, dtype=jnp.float32) * w[..., None], axis=1).astype(xt.dtype)
    out = jnp.zeros_like(xt)
    for e in range(N_EXPERTS):
        hid = jax.nn.silu(xt @ w_gate[e]) * (xt @ w_up[e])
        out = out + comb[:, e:e + 1] * (hid @ w_down[e])
    return out.reshape(b, s, d)


def _dense(key, shape, fan_in):
    return jax.random.normal(key, shape, jnp.float32) * (fan_in ** -0.5)


def setup_inputs(seed: int = 0) -> dict:
    key = jax.random.key(seed)
    ks = jax.random.split(key, 40)
    L = DEPTH
    f32 = jnp.float32
    u = jax.random.uniform(ks[16], (L, LRU_WIDTH), f32, minval=0.9, maxval=0.999)
    sa = u ** (1.0 / LRU_C)
    return {
        'x': jax.random.normal(ks[0], (BATCH, SEQ, D_MODEL), f32),
        'p': jax.random.normal(ks[1], (L, BATCH, SEQ, PLE_DIM), f32),
        'ln_mix': 1.0 + 0.05 * jax.random.normal(ks[2], (L, D_MODEL), f32),
        'w_in': _dense(ks[3], (L, D_MODEL, IN_WIDTH), D_MODEL),
        'cmp_k_pos': 0.02 * jax.random.normal(ks[4], (L, CMP_BLOCK, HEAD_DIM), f32),
        'cmp_k_w1': _dense(ks[5], (L, CMP_BLOCK * HEAD_DIM, CMP_HIDDEN), CMP_BLOCK * HEAD_DIM),
        'cmp_k_w2': _dense(ks[6], (L, CMP_HIDDEN, HEAD_DIM), CMP_HIDDEN),
        'cmp_v_pos': 0.02 * jax.random.normal(ks[7], (L, CMP_BLOCK, HEAD_DIM), f32),
        'cmp_v_w1': _dense(ks[8], (L, CMP_BLOCK * HEAD_DIM, CMP_HIDDEN), CMP_BLOCK * HEAD_DIM),
        'cmp_v_w2': _dense(ks[9], (L, CMP_HIDDEN, HEAD_DIM), CMP_HIDDEN),
        'conv_w': _dense(ks[10], (L, CONV_WIDTH, LRU_WIDTH), CONV_WIDTH),
        'conv_b': 0.01 * jax.random.normal(ks[11], (L, LRU_WIDTH), f32),
        'lru_wa': _dense(ks[12], (L, LRU_BLOCKS, LRU_BW, LRU_BW), LRU_BW),
        'lru_ba': 0.01 * jax.random.normal(ks[13], (L, LRU_BLOCKS, LRU_BW), f32),
        'lru_wx': _dense(ks[14], (L, LRU_BLOCKS, LRU_BW, LRU_BW), LRU_BW),
        'lru_bx': 0.01 * jax.random.normal(ks[15], (L, LRU_BLOCKS, LRU_BW), f32),
        'lru_lambda': jnp.log(sa) - jnp.log1p(-sa),
        'w_nsa_up': _dense(ks[17], (L, NSA_Q_WIDTH, D_MODEL), NSA_Q_WIDTH),
        'w_lru_up': _dense(ks[18], (L, LRU_WIDTH, D_MODEL), LRU_WIDTH),
        'w_out': _dense(ks[19], (L, D_MODEL, D_MODEL), D_MODEL),
        'ln_ffn': 1.0 + 0.05 * jax.random.normal(ks[20], (L, D_MODEL), f32),
        'w_grp': _dense(ks[21], (L, D_MODEL, N_GROUPS), D_MODEL),
        'b_grp': 0.01 * jax.random.normal(ks[22], (L, N_GROUPS), f32),
        'w_exp': _dense(ks[23], (L, D_MODEL, N_EXPERTS), D_MODEL),
        'b_exp': 0.01 * jax.random.normal(ks[24], (L, N_EXPERTS), f32),
        'w_gate': _dense(ks[25], (L, N_EXPERTS, D_MODEL, D_EXPERT), D_MODEL),
        'w_up': _dense(ks[26], (L, N_EXPERTS, D_MODEL, D_EXPERT), D_MODEL),
        'w_down': _dense(ks[27], (L, N_EXPERTS, D_EXPERT, D_MODEL), D_EXPERT),
        'ln_ple': 1.0 + 0.05 * jax.random.normal(ks[28], (L, D_MODEL), f32),
        'w_ple': _dense(ks[29], (L, PLE_DIM, D_MODEL), PLE_DIM),
        'w_ple_gate': _dense(ks[30], (L, D_MODEL, D_MODEL), D_MODEL),
        'ln_final': 1.0 + 0.05 * jax.random.normal(ks[31], (D_MODEL,), f32),
    }


def reference(x, p, ln_mix, w_in, cmp_k_pos, cmp_k_w1, cmp_k_w2, cmp_v_pos, cmp_v_w1, cmp_v_w2, conv_w, conv_b, lru_wa, lru_ba, lru_wx, lru_bx, lru_lambda, w_nsa_up, w_lru_up, w_out, ln_ffn, w_grp, b_grp, w_exp, b_exp, w_gate, w_up, w_down, ln_ple, w_ple, w_ple_gate, ln_final):
    split_at = [int(v) for v in np.cumsum(IN_SPLITS)[:-1]]
    for i in range(DEPTH):
        h = _rms_norm(x, ln_mix[i])
        z = h @ w_in[i]
        q, k_c, v_c, k_s, v_s, k_w, v_w, nsa_g, lru_x, lru_y, mg_a, mg_b = jnp.split(z, split_at, axis=-1)
        y_a = _nsa(q, k_c, v_c, k_s, v_s, k_w, v_w, nsa_g, cmp_k_pos[i], cmp_k_w1[i], cmp_k_w2[i], cmp_v_pos[i], cmp_v_w1[i], cmp_v_w2[i]) @ w_nsa_up[i]
        y_b = _rg_lru_branch(lru_x, lru_y, conv_w[i], conv_b[i], lru_wa[i], lru_ba[i], lru_wx[i], lru_bx[i], lru_lambda[i]) @ w_lru_up[i]
        merged = jax.nn.sigmoid(mg_a) * y_a + jax.nn.sigmoid(mg_b) * y_b
        x = x + merged @ w_out[i]
        x = x + _hier_moe(_rms_norm(x, ln_ffn[i]), w_grp[i], b_grp[i], w_exp[i], b_exp[i], w_gate[i], w_up[i], w_down[i])
        ple_gate = jax.nn.sigmoid(_rms_norm(x, ln_ple[i]) @ w_ple_gate[i])
        x = x + ple_gate * (p[i] @ w_ple[i])
    return _rms_norm(x, ln_final)
```

```python
import contextlib
import numpy as np
import concourse.bass as bass
import concourse.mybir as mybir
from concourse.bass_utils import run_bass_kernel_spmd

F32 = mybir.dt.float32
BF16 = mybir.dt.bfloat16
I32 = mybir.dt.int32
AF = mybir.ActivationFunctionType
ALU = mybir.AluOpType
AX = mybir.AxisListType

SAME_ENGINE_SYNC = True
N_DMA_SEMS = 48


class Buf:
    __slots__ = ("name", "w", "r")

    def __init__(self, name=""):
        self.name = name
        self.w = []
        self.r = {}


class Eng:
    def __init__(self, name, h, sem):
        self.name = name
        self.h = h
        self.sem = sem
        self.count = 0
        self.seen = {}


class K:
    def __init__(self, nc, es):
        self.nc = nc
        self.es = es
        self.E = {}
        for name, h in (("pe", nc.tensor), ("act", nc.scalar), ("dve", nc.vector),
                        ("pool", nc.gpsimd), ("sp", nc.sync)):
            sem = es.enter_context(nc.semaphore("sem_" + name))
            self.E[name] = Eng(name, h, sem)
        self.dsem = [es.enter_context(nc.semaphore("dsem%d" % i)) for i in range(N_DMA_SEMS)]
        self.dma_i = 0
        self.dma_tix = []
        self.out_tix = []
        self.nbuf = 0
        self._bscr = self.sb("bar_scr", [128, 8], F32)
        self._bb = {n: self.buf("bar_" + n) for n in ("pe", "act", "dve", "pool")}
        self.op("dve", lambda e: e.memset(self._bscr[:, :], 0.0), writes=list(self._bb.values()))

    ARENA = 204 * 1024

    def _arena_init(self):
        self.arena = self.es.enter_context(self.nc.sbuf_tensor("arena", [128, self.ARENA // 2], BF16))
        self.free_list = [(0, self.ARENA)]
        self.live = {}

    def sb(self, name, shape, dt, es=None):
        if not hasattr(self, "arena"):
            self._arena_init()
        esz = {F32: 4, BF16: 2, I32: 4}[dt]
        n = 1
        for d in shape[1:]:
            n *= d
        nbytes = (n * esz + 63) // 64 * 64
        for idx, (off, sz) in enumerate(self.free_list):
            if sz >= nbytes:
                break
        else:
            raise RuntimeError("arena full allocating %s (%d B); live=%s" % (name, nbytes, sorted((v[1], k_) for k_, v in self.live.items())))
        if sz == nbytes:
            self.free_list.pop(idx)
        else:
            self.free_list[idx] = (off + nbytes, sz - nbytes)
        assert name not in self.live, name
        self.live[name] = (off, nbytes)
        v = self.arena[0:shape[0], off // 2:(off + n * esz) // 2]
        if dt != BF16:
            v = v.bitcast(dt)
        if len(shape) > 2:
            names = " ".join("d%d" % i for i in range(1, len(shape)))
            kw = {"d%d" % i: shape[i] for i in range(2, len(shape))}
            v = v.rearrange("p (%s) -> p %s" % (names, names), **kw)
        if es is not None:
            es.callback(self.sb_free, name)
        return v

    def sb_free(self, name):
        off, nbytes = self.live.pop(name)
        fl = self.free_list + [(off, nbytes)]
        fl.sort()
        merged = []
        for o, z in fl:
            if merged and merged[-1][0] + merged[-1][1] == o:
                merged[-1] = (merged[-1][0], merged[-1][1] + z)
            else:
                merged.append((o, z))
        self.free_list = merged

    def ps(self, name, shape, dt, es=None):
        return (es or self.es).enter_context(self.nc.psum_tensor("ps_" + name, list(shape), dt))

    def buf(self, name=""):
        self.nbuf += 1
        return Buf(name)

    def _need(self, E, t):
        sem, val, ename = t
        if ename == E.name:
            if E.name == "pe" or not SAME_ENGINE_SYNC:
                return
        if ename is not None:
            assert self.E[ename].count >= val, "waiting on unsignaled ticket of %s" % ename
        key = id(sem)
        if E.seen.get(key, 0) >= val:
            return
        E.h.wait_ge(sem, val)
        E.seen[key] = val

    def _deps(self, E, reads, writes):
        for b in reads:
            for t in b.w:
                self._need(E, t)
        for b in writes:
            for t in b.w:
                self._need(E, t)
            for t in b.r.values():
                self._need(E, t)

    def _record(self, t, reads, writes):
        key = id(t[0])
        for b in reads:
            old = b.r.get(key)
            if old is None or old[1] < t[1]:
                b.r[key] = t
        for b in writes:
            b.w = [t]
            b.r = {}

    def op(self, eng, fn, reads=(), writes=(), sig=True):
        E = self.E[eng]
        self._deps(E, reads, writes)
        ins = fn(E.h)
        if sig:
            E.count += 1
            ins.then_inc(E.sem, 1)
            t = (E.sem, E.count, E.name)
        else:
            t = (E.sem, E.count + 1, E.name)
        self._record(t, reads, writes)
        return t

    def dma(self, queue, out, in_, reads=(), writes=(), is_output=False, add=False, **kw):
        E = self.E[queue]
        if add:
            self._deps(E, reads, ())
        else:
            self._deps(E, reads, writes)
        i = self.dma_i
        self.dma_i += 1
        sem = self.dsem[i % N_DMA_SEMS]
        val = 16 * (i // N_DMA_SEMS + 1)
        if i >= N_DMA_SEMS:
            self._need(E, self.dma_tix[i - N_DMA_SEMS])
        E.h.dma_start(out=out, in_=in_, **kw).then_inc(sem, 16)
        t = (sem, val, None)
        self.dma_tix.append(t)
        for b in reads:
            b.r[("d", i)] = t
        for b in writes:
            if add:
                b.w = b.w + [t]
            else:
                b.w = [t]
                b.r = {}
        if is_output:
            self.out_tix.append(t)
        return t

    def barrier(self):
        names = ["pe", "act", "dve", "pool"]
        sc = self._bscr
        self.op("act", lambda e: e.activation(out=sc[0:32, 0:1], in_=sc[0:32, 1:2], func=AF.Copy), writes=[self._bb["act"]])
        self.op("dve", lambda e: e.memset(sc[0:32, 2:3], 0.0), writes=[self._bb["dve"]])
        self.op("pool", lambda e: e.memset(sc[0:32, 3:4], 0.0), writes=[self._bb["pool"]])
        for n in names + ["sp"]:
            E = self.E[n]
            for m in names:
                if m != n:
                    F = self.E[m]
                    self._need(E, (F.sem, F.count, F.name))
            for t in self.dma_tix[-N_DMA_SEMS:]:
                self._need(E, t)

    def finish(self):
        E = self.E["sp"]
        for t in self.out_tix:
            self._need(E, t)


D = 2048
NKT = 16
S = 4096
NU = 32
NJ = 8
NTOK = 1024
HD = 64
IN_SPLITS = (1024, 256, 256, 256, 256, 256, 256, 48, 1024, 1024, 2048, 2048)
OFF = [0]
for _v in IN_SPLITS:
    OFF.append(OFF[-1] + _v)
(O_Q, O_KC, O_VC, O_KS, O_VS, O_KW, O_VW, O_G, O_LX, O_LY, O_MA, O_MB, O_END) = OFF
EPS = 1e-6


def bc(ap, shape):
    return ap.to_broadcast(list(shape))


class Prog:
    def __init__(self, dbg=None):
        self.dbg = dbg or {}
        nc = bass.Bass("TRN2", target_bir_lowering=False)
        self.nc = nc
        self.din = LazyIn(self)
        self.dout = {}

    def inp(self, name, shape, dt=F32):
        t = self.nc.dram_tensor(name, list(shape), dt, kind="ExternalInput").ap()
        self.din[name] = t
        return t

    def outp(self, name, shape, dt=F32):
        t = self.nc.dram_tensor(name, list(shape), dt, kind="ExternalOutput").ap()
        self.dout[name] = t
        return t


class St:
    pass


def mm_group(k, out_ap, pairs, reads, writes, sig_last=True):
    n = len(pairs)
    for i, (l, r) in enumerate(pairs):
        k.op("pe", lambda e: e.matmul(out_ap, l, r, start=(i == 0), stop=(i == n - 1)),
             reads=reads, writes=writes, sig=(sig_last and i == n - 1))


def mm1(k, out_ap, l, r, start, reads, writes, sig=False):
    k.op("pe", lambda e: e.matmul(out_ap, l, r, start=start, stop=True, skip_group_check=True),
         reads=reads, writes=writes, sig=sig)


IN_SHAPES = {
    "xs": ([S, D], F32), "pown": ([NTOK, 256], F32), "w_in": ([D, O_END], F32),
    "ln_mix": ([D], F32), "ln_ffn": ([D], F32), "ln_ple": ([D], F32), "ln_final": ([D], F32),
    "wa_bd": ([8, 128, 128], F32), "wx_bd": ([8, 128, 128], F32), "lru_small": ([128, 8, 8], F32),
    "cmp_k_pos": ([32, 64], F32), "cmp_k_w1": ([2048, 128], F32), "cmp_k_w2": ([128, 64], F32),
    "cmp_v_pos": ([32, 64], F32), "cmp_v_w1": ([2048, 128], F32), "cmp_v_w2": ([128, 64], F32),
    "w_nsa_up": ([1024, D], F32), "w_lru_up": ([1024, D], F32), "w_out": ([D, D], F32),
    "w_r": ([D, 36], F32), "b_r": ([36], F32),
    "w_gate": ([32, D, 512], F32), "w_up": ([32, D, 512], F32), "w_down": ([32, 512, D], F32),
    "w_ple": ([256, D], F32), "w_ple_gate": ([D, D], F32),
    "ident": ([128, 128], F32),
    "vrow": ([128, 512], F32),
    "vcol": ([128, NU], F32),
    "cmaskT": ([128, NJ, 2, 128], F32),
    "ovm": ([128, 2, 64], F32),
    "scV": ([128, NJ, 64], F32), "scN": ([128, NJ, 64], F32), "scF": ([128, NJ, 64], F32),
    "eall": ([128, S], BF16),
    "trineg": ([128, 2, 512], BF16),
}


class LazyIn(dict):
    def __init__(self, P):
        super().__init__()
        self.P = P

    def __missing__(self, name):
        shape, dt = IN_SHAPES[name]
        t = self.P.nc.dram_tensor(name, list(shape), dt, kind="ExternalInput").ap()
        self[name] = t
        return t


def setup(st):
    k, nc, P = st.k, st.nc, st.P
    din = P.din
    st.identf = k.sb("identf", [128, 128], F32); st.b_identf = k.buf()
    st.identb = k.sb("identb", [128, 128], BF16); st.b_identb = k.buf()
    k.dma("sp", st.identf[:], din["ident"], writes=[st.b_identf])
    k.dma("pool", st.identb[:], din["ident"], writes=[st.b_identb])
    st.gains = {}
    for nm in ("ln_mix", "ln_ffn", "ln_ple"):
        g = k.sb("g_" + nm, [128, NKT], F32)
        b = k.buf()
        k.dma("sp", g[:], din[nm].rearrange("(kt p) -> p kt", p=128), writes=[b],
              allow_slow_non_contiguous=True)
        st.gains[nm] = (g, b)
    st.tp_view = [k.ps("tp%d" % i, [128, 2048], BF16) for i in range(2)]
    st.b_tp = [k.buf("tp%d" % i) for i in range(2)]
    st.pb = [k.ps("pb%d" % i, [128, 512], F32) for i in range(4)]
    st.bpb = [k.buf("pb%d" % i) for i in range(4)]
    st.stat = k.sb("stat", [128, 4, 4], F32)
    st.bstat = [k.buf() for _ in range(4)]
    st.nt_i = 0
    st.cneg = k.sb("cneg", [128, 2], F32); st.b_cneg = k.buf()
    k.op("pool", lambda e: e.memset(st.cneg[:, 0:1], -0.5), writes=[st.b_cneg])
    k.op("pool", lambda e: e.memset(st.cneg[:, 1:2], 0.5), writes=[st.b_cneg])


def norm_front(st, src_ap, src_bufs, xb, junk, mul_eng="dve"):
    k = st.k
    i = st.nt_i % 4
    st.nt_i += 1
    sv = st.stat[:, i, :]
    bs = st.bstat[i]
    jt, bj = junk
    xbt, bxb = xb
    k.op("act", lambda e: e.activation(out=jt[:], in_=src_ap, func=AF.Square, accum_out=sv[:, 0:1]),
         reads=src_bufs, writes=[bj, bs])
    k.op("dve", lambda e: e.tensor_scalar(out=sv[:, 1:2], in0=sv[:, 0:1], scalar1=1.0 / D, scalar2=EPS,
                                           op0=ALU.mult, op1=ALU.add), reads=[bs], writes=[bs])
    k.op("pool", lambda e: e.tensor_tensor(out=sv[:, 3:4], in0=sv[:, 1:2], in1=st.cneg[:, 0:1], op=ALU.pow), reads=[bs, st.b_cneg], writes=[bs])
    k.op(mul_eng, lambda e: e.tensor_scalar(out=xbt[:], in0=src_ap, scalar1=sv[:, 3:4], scalar2=None, op0=ALU.mult),
         reads=list(src_bufs) + [bs], writes=[bxb])


def norm_back(st, xb, gain, dst_ap, dst_buf, tp_banks):
    k = st.k
    g, bg = gain
    xbt, bxb = xb
    tp, btp = tp_banks
    for kk in range(NKT):
        k.op("pe", lambda e: e.transpose(out=tp[:, kk * 128:(kk + 1) * 128], in_=xbt[:, kk * 128:(kk + 1) * 128],
                                         identity=st.identb[:]),
             reads=[bxb, st.b_identb], writes=[btp], sig=(kk == NKT - 1))
    k.op("dve", lambda e: e.tensor_tensor(out=dst_ap, in0=tp[:, :].rearrange("p (k t) -> p k t", t=128),
                                           in1=bc(g[:, :].unsqueeze(2), [128, NKT, 128]), op=ALU.mult),
         reads=[btp, bg], writes=[dst_buf])


def norm_transpose(st, src_ap, src_bufs, gain, dst_ap, dst_buf, xstage, xb, tp_banks, junk):
    norm_front(st, src_ap, src_bufs, xb, junk)
    norm_back(st, xb, gain, dst_ap, dst_buf, tp_banks)


def phase_lru(st):
    k, nc, P = st.k, st.nc, st.P
    din = P.din
    with contextlib.ExitStack() as es:
        Wlx = k.sb("Wlx", [128, NKT, 1024], BF16, es); bWlx = k.buf()
        for q4 in range(4):
            k.dma("pool", Wlx[:, q4 * 4:(q4 + 1) * 4, :],
                  din["w_in"][q4 * 512:(q4 + 1) * 512, O_LX:O_LX + 1024].rearrange("(kt p) n -> p kt n", p=128),
                  writes=[bWlx], add=(q4 > 0))
        WaBD = k.sb("WaBD", [128, 8, 128], F32, es); bWa = k.buf()
        WxBD = k.sb("WxBD", [128, 8, 128], F32, es); bWx = k.buf()
        k.dma("sp", WaBD[:], din["wa_bd"].rearrange("c p n -> p c n"), writes=[bWa])
        k.dma("sp", WxBD[:], din["wx_bd"].rearrange("c p n -> p c n"), writes=[bWx])
        sm = k.sb("lru_sm", [128, 8, 16], F32, es); bsm = k.buf()
        k.dma("sp", sm[:, :, 0:8], din["lru_small"], writes=[bsm])
        k.op("act", lambda e: e.activation(out=sm[:, :, 9:10], in_=sm[:, :, 7:8], func=AF.Exp, scale=-1.0),
             reads=[bsm], writes=[bsm])
        k.op("act", lambda e: e.activation(out=sm[:, :, 10:11], in_=sm[:, :, 9:10], func=AF.Ln, bias=1.0),
             reads=[bsm], writes=[bsm])
        k.op("dve", lambda e: e.tensor_scalar(out=sm[:, :, 8:9], in0=sm[:, :, 10:11], scalar1=-8.0, scalar2=None,
                                               op0=ALU.mult), reads=[bsm], writes=[bsm])
        k.op("dve", lambda e: e.tensor_scalar(out=sm[:, :, 11:13], in0=sm[:, :, 5:7], scalar1=0.5, scalar2=None, op0=ALU.mult),
             reads=[bsm], writes=[bsm])
        k.op("dve", lambda e: e.tensor_scalar(out=sm[:, :, 13:14], in0=sm[:, :, 10:11], scalar1=-4.0, scalar2=None, op0=ALU.mult),
             reads=[bsm], writes=[bsm])
        k.op("dve", lambda e: e.tensor_scalar(out=sm[:, :, 14:15], in0=sm[:, :, 10:11], scalar1=-8.0, scalar2=None, op0=ALU.mult),
             reads=[bsm], writes=[bsm])
        vrow = k.sb("vrow", [128, 512], F32, es); bvrow = k.buf()
        k.dma("sp", vrow[:], din["vrow"], writes=[bvrow])
        xbuf = k.sb("xbuf", [128, 8, 515], F32, es); bxbuf = [k.buf() for _ in range(8)]
        k.op("pool", lambda e: e.memset(xbuf[:, :, 0:3], 0.0), writes=bxbuf)
        carry = k.sb("carry", [128, 8], F32, es); bcarry = [k.buf() for _ in range(8)]
        k.op("pool", lambda e: e.memset(carry[:], 0.0), writes=bcarry)
        xst = [k.sb("xst%d" % i, [128, D], F32, es) for i in range(2)]; bxst = [k.buf() for _ in range(2)]
        xb = [(k.sb("xb%d" % i, [128, D], BF16, es), k.buf()) for i in range(1)]
        junk = (k.sb("junk", [128, D], BF16, es), k.buf())
        hT = [k.sb("hT%d" % i, [128, NKT, 512], BF16, es) for i in range(2)]
        bhT = [[k.buf() for _ in range(4)] for _ in range(2)]
        NT = 4
        tmp = [k.sb("ltmp%d" % i, [128, 5, 512], F32, es) for i in range(NT)]
        tmpb = [k.sb("ltmpb%d" % i, [128, 512], BF16, es) for i in range(NT)]
        btmp = [[k.buf() for _ in range(9)] for _ in range(NT)]
        nti = [0]

        def norms(c):
            hb = c % 2
            for uu in range(4):
                u = 4 * c + uu
                xi = nti[0] % 2
                k.dma("sp", xst[xi][:], din["xs"][u * 128:(u + 1) * 128, :], writes=[bxst[xi]])
                tpi = nti[0] % 2
                norm_front(st, xst[xi][:], [bxst[xi]], xb[0], junk)
                norm_back(st, xb[0], st.gains["ln_mix"], hT[hb][:, :, uu * 128:(uu + 1) * 128], bhT[hb][uu],
                          (st.tp_view[tpi][:, :], st.b_tp[tpi]))
                nti[0] += 1

        def stageA(c, ct):
            hb = c % 2
            s = (c * 8 + ct) % NT
            T = tmp[s]; B = btmp[s]
            pj = st.pb[ct % 2]; bpj = st.bpb[ct % 2]
            mm_group(k, pj[:, :], [(Wlx[:, kk, ct * 128:(ct + 1) * 128], hT[hb][:, kk, :]) for kk in range(NKT)],
                     reads=[bWlx] + bhT[hb], writes=[bpj])
            bx_ = bxbuf[ct]
            k.op("act", lambda e: e.activation(out=xbuf[:, ct, 3:515], in_=pj[:, :], func=AF.Copy),
                 reads=[bpj], writes=[bx_])
            xc = T[:, 0, :]
            k.op("act", lambda e: e.activation(out=xc, in_=pj[:, :], func=AF.Identity, scale=sm[:, ct, 3:4], bias=sm[:, ct, 4:5]),
                 reads=[bpj, bsm], writes=[B[0]])
            for w in (2, 1, 0):
                k.op("dve", lambda e: e.scalar_tensor_tensor(out=xc, in0=xbuf[:, ct, w:w + 512], scalar=sm[:, ct, w:w + 1],
                                                              in1=xc, op0=ALU.mult, op1=ALU.add),
                     reads=[bx_, bsm, B[0]], writes=[B[0]])
            k.op("pool", lambda e: e.tensor_copy(out=xbuf[:, ct, 0:3], in_=xbuf[:, ct, 512:515]),
                 reads=[bx_], writes=[bx_])

        def stageB1(c, ct):
            s = (c * 8 + ct) % NT
            T = tmp[s]; B = btmp[s]
            xc = T[:, 0, :]
            pr = st.pb[2]; pi_ = st.pb[3]
            k.op("pe", lambda e: e.matmul(pr[:, :], WaBD[:, ct, :], xc, start=True, stop=True),
                 reads=[bWa, B[0]], writes=[st.bpb[2]])
            k.op("pe", lambda e: e.matmul(pi_[:, :], WxBD[:, ct, :], xc, start=True, stop=True),
                 reads=[bWx, B[0]], writes=[st.bpb[3]])
            r_ = T[:, 1, :]; i_ = T[:, 2, :]; a_ = T[:, 3, :]; a2 = T[:, 4, :]
            k.op("act", lambda e: e.activation(out=r_, in_=pr[:, :], func=AF.Tanh, scale=0.5, bias=sm[:, ct, 11:12]),
                 reads=[st.bpb[2], bsm], writes=[B[1]])
            k.op("act", lambda e: e.activation(out=i_, in_=pi_[:, :], func=AF.Tanh, scale=0.5, bias=sm[:, ct, 12:13]),
                 reads=[st.bpb[3], bsm], writes=[B[2]])
            k.op("act", lambda e: e.activation(out=a_, in_=r_, func=AF.Exp, scale=sm[:, ct, 13:14], bias=sm[:, ct, 13:14]),
                 reads=[B[1], bsm], writes=[B[3]])
            k.op("act", lambda e: e.activation(out=a2, in_=r_, func=AF.Exp, scale=sm[:, ct, 14:15], bias=sm[:, ct, 14:15]),
                 reads=[B[1], bsm], writes=[B[4]])

        def stageB2(c, ct):
            s = (c * 8 + ct) % NT
            T = tmp[s]; B = btmp[s]
            xc = T[:, 0, :]
            hs = T[:, 1, :]; i_ = T[:, 2, :]; a_ = T[:, 3, :]; a2 = T[:, 4, :]
            mu = a2; u_ = i_
            k.op("act", lambda e: e.activation(out=mu, in_=a2, func=AF.Sqrt, scale=-1.0, bias=1.0 + 2.0 ** -22),
                 reads=[B[4]], writes=[B[4]])
            k.op("dve", lambda e: e.scalar_tensor_tensor(out=u_, in0=i_, scalar=1.0, in1=xc, op0=ALU.add, op1=ALU.mult),
                 reads=[B[2], B[0]], writes=[B[2]])
            k.op("dve", lambda e: e.scalar_tensor_tensor(out=u_, in0=u_, scalar=0.5, in1=mu, op0=ALU.mult, op1=ALU.mult),
                 reads=[B[2], B[4]], writes=[B[2]])
            if c == 0:
                k.op("dve", lambda e: e.tensor_tensor(out=u_, in0=u_, in1=vrow[:, :], op=ALU.mult),
                     reads=[B[2], bvrow], writes=[B[2]])
            k.op("dve", lambda e: e.tensor_tensor_scan(out=hs, data0=a_, data1=u_, initial=carry[:, ct:ct + 1],
                                                        op0=ALU.mult, op1=ALU.add),
                 reads=[B[3], B[2], bcarry[ct]], writes=[B[1]])
            k.op("dve", lambda e: e.tensor_copy(out=carry[:, ct:ct + 1], in_=hs[:, 511:512]),
                 reads=[B[1]], writes=[bcarry[ct]])
            k.op("pool", lambda e: e.tensor_copy(out=st.hstate[:, ct, c * 128:(c + 1) * 128], in_=hs[:, 384:512]),
                 reads=[B[1]], writes=[st.b_hstate])

        norms(0)
        for m in range(33):
            if m < 32:
                for n in (2 * m, 2 * m + 1):
                    stageA(n // 8, n % 8)
            if m >= 1:
                for n in (2 * m - 2, 2 * m - 1):
                    stageB1(n // 8, n % 8)
                for n in (2 * m - 2, 2 * m - 1):
                    stageB2(n // 8, n % 8)
            if m % 4 == 2 and m // 4 + 1 < 8:
                norms(m // 4 + 1)
        k.barrier()


def build(upto="all", dbg=False, n_experts=32):
    P = Prog()
    out = P.outp("out", [NTOK, D])
    nc = P.nc
    with contextlib.ExitStack() as es:
        k = K(nc, es)
        st = St()
        st.k, st.nc, st.P = k, nc, P
        setup(st)
        alloc_mixer(st)
        phase_kv(st)
        if dbg and upto == "kv":
            P.outp("d_KT", [128, 4, S], BF16); P.outp("d_Vs", [128, NU, 4, 65], BF16)
            P.outp("d_Vw", [128, NU, 4, 65], BF16); P.outp("d_kcv", [128, 4, 256], BF16)
            P.outp("d_VC", [128, 2, 4, 129], BF16)
            k.dma("sp", P.dout["d_KT"], st.KT[:, :, :], reads=st.b_KT, is_output=True)
            k.dma("sp", P.dout["d_Vs"], st.Vs[:, :, :, :], reads=st.b_V, is_output=True)
            k.dma("sp", P.dout["d_Vw"], st.Vw[:, :, :, :], reads=st.b_V, is_output=True)
            k.dma("sp", P.dout["d_kcv"], st.kcv[:, :, :], reads=[st.b_kcv], is_output=True)
            k.dma("sp", P.dout["d_VC"], st.VC[:, :, :, :], reads=[st.b_VC], is_output=True)
            k.finish()
            return P
        phase_attn(st)
        if dbg and upto == "attn":
            P.outp("d_nsaT", [128, 8, NTOK], BF16)
            k.dma("sp", P.dout["d_nsaT"], st.nsaT[:, :, :], reads=[st.b_nsaT], is_output=True)
            k.finish()
            return P
        free_mixer(st)
        st.hstate = k.sb("hstate", [128, 8, NTOK], BF16); st.b_hstate = k.buf()
        phase_lru(st)
        if dbg and upto == "lru":
            P.outp("d_hstate", [128, 8, NTOK], BF16)
            k.dma("sp", P.dout["d_hstate"], st.hstate[:], reads=[st.b_hstate], is_output=True)
            k.finish()
            return P
        st.n_experts = n_experts
        phase_post(st, out)
        k.finish()
    return P


def host_consts(q):
    sh = 3 - q
    c = {}
    c["ident"] = np.eye(128, dtype=np.float32)
    pos = np.arange(512)
    c["vrow"] = np.broadcast_to((pos >= 128 * sh).astype(np.float32), (128, 512)).copy()
    c["vcol"] = np.broadcast_to((np.arange(NU) >= sh).astype(np.float32), (128, NU)).copy()
    slot = np.arange(256)
    cp = slot - 1
    cvalid = (cp >= 8 * sh)
    cm = np.zeros((128, NJ, 2, 128), np.float32)
    for j in range(NJ):
        u = 4 * j + 3
        tpos = 128 * u + np.arange(128)
        m = ((16 * cp[:, None] + 31) <= tpos[None, :]) & cvalid[:, None]
        cm[:, j, :, :] = m.reshape(2, 128, 128).transpose(1, 0, 2)
    c["cmaskT"] = cm
    c0 = cp * 16
    s0 = np.arange(64) * 64
    ov = np.minimum(c0[:, None] + 32, s0[None, :] + 64) - np.maximum(c0[:, None], s0[None, :])
    ov = np.clip(ov, 0, None) / 32.0
    ov[~cvalid] = 0.0
    c["ovm"] = ov.reshape(2, 128, 64).transpose(1, 0, 2).astype(np.float32).copy()
    j0 = 2 * sh
    jb = np.arange(64)
    scV = np.zeros((128, NJ, 64), np.float32); scN = np.zeros_like(scV); scF = np.zeros_like(scV)
    for j in range(NJ):
        u = 4 * j + 3
        blk = (128 * u + np.arange(128)) // 64
        V = (jb[None, :] >= j0) & (jb[None, :] <= blk[:, None])
        F = ((jb[None, :] == j0) | (jb[None, :] == blk[:, None]) | (jb[None, :] == blk[:, None] - 1)) & V
        scV[:, j] = V; scN[:, j] = V.astype(np.float32) - 1.0; scF[:, j] = np.where(F, 1e4, -2.0)
    c["scV"], c["scN"], c["scF"] = scV, scN, scF
    import ml_dtypes
    ea = (np.arange(128)[:, None] == (np.arange(S)[None, :] // 64)).astype(np.float32)
    c["eall"] = ea.astype(ml_dtypes.bfloat16)
    kk = np.arange(128)[:, None]; tt = np.arange(128)[None, :]
    tri = np.where(kk <= tt, 0.0, -30000.0)
    atri = np.where(kk > tt, 0.0, -30000.0)
    tn = np.stack([np.tile(tri, (1, 4)), np.tile(atri, (1, 4))], axis=1)
    c["trineg"] = tn.astype(ml_dtypes.bfloat16)
    return c


def prep_inputs(inputs):
    f = lambda a: np.ascontiguousarray(np.asarray(a, dtype=np.float32))
    x = f(inputs["x"]); p = f(inputs["p"])[0]
    sh_w = {}
    sh_w["w_in"] = f(inputs["w_in"])[0]
    for nm in ("ln_mix", "ln_ffn", "ln_ple",
               "cmp_k_pos", "cmp_k_w1", "cmp_k_w2", "cmp_v_pos", "cmp_v_w1", "cmp_v_w2",
               "w_nsa_up", "w_lru_up", "w_out", "w_gate", "w_up", "w_down", "w_ple", "w_ple_gate"):
        sh_w[nm] = f(inputs[nm])[0]
    cols = [f(inputs["conv_w"])[0][w] for w in range(4)] + [f(inputs["conv_b"])[0], f(inputs["lru_ba"])[0].reshape(1024),
                                                            f(inputs["lru_bx"])[0].reshape(1024), f(inputs["lru_lambda"])[0]]
    sh_w["lru_small"] = np.ascontiguousarray(np.stack(cols, axis=-1).reshape(8, 128, 8).transpose(1, 0, 2))
    sh_w["ln_final"] = f(inputs["ln_final"])
    sh_w["w_r"] = np.concatenate([f(inputs["w_grp"])[0], f(inputs["w_exp"])[0]], axis=1)
    sh_w["b_r"] = np.concatenate([f(inputs["b_grp"])[0], f(inputs["b_exp"])[0]], axis=0)
    for nm, src in (("wa_bd", "lru_wa"), ("wx_bd", "lru_wx")):
        w = f(inputs[src])[0]
        bd = np.zeros((8, 128, 128), np.float32)
        for n in range(16):
            t, o = divmod(n, 2)
            bd[t, o * 64:(o + 1) * 64, o * 64:(o + 1) * 64] = w[n]
        sh_w[nm] = bd
    in_maps = []
    for c in range(8):
        b, q = divmod(c, 4)
        sh = 3 - q
        xs = np.zeros((S, D), np.float32)
        xs[128 * sh:] = x[b, :S - 128 * sh]
        rows = np.concatenate([np.arange(128 * (4 * j + q), 128 * (4 * j + q + 1)) for j in range(NJ)])
        m = dict(sh_w)
        m["xs"] = xs
        m["pown"] = np.ascontiguousarray(p[b, rows])
        m.update(host_consts(q))
        in_maps.append(m)
    return in_maps


def assemble(results, key="out"):
    out = np.zeros((2, S, D), np.float32)
    for c in range(8):
        b, q = divmod(c, 4)
        o = np.asarray(results[c][key]).reshape(NJ, 128, D)
        for j in range(NJ):
            r = 4 * j + q
            out[b, 128 * r:128 * (r + 1)] = o[j]
    return out


_CACHE = {}


def kernel(**inputs):
    if "P" not in _CACHE:
        _CACHE["P"] = build()
    P = _CACHE["P"]
    in_maps = prep_inputs(inputs)
    in_maps = [{n: m[n] for n in P.din} for m in in_maps]
    res = run_bass_kernel_spmd(P.nc, in_maps, core_ids=list(range(8)))
    return assemble(res.results)


def alloc_mixer(st):
    k = st.k
    st.KT = k.sb("KT", [128, 4, S], BF16); st.b_KT = [k.buf() for _ in range(8)]
    st.Vs = k.sb("Vs", [128, NU, 4, 65], BF16); st.Vw = k.sb("Vw", [128, NU, 4, 65], BF16)
    st.b_V = [k.buf() for _ in range(NU)]
    st.kcv = k.sb("kcv", [128, 4, 256], BF16); st.b_kcv = k.buf()
    st.VC = k.sb("VC", [128, 2, 4, 129], BF16); st.b_VC = k.buf()


def free_mixer(st):
    for n in ("KT", "Vs", "Vw", "kcv", "VC"):
        st.k.sb_free(n)


def phase_kv(st):
    k, nc, P = st.k, st.nc, st.P
    din = P.din
    with contextlib.ExitStack() as es:
        xst = [k.sb("xst%d" % i, [128, D], F32, es) for i in range(2)]; bxst = [k.buf() for _ in range(2)]
        xbs = [(k.sb("xb%d" % i, [128, D], BF16, es), k.buf()) for i in range(4)]
        hT = k.sb("hT0", [128, NKT, 512], BF16, es); bhT = [k.buf() for _ in range(4)]
        gk = k.sb("gk", [128, 2, 128], BF16, es); bgk = k.buf()
        nti = [0]

        def fronts(c):
            for uu in range(4):
                u = 4 * c + uu
                xi = nti[0] % 2
                nti[0] += 1
                k.dma("sp", xst[xi][:, :], din["xs"][u * 128:(u + 1) * 128, :], writes=[bxst[xi]])
                norm_front(st, xst[xi][:, :], [bxst[xi]], xbs[uu], xbs[uu])

        def backs(c):
            for uu in range(4):
                norm_back(st, xbs[uu], st.gains["ln_mix"], hT[:, :, uu * 128:(uu + 1) * 128], bhT[uu],
                          (st.tp_view[uu % 2][:, :], st.b_tp[uu % 2]))

        fronts(0)
        Wkv = k.sb("Wkv", [128, NKT, 4, 128], BF16, es); bWkv = [k.buf() for _ in range(4)]
        Wcmp = k.sb("Wcmp", [128, NKT, 4, 128], BF16, es); bWcmp = [k.buf() for _ in range(4)]
        Wv = k.sb("Wv", [128, NKT, 512], BF16, es); bWv = k.buf()
        for g in range(4):
            for (W, bW, o1, o2) in ((Wkv, bWkv, O_KS, O_KW), (Wcmp, bWcmp, O_KC, O_VC)):
                for half, o in ((0, o1), (1, o2)):
                    k.dma("pool", W[:, :, g, half * 64:(half + 1) * 64],
                          din["w_in"][:, o + g * 64:o + (g + 1) * 64].rearrange("(kt p) d -> p kt d", p=128),
                          writes=[bW[g]], add=(half == 1))
        k.dma("pool", Wv[:, :, 0:256], din["w_in"][:, O_VS:O_VS + 256].rearrange("(kt p) d -> p kt d", p=128), writes=[bWv])
        k.dma("pool", Wv[:, :, 256:512], din["w_in"][:, O_VW:O_VW + 256].rearrange("(kt p) d -> p kt d", p=128),
              writes=[bWv], add=True)
        w1 = k.sb("w1", [128, 32, 128], BF16, es); bw1 = k.buf()
        k.dma("pool", w1[0:64, :, :], din["cmp_k_w1"].rearrange("(l d) h -> d l h", d=64), writes=[bw1])
        k.dma("pool", w1[64:128, :, :], din["cmp_v_w1"].rearrange("(l d) h -> d l h", d=64), writes=[bw1], add=True)
        w2p = k.sb("w2p", [128, 2, 128], BF16, es); bw2 = k.buf()
        k.op("pool", lambda e: e.memset(w2p[:, :, :], 0.0), writes=[bw2])
        k.dma("pool", w2p[:, 0, 0:64], din["cmp_k_w2"], writes=[bw2])
        k.dma("pool", w2p[:, 1, 64:128], din["cmp_v_w2"], writes=[bw2], add=True)
        posT = k.sb("posT", [128, 32], BF16, es); bpos = k.buf()
        k.dma("pool", posT[0:64, :], din["cmp_k_pos"].rearrange("l d -> d l"), writes=[bpos], allow_slow_non_contiguous=True)
        k.dma("pool", posT[64:128, :], din["cmp_v_pos"].rearrange("l d -> d l"), writes=[bpos], add=True,
              allow_slow_non_contiguous=True)
        vcol = k.sb("vcol", [128, NU], F32, es); bvcol = k.buf()
        k.dma("sp", vcol[:, :], din["vcol"], writes=[bvcol])
        ovm = k.sb("ovm", [128, 2, 64], F32, es); bovm = k.buf()
        k.dma("sp", ovm[:, :, :], din["ovm"], writes=[bovm])
        cbias = k.sb("cbias", [128, 2], F32, es); bcb = k.buf()
        KC = k.sb("KCbuf", [128, 4, 528], BF16, es); bKC = k.buf()
        k.op("pool", lambda e: e.memset(KC[:, :, 0:16], 0.0), writes=[bKC])
        for c in range(8):
            backs(c)
            if c > 0:
                k.op("pool", lambda e: e.tensor_copy(out=KC[:, :, 0:16], in_=KC[:, :, 512:528]), reads=[bKC], writes=[bKC])
            pi = 0
            for g in range(4):
                for which in range(2):
                    W, bW = (Wkv, bWkv) if which == 0 else (Wcmp, bWcmp)
                    pj = st.pb[pi % 2]; bpj = st.bpb[pi % 2]; pi += 1
                    mm_group(k, pj[:, :], [(W[:, kk, g, :], hT[:, kk, :]) for kk in range(NKT)],
                             reads=[bW[g]] + bhT, writes=[bpj])
                    if which == 0:
                        k.op("act", lambda e: e.activation(out=st.KT[:, g, c * 512:(c + 1) * 512], in_=pj[:, :], func=AF.Copy),
                             reads=[bpj], writes=[st.b_KT[c]])
                    else:
                        k.op("act", lambda e: e.activation(out=KC[:, g, 16:528], in_=pj[:, :], func=AF.Copy),
                             reads=[bpj], writes=[bKC])
            if c + 1 < 8:
                fronts(c + 1)
            for uu in range(4):
                u = 4 * c + uu
                pj = st.pb[pi % 2]; bpj = st.bpb[pi % 2]; pi += 1
                mm_group(k, pj[:, :], [(hT[:, kk, uu * 128:(uu + 1) * 128], Wv[:, kk, :]) for kk in range(NKT)],
                         reads=[bWv] + bhT, writes=[bpj])
                k.op("dve", lambda e: e.tensor_copy(out=st.Vs[:, u, :, 0:64], in_=pj[:, 0:256].rearrange("p (g d) -> p g d", d=64)),
                     reads=[bpj], writes=[st.b_V[u]])
                k.op("dve", lambda e: e.tensor_copy(out=st.Vw[:, u, :, 0:64], in_=pj[:, 256:512].rearrange("p (g d) -> p g d", d=64)),
                     reads=[bpj], writes=[st.b_V[u]])
                k.op("pool", lambda e: e.tensor_copy(out=st.Vs[:, u, :, 64:65], in_=bc(vcol[:, u:u + 1].unsqueeze(1), [128, 4, 1])),
                     reads=[bvcol], writes=[st.b_V[u]])
                k.op("pool", lambda e: e.tensor_copy(out=st.Vw[:, u, :, 64:65], in_=bc(vcol[:, u:u + 1].unsqueeze(1), [128, 4, 1])),
                     reads=[bvcol], writes=[st.b_V[u]])
            if c == 0:
                for hv in range(2):
                    lo = hv * 64
                    pc = st.pb[2 + hv]; bpc = st.bpb[2 + hv]
                    mm_group(k, pc[:, 0:1], [(w1[lo:lo + 64, l, :], posT[lo:lo + 64, l:l + 1]) for l in range(32)],
                             reads=[bw1, bpos], writes=[bpc])
                    k.op("dve", lambda e: e.tensor_copy(out=cbias[:, hv:hv + 1], in_=pc[:, 0:1]), reads=[bpc], writes=[bcb])
            ph = st.pb[2]; bph = st.bpb[2]
            ph2 = st.pb[3]; bph2 = st.bpb[3]
            for hv, (pp, bpp) in enumerate(((ph, bph), (ph2, bph2))):
                lo = hv * 64
                mm_group(k, pp[:, 0:128].rearrange("p (g b) -> p g b", b=32),
                         [(w1[lo:lo + 64, l, :], KC[lo:lo + 64, :, l:l + 497:16]) for l in range(32)],
                         reads=[bw1, bKC], writes=[bpp])
                k.op("act", lambda e: e.activation(out=gk[:, hv, :], in_=pp[:, 0:128], func=AF.Gelu_apprx_tanh,
                                                    bias=cbias[:, hv:hv + 1]), reads=[bpp, bcb], writes=[bgk])
            mm_group(k, ph[:, 128:256], [(w2p[:, 0, :], gk[:, 0, :]), (w2p[:, 1, :], gk[:, 1, :])],
                     reads=[bw2, bgk], writes=[bph])
            k.op("dve", lambda e: e.tensor_copy(out=st.kcv[:, :, c * 32:(c + 1) * 32],
                                                 in_=ph[:, 128:256].rearrange("p (g b) -> p g b", b=32)),
                 reads=[bph], writes=[st.b_kcv])
        tpb = st.tp_view[0]; btpb = st.b_tp[0]
        for ct in range(2):
            for g in range(4):
                k.op("pe", lambda e: e.transpose(out=tpb[:, (ct * 4 + g) * 64:(ct * 4 + g + 1) * 64],
                                                 in_=st.kcv[64:128, g, ct * 128:(ct + 1) * 128],
                                                 identity=st.identb[64:128, 64:128]),
                     reads=[st.b_kcv, st.b_identb], writes=[btpb], sig=(ct == 1 and g == 3))
        k.op("dve", lambda e: e.tensor_copy(out=st.VC[:, :, :, 0:64],
                                             in_=tpb[:, 0:512].rearrange("p (c g d) -> p c g d", c=2, g=4)),
             reads=[btpb], writes=[st.b_VC])
        k.op("pool", lambda e: e.memset(st.VC[:, :, :, 64:65], 1.0), writes=[st.b_VC])
        for g in range(4):
            k.op("pool", lambda e: e.tensor_copy(out=st.VC[:, :, g, 65:129], in_=ovm[:, :, :]), reads=[bovm], writes=[st.b_VC])
        k.barrier()


def phase_attn(st):
    k, nc, P = st.k, st.nc, st.P
    din = P.din
    st.nsaT = k.sb("nsaT", [128, 8, NTOK], BF16); st.b_nsaT = k.buf()
    with contextlib.ExitStack() as es:
        eall = k.sb("eall", [128, S], BF16, es); beall = k.buf()
        k.dma("sp", eall[:, :], din["eall"], writes=[beall])
        trineg = k.sb("trineg", [128, 2, 512], BF16, es); btri = k.buf()
        k.dma("sp", trineg[:, :, :], din["trineg"], writes=[btri])
        cmask = k.sb("cmask", [128, NJ, 2, 128], BF16, es); bcm = k.buf()
        k.dma("pool", cmask[:, :, :, :], din["cmaskT"], writes=[bcm])
        scc = k.sb("scc", [128, 3, NJ, 64], F32, es); bscc = k.buf()
        for i_, nm in enumerate(("scV", "scN", "scF")):
            k.dma("sp", scc[:, i_, :, :], din[nm], writes=[bscc], add=(i_ > 0))
        hTo = k.sb("hTo", [128, NKT, NTOK], BF16, es); bhTo = [k.buf() for _ in range(NJ)]
        gsb = k.sb("gsb", [128, NJ, 48], F32, es); bgsb = k.buf()
        Wg48 = k.sb("Wg48", [128, NKT, 48], BF16, es); bWg = k.buf()
        k.dma("pool", Wg48[:, :, :], din["w_in"][:, O_G:O_G + 48].rearrange("(kt p) d -> p kt d", p=128), writes=[bWg])
        with contextlib.ExitStack() as es2:
            xst = [k.sb("xst%d" % i, [128, D], F32, es2) for i in range(2)]; bxst = [k.buf() for _ in range(2)]
            xbs = [(k.sb("xb%d" % i, [128, D], BF16, es2), k.buf()) for i in range(4)]
            for j in range(NJ):
                if j % 4 == 0:
                    for j2 in range(j, j + 4):
                        u = 4 * j2 + 3
                        xi = j2 % 2
                        k.dma("sp", xst[xi][:, :], din["xs"][u * 128:(u + 1) * 128, :], writes=[bxst[xi]])
                        norm_front(st, xst[xi][:, :], [bxst[xi]], xbs[j2 % 4], xbs[j2 % 4])
                norm_back(st, xbs[j % 4], st.gains["ln_mix"], hTo[:, :, j * 128:(j + 1) * 128], bhTo[j],
                          (st.tp_view[j % 2][:, :], st.b_tp[j % 2]))
                pg = st.pb[j % 2]; bpg = st.bpb[j % 2]
                mm_group(k, pg[:, 0:48], [(hTo[:, kk, j * 128:(j + 1) * 128], Wg48[:, kk, :]) for kk in range(NKT)],
                         reads=[bhTo[j], bWg], writes=[bpg])
                k.op("act", lambda e: e.activation(out=gsb[:, j, :], in_=pg[:, 0:48], func=AF.Sigmoid),
                     reads=[bpg], writes=[bgsb])
            k.barrier()
        Wq = k.sb("Wq", [128, NKT, 4, 128], BF16, es); bWq = k.buf()
        qTs = k.sb("qTs", [128, 4, NTOK], BF16, es); qTw = k.sb("qTw", [128, 4, NTOK], BF16, es); bqT = k.buf()
        k.op("pool", lambda e: e.memset(qTs[64:128, :, :], 0.0), writes=[bqT])
        k.op("pool", lambda e: e.memset(qTw[0:64, :, :], 0.0), writes=[bqT])
        EcTA = k.sb("EcT", [128, 2, 2, 512], BF16, es); bEcA = [[k.buf(), k.buf()], [k.buf(), k.buf()]]
        NPT = 3
        PT = [k.sb("PT%d" % i, [128, 512], BF16, es) for i in range(NPT)]; bPT = [k.buf() for _ in range(NPT)]
        smA = k.sb("att_sm", [128, 2, 64], F32, es); bsmA = [k.buf(), k.buf()]
        impA = k.sb("imp", [128, 2, 3, 64], F32, es); bimpA = [k.buf(), k.buf()]
        NEGT = k.sb("NEGT", [128, 4, 128], BF16, es); bNEG = k.buf()
        k.op("pool", lambda e: e.memset(NEGT[:, :, :], 0.0), writes=[bNEG])
        stgA = k.sb("stg", [128, 2, 2, 256], F32, es); bstgA = [k.buf(), k.buf()]
        stgbA = k.sb("stgb", [128, 2, 256], BF16, es); bstgbA = [k.buf(), k.buf()]
        tpf = st.tp_view[1][:, :].bitcast(F32)
        btpf = st.b_tp[1]
        tp0f = st.tp_view[0][:, :].bitcast(F32)
        tpn = st.tp_view[0]; btpn = st.b_tp[0]
        Oc = tpf[:, 512:1024]; bOc = k.buf()
        Ic = tp0f[:, 512:1024]; bIc = k.buf()
        pti = 0
        sbank = 0

        def run_branch(units, Obank, bO, vfirst_start):
            nonlocal pti, sbank
            n = len(units)
            slots = []
            first_pv = [True]

            def emit_S(i):
                nonlocal sbank
                sb_ = sbank % 2
                sbank += 1
                Sb = st.pb[sb_]; bS = st.bpb[sb_]
                sc = units[i]["score"]
                for mi, (cols, l, r, rd) in enumerate(sc):
                    o_ = Sb[:, cols[0]:cols[1]]
                    if len(r.shape) == 3:
                        o_ = o_.rearrange("p (h t) -> p h t", h=r.shape[1])
                    mm1(k, o_, l, r, start=(mi == 0), reads=rd, writes=[bS], sig=(mi == len(sc) - 1))
                return Sb, bS

            def emit_exp(i, Sb, bS):
                nonlocal pti
                p_ = pti % NPT
                pti += 1
                k.op("act", lambda e: e.activation(out=PT[p_][:, :], in_=Sb[:, :], func=AF.Exp, scale=0.125),
                     reads=[bS], writes=[bPT[p_]])
                return p_

            def emit_PV(i, p_):
                vr, vb = units[i]["v"]
                for hh in range(4):
                    mm1(k, Obank[:, hh * 65:(hh + 1) * 65], PT[p_][:, hh * 128:(hh + 1) * 128], vr,
                        start=(first_pv[0]), reads=[bPT[p_]] + vb, writes=[bO], sig=(hh == 3))
                    first_pv[0] = False

            pend = []
            for i in range(n):
                Sb, bS = emit_S(i)
                p_ = emit_exp(i, Sb, bS)
                pend.append((i, p_))
                if len(pend) >= 3:
                    ii, pp = pend.pop(0)
                    emit_PV(ii, pp)
            for ii, pp in pend:
                emit_PV(ii, pp)

        for g in range(4):
            first = True
            for hh in range(4):
                h = 4 * g + hh
                for half in range(2):
                    k.dma("pool", Wq[:, :, hh, half * 64:(half + 1) * 64],
                          din["w_in"][:, O_Q + h * 64:O_Q + (h + 1) * 64].rearrange("(kt p) d -> p kt d", p=128),
                          writes=[bWq], add=not first)
                    first = False
            for hh in range(4):
                for half in range(2):
                    pj = st.pb[(hh * 2 + half) % 2]; bpj = st.bpb[(hh * 2 + half) % 2]
                    mm_group(k, pj[:, :], [(Wq[:, kk, hh, :], hTo[:, kk, half * 512:(half + 1) * 512]) for kk in range(NKT)],
                             reads=[bWq] + bhTo, writes=[bpj])
                    k.op("act", lambda e: e.activation(out=qTs[0:64, hh, half * 512:(half + 1) * 512], in_=pj[0:64, :], func=AF.Copy),
                         reads=[bpj], writes=[bqT])
                    k.op("dve", lambda e: e.tensor_copy(out=qTw[64:128, hh, half * 512:(half + 1) * 512], in_=pj[64:128, :]),
                         reads=[bpj], writes=[bqT])
            def ctx(j):
                p = j % 2
                return dict(u=4 * j + 3, tok=slice(j * 128, (j + 1) * 128), sm=smA[:, p, :], bsm=bsmA[p], imp=impA[:, p, :, :], bimp=bimpA[p],
                            stg=stgA[:, p, :, :], bstg=bstgA[p], EcT=EcTA[:, p, :, :], bEc=bEcA[p],
                            gv=gsb[:, j, :].rearrange("p (h b) -> p h b", b=3))

            def cmp_a1(j):
                nonlocal sbank
                c_ = ctx(j); u = c_["u"]; tok = c_["tok"]; sm = c_["sm"]; bsm = c_["bsm"]; imp = c_["imp"]; bimp = c_["bimp"]
                stg = c_["stg"]; bstg = c_["bstg"]; EcT = c_["EcT"]; bEc = c_["bEc"]; gv = c_["gv"]
                Oc3 = Oc[:, 0:260].rearrange("p (h d) -> p h d", d=65)
                for ct in range(2):
                    Sb = st.pb[sbank % 2]; bS = st.bpb[sbank % 2]; sbank += 1
                    mm1(k, Sb[:, :].rearrange("p (h t) -> p h t", h=4), st.kcv[:, g, ct * 128:(ct + 1) * 128], qTs[:, :, tok],
                        start=True, reads=[st.b_kcv, bqT], writes=[bS], sig=True)
                    k.op("act", lambda e: e.activation(out=EcT[:, ct, :], in_=Sb[:, :], func=AF.Exp, scale=0.125),
                         reads=[bS], writes=[bEc[ct]])
                    k.op("dve", lambda e: e.tensor_tensor(out=EcT[:, ct, :].rearrange("p (h t) -> p h t", h=4),
                                                           in0=EcT[:, ct, :].rearrange("p (h t) -> p h t", h=4),
                                                           in1=bc(cmask[:, j, ct, :].unsqueeze(1), [128, 4, 128]), op=ALU.mult),
                         reads=[bEc[ct], bcm], writes=[bEc[ct]])

            def cmp_a2(j):
                c_ = ctx(j); u = c_["u"]; tok = c_["tok"]; sm = c_["sm"]; bsm = c_["bsm"]; imp = c_["imp"]; bimp = c_["bimp"]
                stg = c_["stg"]; bstg = c_["bstg"]; EcT = c_["EcT"]; bEc = c_["bEc"]; gv = c_["gv"]
                Oc3 = Oc[:, 0:260].rearrange("p (h d) -> p h d", d=65)
                fo = True
                for hh in range(4):
                    for ct in range(2):
                        mm1(k, Oc[:, hh * 65:(hh + 1) * 65], EcT[:, ct, hh * 128:(hh + 1) * 128], st.VC[:, ct, g, 0:65],
                            start=fo, reads=[bEc[ct], st.b_VC], writes=[bOc], sig=(hh == 3 and ct == 1))
                        mm1(k, Ic[:, hh * 64:(hh + 1) * 64], EcT[:, ct, hh * 128:(hh + 1) * 128], st.VC[:, ct, g, 65:129],
                            start=fo, reads=[bEc[ct], st.b_VC], writes=[bIc], sig=(hh == 3 and ct == 1))
                        fo = False
                k.op("dve", lambda e: e.tensor_scalar(out=sm[:, 0:4], in0=Oc3[:, :, 64], scalar1=1e-30, scalar2=None, op0=ALU.max),
                     reads=[bOc], writes=[bsm])
                k.op("dve", lambda e: e.reciprocal(out=sm[:, 0:4], in_=sm[:, 0:4]), reads=[bsm], writes=[bsm])
                k.op("dve", lambda e: e.tensor_scalar(out=imp[:, 0, :], in0=Ic[:, 0:64], scalar1=sm[:, 0:1], scalar2=None, op0=ALU.mult),
                     reads=[bIc, bsm], writes=[bimp])
                for hh in range(1, 4):
                    k.op("dve", lambda e: e.scalar_tensor_tensor(out=imp[:, 0, :], in0=Ic[:, hh * 64:(hh + 1) * 64], scalar=sm[:, hh:hh + 1],
                                                                  in1=imp[:, 0, :], op0=ALU.mult, op1=ALU.add),
                         reads=[bIc, bsm, bimp], writes=[bimp])
                k.op("dve", lambda e: e.tensor_tensor(out=imp[:, 0, :], in0=imp[:, 0, :], in1=scc[:, 0, j, :], op=ALU.mult),
                     reads=[bimp, bscc], writes=[bimp])
                k.op("dve", lambda e: e.tensor_tensor(out=imp[:, 0, :], in0=imp[:, 0, :], in1=scc[:, 1, j, :], op=ALU.add),
                     reads=[bimp, bscc], writes=[bimp])
                k.op("dve", lambda e: e.tensor_tensor(out=imp[:, 0, :], in0=imp[:, 0, :], in1=scc[:, 2, j, :], op=ALU.max),
                     reads=[bimp, bscc], writes=[bimp])
                k.op("dve", lambda e: e.max(out=sm[:, 16:24], in_=imp[:, 0, :]), reads=[bimp], writes=[bsm])
                k.op("dve", lambda e: e.match_replace(out=imp[:, 1, :], in_to_replace=sm[:, 16:24], in_values=imp[:, 0, :], imm_value=-1e30),
                     reads=[bimp, bsm], writes=[bimp])
                k.op("dve", lambda e: e.max(out=sm[:, 24:32], in_=imp[:, 1, :]), reads=[bimp], writes=[bsm])
                k.op("dve", lambda e: e.tensor_scalar(out=sm[:, 32:33], in0=sm[:, 31:32], scalar1=0.0, scalar2=None, op0=ALU.max),
                     reads=[bsm], writes=[bsm])
                k.op("dve", lambda e: e.tensor_scalar(out=imp[:, 2, :], in0=imp[:, 0, :], scalar1=sm[:, 32:33], scalar2=30000.0,
                                                       op0=ALU.is_ge, op1=ALU.mult), reads=[bimp, bsm], writes=[bimp])
                k.op("dve", lambda e: e.tensor_scalar(out=imp[:, 2, :], in0=imp[:, 2, :], scalar1=-30000.0, scalar2=None, op0=ALU.add),
                     reads=[bimp], writes=[bimp])
                k.op("dve", lambda e: e.tensor_tensor(out=sm[:, 12:16], in0=sm[:, 0:4], in1=gv[:, 4 * g:4 * g + 4, 0], op=ALU.mult),
                     reads=[bsm, bgsb], writes=[bsm])
                k.op("dve", lambda e: e.tensor_tensor(out=stg[:, 0, :].rearrange("p (h d) -> p h d", d=64), in0=Oc3[:, :, 0:64],
                                                       in1=bc(sm[:, 12:16].unsqueeze(2), [128, 4, 64]), op=ALU.mult),
                     reads=[bOc, bsm], writes=[bstg])

            def cmp_b(j):
                c_ = ctx(j); imp = c_["imp"]; bimp = c_["bimp"]
                k.op("pe", lambda e: e.transpose(out=tpf[0:64, 0:128], in_=imp[:, 2, :], identity=st.identf[:, :]),
                     reads=[bimp, st.b_identf], writes=[btpf])
                k.op("dve", lambda e: e.tensor_copy(out=NEGT[0:64, :, :], in_=bc(tpf[0:64, 0:128].unsqueeze(1), [64, 4, 128])),
                     reads=[btpf], writes=[bNEG])

            def slc_(j):
                c_ = ctx(j); u = c_["u"]; tok = c_["tok"]; sm = c_["sm"]; bsm = c_["bsm"]; stg = c_["stg"]; bstg = c_["bstg"]; gv = c_["gv"]
                NEG2 = NEGT[:, :, :].rearrange("p h t -> p (h t)")
                units = []
                for kt in range(u + 1):
                    ksl = slice(kt * 128, (kt + 1) * 128)
                    sc = [((0, 512), eall[:, ksl], NEG2, [beall, bNEG])]
                    if kt == u:
                        sc.append(((0, 512), st.identb[:, :], trineg[:, 0, :], [st.b_identb, btri]))
                    sc.append(((0, 512), st.KT[:, g, ksl], qTs[:, :, tok], [st.b_KT[kt // 4], bqT]))
                    units.append(dict(score=sc, v=(st.Vs[:, kt, g, :], [st.b_V[kt]])))
                Os = st.pb[2]; bOs = st.bpb[2]
                run_branch(units, Os, bOs, True)
                Os3 = Os[:, 0:260].rearrange("p (h d) -> p h d", d=65)
                k.op("dve", lambda e: e.tensor_scalar(out=sm[:, 4:8], in0=Os3[:, :, 64], scalar1=1e-30, scalar2=None, op0=ALU.max),
                     reads=[bOs], writes=[bsm])
                k.op("dve", lambda e: e.reciprocal(out=sm[:, 4:8], in_=sm[:, 4:8]), reads=[bsm], writes=[bsm])
                k.op("dve", lambda e: e.tensor_tensor(out=sm[:, 12:16], in0=sm[:, 4:8], in1=gv[:, 4 * g:4 * g + 4, 1], op=ALU.mult),
                     reads=[bsm, bgsb], writes=[bsm])
                k.op("dve", lambda e: e.tensor_tensor(out=stg[:, 1, :].rearrange("p (h d) -> p h d", d=64), in0=Os3[:, :, 0:64],
                                                       in1=bc(sm[:, 12:16].unsqueeze(2), [128, 4, 64]), op=ALU.mult),
                     reads=[bOs, bsm], writes=[bstg])
                k.op("dve", lambda e: e.tensor_tensor(out=stg[:, 0, :], in0=stg[:, 0, :], in1=stg[:, 1, :], op=ALU.add),
                     reads=[bstg], writes=[bstg])

            def win_(j):
                c_ = ctx(j); u = c_["u"]; tok = c_["tok"]; sm = c_["sm"]; bsm = c_["bsm"]; stg = c_["stg"]; bstg = c_["bstg"]; gv = c_["gv"]
                units = []
                for kt in range(max(u - 4, 0), u + 1):
                    ksl = slice(kt * 128, (kt + 1) * 128)
                    sc = []
                    if kt == u:
                        sc.append(((0, 512), st.identb[:, :], trineg[:, 0, :], [st.b_identb, btri]))
                    if kt == u - 4:
                        sc.append(((0, 512), st.identb[:, :], trineg[:, 1, :], [st.b_identb, btri]))
                    sc.append(((0, 512), st.KT[:, g, ksl], qTw[:, :, tok], [st.b_KT[kt // 4], bqT]))
                    units.append(dict(score=sc, v=(st.Vw[:, kt, g, :], [st.b_V[kt]])))
                Ow = st.pb[3]; bOw = st.bpb[3]
                run_branch(units, Ow, bOw, True)
                Ow3 = Ow[:, 0:260].rearrange("p (h d) -> p h d", d=65)
                k.op("dve", lambda e: e.tensor_scalar(out=sm[:, 8:12], in0=Ow3[:, :, 64], scalar1=1e-30, scalar2=None, op0=ALU.max),
                     reads=[bOw], writes=[bsm])
                k.op("dve", lambda e: e.reciprocal(out=sm[:, 8:12], in_=sm[:, 8:12]), reads=[bsm], writes=[bsm])
                k.op("dve", lambda e: e.tensor_tensor(out=sm[:, 12:16], in0=sm[:, 8:12], in1=gv[:, 4 * g:4 * g + 4, 2], op=ALU.mult),
                     reads=[bsm, bgsb], writes=[bsm])
                k.op("dve", lambda e: e.tensor_tensor(out=stg[:, 1, :].rearrange("p (h d) -> p h d", d=64), in0=Ow3[:, :, 0:64],
                                                       in1=bc(sm[:, 12:16].unsqueeze(2), [128, 4, 64]), op=ALU.mult),
                     reads=[bOw, bsm], writes=[bstg])
                stgb = stgbA[:, j % 2, :]; bstgb = bstgbA[j % 2]
                k.op("dve", lambda e: e.tensor_tensor(out=stgb[:, :], in0=stg[:, 0, :], in1=stg[:, 1, :], op=ALU.add),
                     reads=[bstg], writes=[bstgb])

            def fin_(j):
                tok = slice(j * 128, (j + 1) * 128)
                stgb = stgbA[:, j % 2, :]; bstgb = bstgbA[j % 2]
                for t2 in range(2):
                    k.op("pe", lambda e: e.transpose(out=tpn[:, t2 * 128:(t2 + 1) * 128], in_=stgb[:, t2 * 128:(t2 + 1) * 128],
                                                     identity=st.identb[:, :]),
                         reads=[bstgb, st.b_identb], writes=[btpn], sig=(t2 == 1))
                k.op("act", lambda e: e.activation(out=st.nsaT[:, 2 * g:2 * g + 2, tok],
                                                    in_=tpn[:, 0:256].rearrange("p (a t) -> p a t", a=2), func=AF.Copy),
                     reads=[btpn], writes=[st.b_nsaT])

            cmp_a1(0)
            cmp_a2(0)
            cmp_b(0)
            for j in range(NJ):
                if j + 1 < NJ:
                    cmp_a1(j + 1)
                slc_(j)
                if j + 1 < NJ:
                    cmp_a2(j + 1)
                win_(j)
                if j + 1 < NJ:
                    cmp_b(j + 1)
                if j >= 1:
                    fin_(j - 1)
            fin_(NJ - 1)
        k.barrier()


def phase_post(st, out_ap):
    k, nc, P = st.k, st.nc, st.P
    din = P.din
    WS = [k.sb("wslot%d" % i, [128, NKT, 512], BF16) for i in range(4)]
    bWS = [k.buf() for _ in range(4)]
    wsi = [0]

    def wslot():
        i = wsi[0] % 4
        wsi[0] += 1
        return WS[i], bWS[i]

    mergedT = k.sb("mergedT", [128, NKT, NTOK], BF16); bmT = [k.buf() for _ in range(NKT)]
    with contextlib.ExitStack() as es:
        hTo = k.sb("hTo", [128, NKT, NTOK], BF16, es); bhTo = [k.buf() for _ in range(NJ)]
        xst = [k.sb("xst%d" % i, [128, D], F32, es) for i in range(2)]; bxst = [k.buf() for _ in range(2)]
        xbs = [(k.sb("xb%d" % i, [128, D], BF16, es), k.buf()) for i in range(2)]
        for j in range(NJ):
            if j % 2 == 0:
                for j2 in range(j, j + 2):
                    u = 4 * j2 + 3
                    xi = j2 % 2
                    k.dma("sp", xst[xi][:, :], din["xs"][u * 128:(u + 1) * 128, :], writes=[bxst[xi]])
                    norm_front(st, xst[xi][:, :], [bxst[xi]], xbs[j2 % 2], xbs[j2 % 2])
            norm_back(st, xbs[j % 2], st.gains["ln_mix"], hTo[:, :, j * 128:(j + 1) * 128], bhTo[j],
                      (st.tp_view[j % 2][:, :], st.b_tp[j % 2]))
        tmp = [k.sb("b1tmp%d" % i, [128, 2, 512], F32, es) for i in range(2)]; btmp = [k.buf() for _ in range(2)]
        ti = 0
        pbi = 0
        for cc in range(2):
            W, bW = wslot()
            k.dma("pool", W[:, :, :], din["w_in"][:, O_LY + cc * 512:O_LY + (cc + 1) * 512].rearrange("(kt p) n -> p kt n", p=128),
                  writes=[bW])
            for ct in range(4):
                for half in range(2):
                    hs = slice(half * 512, (half + 1) * 512)
                    pj = st.pb[pbi % 4]; bpj = st.bpb[pbi % 4]; pbi += 1
                    mm_group(k, pj[:, :], [(W[:, kk, ct * 128:(ct + 1) * 128], hTo[:, kk, hs]) for kk in range(NKT)],
                             reads=[bW] + bhTo, writes=[bpj])
                    T = tmp[ti % 2]; bT = btmp[ti % 2]; ti += 1
                    k.op("act", lambda e: e.activation(out=T[:, 0, :], in_=pj[:, :], func=AF.Gelu_apprx_tanh), reads=[bpj], writes=[bT])
                    k.op("dve", lambda e: e.tensor_tensor(out=st.hstate[:, cc * 4 + ct, hs], in0=st.hstate[:, cc * 4 + ct, hs],
                                                           in1=T[:, 0, :], op=ALU.mult), reads=[bT, st.b_hstate], writes=[st.b_hstate])
        lruT = st.hstate
        tpfB = [st.tp_view[i][:, :].bitcast(F32) for i in range(2)]
        banksets = [([st.pb[i][:, :] for i in range(4)], [st.bpb[i] for i in range(4)]),
                    ([tpfB[0][:, 0:512], tpfB[0][:, 512:1024], tpfB[1][:, 0:512], tpfB[1][:, 512:1024]], [k.buf() for _ in range(4)])]
        mi_ = [0]
        k.barrier()
        for dc in range(4):
            cs = slice(dc * 512, (dc + 1) * 512)
            Wn, bWn = wslot(); Wl, bWl = wslot(); Wa, bWa = wslot(); Wb, bWb = wslot()
            Wn3 = Wn[:, 0:8, :]; Wl3 = Wl[:, 0:8, :]
            k.dma("pool", Wn3, din["w_nsa_up"][:, cs].rearrange("(kt p) n -> p kt n", p=128), writes=[bWn])
            k.dma("pool", Wl3, din["w_lru_up"][:, cs].rearrange("(kt p) n -> p kt n", p=128), writes=[bWl])
            k.dma("pool", Wa[:, :, :], din["w_in"][:, O_MA + dc * 512:O_MA + (dc + 1) * 512].rearrange("(kt p) n -> p kt n", p=128),
                  writes=[bWa])
            k.dma("pool", Wb[:, :, :], din["w_in"][:, O_MB + dc * 512:O_MB + (dc + 1) * 512].rearrange("(kt p) n -> p kt n", p=128),
                  writes=[bWb])
            for dt in range(4):
                ds_ = slice(dt * 128, (dt + 1) * 128)
                for half in range(2):
                    hs = slice(half * 512, (half + 1) * 512)
                    PB, BPB = banksets[mi_[0] % 2]
                    mi_[0] += 1
                    mm_group(k, PB[0], [(Wn3[:, kk, ds_], st.nsaT[:, kk, hs]) for kk in range(8)],
                             reads=[bWn, st.b_nsaT], writes=[BPB[0]])
                    mm_group(k, PB[1], [(Wl3[:, kk, ds_], lruT[:, kk, hs]) for kk in range(8)],
                             reads=[bWl, st.b_hstate], writes=[BPB[1]])
                    mm_group(k, PB[2], [(Wa[:, kk, ds_], hTo[:, kk, hs]) for kk in range(NKT)],
                             reads=[bWa] + bhTo, writes=[BPB[2]])
                    mm_group(k, PB[3], [(Wb[:, kk, ds_], hTo[:, kk, hs]) for kk in range(NKT)],
                             reads=[bWb] + bhTo, writes=[BPB[3]])
                    T = tmp[ti % 2]; bT = btmp[ti % 2]; ti += 1
                    k.op("act", lambda e: e.activation(out=T[:, 0, :], in_=PB[2], func=AF.Sigmoid), reads=[BPB[2]], writes=[bT])
                    k.op("act", lambda e: e.activation(out=T[:, 1, :], in_=PB[3], func=AF.Sigmoid), reads=[BPB[3]], writes=[bT])
                    k.op("dve", lambda e: e.tensor_tensor(out=T[:, 0, :], in0=PB[0], in1=T[:, 0, :], op=ALU.mult),
                         reads=[BPB[0], bT], writes=[bT])
                    k.op("dve", lambda e: e.tensor_tensor(out=T[:, 1, :], in0=PB[1], in1=T[:, 1, :], op=ALU.mult),
                         reads=[BPB[1], bT], writes=[bT])
                    k.op("dve", lambda e: e.tensor_tensor(out=mergedT[:, dc * 4 + dt, hs], in0=T[:, 0, :], in1=T[:, 1, :], op=ALU.add),
                         reads=[bT], writes=[bmT[dc * 4 + dt]])
        k.barrier()
    k.sb_free("hstate"); k.sb_free("nsaT")
    acc = k.sb("acc", [128, NJ, D], F32); bacc = [k.buf() for _ in range(NJ)]
    for j in range(NJ):
        u = 4 * j + 3
        k.dma("sp", acc[:, j, :], din["xs"][u * 128:(u + 1) * 128, :], writes=[bacc[j]])
    pbi = 0
    for dc in range(4):
        cs = slice(dc * 512, (dc + 1) * 512)
        W, bW = wslot()
        k.dma("pool", W[:, :, :], din["w_out"][:, cs].rearrange("(kt p) n -> p kt n", p=128), writes=[bW])
        for j in range(NJ):
            pj = st.pb[pbi % 4]; bpj = st.bpb[pbi % 4]; pbi += 1
            mm_group(k, pj[:, :], [(mergedT[:, kk, j * 128:(j + 1) * 128], W[:, kk, :]) for kk in range(NKT)],
                     reads=[bW] + bmT, writes=[bpj])
            k.op("dve", lambda e: e.tensor_tensor(out=acc[:, j, cs], in0=pj[:, :], in1=acc[:, j, cs], op=ALU.add),
                 reads=[bpj, bacc[j]], writes=[bacc[j]])
    k.barrier()
    k.sb_free("mergedT")
    xnT = k.sb("xnT", [128, NKT, NTOK], BF16); bxnT = [k.buf() for _ in range(NJ)]
    comb = k.sb("comb", [128, NJ, 32], F32); bcomb = k.buf()
    tpf = [st.tp_view[i][:, :].bitcast(F32) for i in range(2)]
    slots = {}

    def load(kind, e):
        W, bW = wslot()
        if kind == "g":
            src = din["w_gate"][e].rearrange("(kt p) n -> p kt n", p=128); dst = W[:, :, :]
        elif kind == "u":
            src = din["w_up"][e].rearrange("(kt p) n -> p kt n", p=128); dst = W[:, :, :]
        else:
            src = din["w_down"][e].rearrange("(ft p) n -> p ft n", p=128)
            dst = W[:, :, :].rearrange("p a b -> p (a b)").rearrange("p (f n) -> p f n", f=4)
        k.dma("pool", dst, src, writes=[bW])
        slots[(kind, e)] = (dst, bW)

    load("g", 0); load("u", 0); load("d", 0)
    with contextlib.ExitStack() as es:
        Wr = k.sb("Wr", [128, NKT, 36], F32, es); bWr = k.buf()
        k.dma("sp", Wr[:, :, :], din["w_r"].rearrange("(kt p) n -> p kt n", p=128), writes=[bWr])
        br = k.sb("br", [128, 36], F32, es); bbr = k.buf()
        k.dma("sp", br[:, :], din["b_r"].partition_broadcast(128), writes=[bbr])
        xs32 = k.sb("xs32", [128, D], F32, es); bxs32 = k.buf()
        xT32 = k.sb("xT32", [128, NKT, 128], F32, es); bxT32 = k.buf()
        junk = (k.sb("junkb", [128, D], BF16, es), k.buf())
        rsA = k.sb("rsm", [128, NJ, 80], F32, es); brs = k.buf()
        rtmp = k.sb("rtmp", [128, NJ, 4, 8], F32, es); brtmp = k.buf()
        gffn, bgffn = st.gains["ln_ffn"]
        for j in range(NJ):
            i = st.nt_i % 4; st.nt_i += 1
            sv = st.stat[:, i, :]; bs = st.bstat[i]
            k.op("act", lambda e: e.activation(out=junk[0][:, :], in_=acc[:, j, :], func=AF.Square, accum_out=sv[:, 0:1]),
                 reads=[bacc[j]], writes=[junk[1], bs])
            k.op("dve", lambda e: e.tensor_scalar(out=sv[:, 1:2], in0=sv[:, 0:1], scalar1=1.0 / D, scalar2=EPS, op0=ALU.mult, op1=ALU.add),
                 reads=[bs], writes=[bs])
            k.op("pool", lambda e: e.tensor_tensor(out=sv[:, 3:4], in0=sv[:, 1:2], in1=st.cneg[:, 0:1], op=ALU.pow), reads=[bs, st.b_cneg], writes=[bs])
            k.op("dve", lambda e: e.tensor_scalar(out=xs32[:, :], in0=acc[:, j, :], scalar1=sv[:, 3:4], scalar2=None, op0=ALU.mult),
                 reads=[bacc[j], bs], writes=[bxs32])
            for hf in range(2):
                tp_ = tpf[hf]; btp_ = st.b_tp[hf]
                for kk in range(8):
                    kq = hf * 8 + kk
                    k.op("pe", lambda e: e.transpose(out=tp_[:, kk * 128:(kk + 1) * 128], in_=xs32[:, kq * 128:(kq + 1) * 128],
                                                     identity=st.identf[:, :]),
                         reads=[bxs32, st.b_identf], writes=[btp_], sig=(kk == 7))
                k.op("dve", lambda e: e.tensor_tensor(out=xT32[:, hf * 8:(hf + 1) * 8, :], in0=tp_[:, :].rearrange("p (k t) -> p k t", t=128),
                                                       in1=bc(gffn[:, hf * 8:(hf + 1) * 8].unsqueeze(2), [128, 8, 128]), op=ALU.mult),
                     reads=[btp_, bgffn], writes=[bxT32])
            k.op("pool", lambda e: e.tensor_copy(out=xnT[:, :, j * 128:(j + 1) * 128], in_=xT32[:, :, :]), reads=[bxT32], writes=[bxnT[j]])
            pr = st.pb[j % 2]; bpr = st.bpb[j % 2]
            mm_group(k, pr[:, 0:36], [(xT32[:, kk, :], Wr[:, kk, :]) for kk in range(NKT)], reads=[bxT32, bWr], writes=[bpr])
            k.op("dve", lambda e: e.tensor_tensor(out=rsA[:, j, 0:36], in0=pr[:, 0:36], in1=br[:, :], op=ALU.add), reads=[bpr, bbr], writes=[brs])
        V = lambda a_, b_: rsA[:, :, a_:b_]
        R = [brs]
        k.op("dve", lambda e: e.reduce_max(out=V(36, 37), in_=V(0, 4), axis=AX.X), reads=R, writes=R)
        k.op("dve", lambda e: e.tensor_tensor(out=V(40, 44), in0=V(0, 4), in1=bc(V(36, 37), [128, NJ, 4]), op=ALU.subtract), reads=R, writes=R)
        k.op("act", lambda e: e.activation(out=V(40, 44), in_=V(40, 44), func=AF.Exp), reads=R, writes=R)
        k.op("dve", lambda e: e.reduce_sum(out=V(38, 39), in_=V(40, 44), axis=AX.X), reads=R, writes=R)
        k.op("dve", lambda e: e.reciprocal(out=V(39, 40), in_=V(38, 39)), reads=R, writes=R)
        k.op("dve", lambda e: e.tensor_tensor(out=V(44, 48), in0=V(0, 4), in1=bc(V(36, 37), [128, NJ, 4]), op=ALU.is_ge), reads=R, writes=R)
        k.op("dve", lambda e: e.tensor_tensor(out=rtmp[:, :, :, :], in0=V(4, 36).rearrange("p j (g x) -> p j g x", g=4),
                                               in1=bc(V(44, 48).unsqueeze(3), [128, NJ, 4, 8]), op=ALU.mult), reads=R, writes=[brtmp])
        k.op("dve", lambda e: e.reduce_sum(out=V(48, 56), in_=rtmp[:, :, :, :].rearrange("p j g x -> p j x g"), axis=AX.X),
             reads=[brtmp], writes=R)
        k.op("dve", lambda e: e.reduce_max(out=V(56, 57), in_=V(48, 56), axis=AX.X), reads=R, writes=R)
        k.op("dve", lambda e: e.tensor_tensor(out=V(48, 56), in0=V(48, 56), in1=bc(V(56, 57), [128, NJ, 8]), op=ALU.subtract), reads=R, writes=R)
        k.op("act", lambda e: e.activation(out=V(48, 56), in_=V(48, 56), func=AF.Exp), reads=R, writes=R)
        k.op("dve", lambda e: e.reduce_max(out=V(59, 60), in_=V(48, 56), axis=AX.X), reads=R, writes=R)
        k.op("dve", lambda e: e.tensor_tensor(out=V(64, 72), in0=V(48, 56), in1=bc(V(59, 60), [128, NJ, 8]), op=ALU.is_ge), reads=R, writes=R)
        k.op("dve", lambda e: e.tensor_scalar(out=V(64, 72), in0=V(64, 72), scalar1=-2.0, scalar2=None, op0=ALU.mult), reads=R, writes=R)
        k.op("dve", lambda e: e.tensor_tensor(out=V(64, 72), in0=V(64, 72), in1=V(48, 56), op=ALU.add), reads=R, writes=R)
        k.op("dve", lambda e: e.reduce_max(out=V(57, 58), in_=V(64, 72), axis=AX.X), reads=R, writes=R)
        k.op("dve", lambda e: e.tensor_tensor(out=V(58, 59), in0=V(57, 58), in1=V(59, 60), op=ALU.add), reads=R, writes=R)
        k.op("dve", lambda e: e.reciprocal(out=V(58, 59), in_=V(58, 59)), reads=R, writes=R)
        k.op("dve", lambda e: e.tensor_tensor(out=V(58, 59), in0=V(58, 59), in1=V(39, 40), op=ALU.mult), reads=R, writes=R)
        k.op("dve", lambda e: e.tensor_tensor(out=V(72, 80), in0=V(48, 56), in1=bc(V(57, 58), [128, NJ, 8]), op=ALU.is_ge), reads=R, writes=R)
        k.op("dve", lambda e: e.tensor_tensor(out=V(72, 80), in0=V(72, 80), in1=V(48, 56), op=ALU.mult), reads=R, writes=R)
        k.op("dve", lambda e: e.tensor_tensor(out=V(72, 80), in0=V(72, 80), in1=bc(V(58, 59), [128, NJ, 8]), op=ALU.mult), reads=R, writes=R)
        k.op("dve", lambda e: e.tensor_tensor(out=comb[:, :, :].rearrange("p j (g x) -> p j g x", g=4),
                                               in0=bc(V(44, 48).unsqueeze(3), [128, NJ, 4, 8]),
                                               in1=bc(V(72, 80).unsqueeze(2), [128, NJ, 4, 8]), op=ALU.mult),
             reads=R, writes=[bcomb])
        k.barrier()
    with contextlib.ExitStack() as es:
        hidT = k.sb("hidT", [128, 4, NTOK], BF16, es); bhid = [k.buf() for _ in range(4)]
        sgt = [k.sb("sgt%d" % i, [128, 512], F32, es) for i in range(2)]; bsgt = [k.buf() for _ in range(2)]
        ob = [tpf[0][:, 0:512], tpf[0][:, 512:1024], tpf[1][:, 0:512], tpf[1][:, 512:1024]]
        bob = [k.buf() for _ in range(4)]
        NE = st.n_experts
        gi = 0
        oi = 0
        for e in range(NE):
            if e + 1 < NE:
                load("g", e + 1)
            Wg, bWg = slots.pop(("g", e)); Wu, bWu = slots.pop(("u", e))
            for f in range(4):
                fs = slice(f * 128, (f + 1) * 128)
                for half in range(2):
                    hs = slice(half * 512, (half + 1) * 512)
                    pg = st.pb[(gi % 2) * 2]; bpg = st.bpb[(gi % 2) * 2]
                    pu = st.pb[(gi % 2) * 2 + 1]; bpu = st.bpb[(gi % 2) * 2 + 1]
                    S_ = sgt[gi % 2]; bS_ = bsgt[gi % 2]
                    gi += 1
                    mm_group(k, pg[:, :], [(Wg[:, kk, fs], xnT[:, kk, hs]) for kk in range(NKT)], reads=[bWg] + bxnT, writes=[bpg])
                    mm_group(k, pu[:, :], [(Wu[:, kk, fs], xnT[:, kk, hs]) for kk in range(NKT)], reads=[bWu] + bxnT, writes=[bpu])
                    k.op("act", lambda e_: e_.activation(out=S_[:, :], in_=pg[:, :], func=AF.Silu), reads=[bpg], writes=[bS_])
                    k.op("dve", lambda e_: e_.tensor_tensor(out=hidT[:, f, hs], in0=pu[:, :], in1=S_[:, :], op=ALU.mult),
                         reads=[bpu, bS_], writes=[bhid[f]])
            if e + 1 < NE:
                load("u", e + 1); load("d", e + 1)
            Wd, bWd = slots.pop(("d", e))
            for j in range(NJ):
                for dc in range(4):
                    cs = slice(dc * 512, (dc + 1) * 512)
                    O_ = ob[oi % 4]; bO_ = bob[oi % 4]; oi += 1
                    mm_group(k, O_, [(hidT[:, f, j * 128:(j + 1) * 128], Wd[:, f, cs]) for f in range(4)],
                             reads=[bWd] + bhid, writes=[bO_])
                    k.op("dve", lambda e_: e_.scalar_tensor_tensor(out=acc[:, j, cs], in0=O_, scalar=comb[:, j, e:e + 1], in1=acc[:, j, cs],
                                                                   op0=ALU.mult, op1=ALU.add),
                         reads=[bO_, bcomb, bacc[j]], writes=[bacc[j]])
        k.barrier()
    with contextlib.ExitStack() as es:
        xb = (k.sb("xb0", [128, D], BF16, es), k.buf())
        for j in range(NJ):
            norm_transpose(st, acc[:, j, :], [bacc[j]], st.gains["ln_ple"], xnT[:, :, j * 128:(j + 1) * 128], bxnT[j],
                           None, xb, (st.tp_view[j % 2][:, :], st.b_tp[j % 2]), xb)
        pT = k.sb("pT", [128, 2, NTOK], BF16, es); bpT = k.buf()
        pst = k.sb("pst", [128, 256], F32, es); bpst = k.buf()
        pstb = k.sb("pstb", [128, 256], BF16, es); bpstb = k.buf()
        for j in range(NJ):
            k.dma("sp", pst[:, :], din["pown"][j * 128:(j + 1) * 128, :], writes=[bpst])
            k.op("dve", lambda e: e.tensor_copy(out=pstb[:, :], in_=pst[:, :]), reads=[bpst], writes=[bpstb])
            tp_ = st.tp_view[j % 2]; btp_ = st.b_tp[j % 2]
            for t2 in range(2):
                k.op("pe", lambda e: e.transpose(out=tp_[:, t2 * 128:(t2 + 1) * 128], in_=pstb[:, t2 * 128:(t2 + 1) * 128], identity=st.identb[:, :]),
                     reads=[bpstb, st.b_identb], writes=[btp_], sig=(t2 == 1))
            k.op("act", lambda e: e.activation(out=pT[:, :, j * 128:(j + 1) * 128], in_=tp_[:, 0:256].rearrange("p (a t) -> p a t", a=2),
                                                func=AF.Copy), reads=[btp_], writes=[bpT])
        Wp = k.sb("Wp", [128, 2, D], BF16, es); bWp = k.buf()
        k.dma("pool", Wp[:, :, :], din["w_ple"].rearrange("(kt p) n -> p kt n", p=128), writes=[bWp])
        sg2 = [k.sb("sg2_%d" % i, [128, 512], F32, es) for i in range(2)]; bsg2 = [k.buf() for _ in range(2)]
        gi = 0
        for dc in range(4):
            cs = slice(dc * 512, (dc + 1) * 512)
            W, bW = wslot()
            k.dma("pool", W[:, :, :], din["w_ple_gate"][:, cs].rearrange("(kt p) n -> p kt n", p=128), writes=[bW])
            for j in range(NJ):
                ts_ = slice(j * 128, (j + 1) * 128)
                pg = st.pb[(gi % 2) * 2]; bpg = st.bpb[(gi % 2) * 2]
                pp = st.pb[(gi % 2) * 2 + 1]; bpp = st.bpb[(gi % 2) * 2 + 1]
                S_ = sg2[gi % 2]; bS_ = bsg2[gi % 2]
                gi += 1
                mm_group(k, pg[:, :], [(xnT[:, kk, ts_], W[:, kk, :]) for kk in range(NKT)], reads=[bW, bxnT[j]], writes=[bpg])
                mm_group(k, pp[:, :], [(pT[:, kk, ts_], Wp[:, kk, cs]) for kk in range(2)], reads=[bWp, bpT], writes=[bpp])
                k.op("act", lambda e: e.activation(out=S_[:, :], in_=pg[:, :], func=AF.Sigmoid), reads=[bpg], writes=[bS_])
                k.op("dve", lambda e: e.tensor_tensor(out=S_[:, :], in0=pp[:, :], in1=S_[:, :], op=ALU.mult), reads=[bpp, bS_], writes=[bS_])
                k.op("dve", lambda e: e.tensor_tensor(out=acc[:, j, cs], in0=acc[:, j, cs], in1=S_[:, :], op=ALU.add),
                     reads=[bS_, bacc[j]], writes=[bacc[j]])
        k.barrier()
    k.sb_free("xnT")
    with contextlib.ExitStack() as es:
        gfin = k.sb("gfin", [128, D], F32, es); bgfin = k.buf()
        k.dma("sp", gfin[:, :], din["ln_final"].partition_broadcast(128), writes=[bgfin])
        ost = [k.sb("ost%d" % i, [128, D], F32, es) for i in range(2)]; bost = [k.buf() for _ in range(2)]
        junk = (k.sb("junkc", [128, D], BF16, es), k.buf())
        for j in range(NJ):
            i = st.nt_i % 4; st.nt_i += 1
            sv = st.stat[:, i, :]; bs = st.bstat[i]
            k.op("act", lambda e: e.activation(out=junk[0][:, :], in_=acc[:, j, :], func=AF.Square, accum_out=sv[:, 0:1]),
                 reads=[bacc[j]], writes=[junk[1], bs])
            k.op("dve", lambda e: e.tensor_scalar(out=sv[:, 1:2], in0=sv[:, 0:1], scalar1=1.0 / D, scalar2=EPS, op0=ALU.mult, op1=ALU.add),
                 reads=[bs], writes=[bs])
            k.op("pool", lambda e: e.tensor_tensor(out=sv[:, 3:4], in0=sv[:, 1:2], in1=st.cneg[:, 0:1], op=ALU.pow), reads=[bs, st.b_cneg], writes=[bs])
            O_ = ost[j % 2]; bO_ = bost[j % 2]
            k.op("dve", lambda e: e.scalar_tensor_tensor(out=O_[:, :], in0=acc[:, j, :], scalar=sv[:, 3:4], in1=gfin[:, :],
                                                          op0=ALU.mult, op1=ALU.mult), reads=[bacc[j], bs, bgfin], writes=[bO_])
            k.dma("sp", out_ap[j * 128:(j + 1) * 128, :], O_[:, :], reads=[bO_], is_output=True)
    for i in range(4):
        k.sb_free("wslot%d" % i)
    k.sb_free("acc"); k.sb_free("comb")
```

```python
import contextlib
import numpy as np
import concourse.bass as bass
import concourse.mybir as mybir
from concourse.bass_utils import run_bass_kernel_spmd

F32 = mybir.dt.float32
BF16 = mybir.dt.bfloat16
I32 = mybir.dt.int32
AF = mybir.ActivationFunctionType
ALU = mybir.AluOpType
AX = mybir.AxisListType

SAME_ENGINE_SYNC = True
N_DMA_SEMS = 48


class Buf:
    __slots__ = ("name", "w", "r")

    def __init__(self, name=""):
        self.name = name
        self.w = []
        self.r = {}


class Eng:
    def __init__(self, name, h, sem):
        self.name = name
        self.h = h
        self.sem = sem
        self.count = 0
        self.seen = {}


class K:
    def __init__(self, nc, es):
        self.nc = nc
        self.es = es
        self.E = {}
        for name, h in (("pe", nc.tensor), ("act", nc.scalar), ("dve", nc.vector),
                        ("pool", nc.gpsimd), ("sp", nc.sync)):
            sem = es.enter_context(nc.semaphore("sem_" + name))
            self.E[name] = Eng(name, h, sem)
        self.dsem = {c: [es.enter_context(nc.semaphore("dsem_%s%d" % (c, i))) for i in range(N_DMA_SEMS // 2)] for c in ("sw", "hw")}
        self.dma_i = {"sw": 0, "hw": 0}
        self.dma_tix = {"sw": [], "hw": []}
        self.dma_n = 0
        self.out_tix = []
        self.nbuf = 0
        self._bscr = self.sb("bar_scr", [128, 8], F32)
        self._bb = {n: self.buf("bar_" + n) for n in ("pe", "act", "dve", "pool")}
        self.op("dve", lambda e: e.memset(self._bscr[:, :], 0.0), writes=list(self._bb.values()))

    ARENA = 204 * 1024

    def _arena_init(self):
        self.arena = self.es.enter_context(self.nc.sbuf_tensor("arena", [128, self.ARENA // 2], BF16))
        self.free_list = [(0, self.ARENA)]
        self.live = {}

    def sb(self, name, shape, dt, es=None):
        if not hasattr(self, "arena"):
            self._arena_init()
        esz = {F32: 4, BF16: 2, I32: 4}[dt]
        n = 1
        for d in shape[1:]:
            n *= d
        nbytes = (n * esz + 63) // 64 * 64
        for idx, (off, sz) in enumerate(self.free_list):
            if sz >= nbytes:
                break
        else:
            raise RuntimeError("arena full allocating %s (%d B); live=%s" % (name, nbytes, sorted((v[1], k_) for k_, v in self.live.items())))
        if sz == nbytes:
            self.free_list.pop(idx)
        else:
            self.free_list[idx] = (off + nbytes, sz - nbytes)
        assert name not in self.live, name
        self.live[name] = (off, nbytes)
        v = self.arena[0:shape[0], off // 2:(off + n * esz) // 2]
        if dt != BF16:
            v = v.bitcast(dt)
        if len(shape) > 2:
            names = " ".join("d%d" % i for i in range(1, len(shape)))
            kw = {"d%d" % i: shape[i] for i in range(2, len(shape))}
            v = v.rearrange("p (%s) -> p %s" % (names, names), **kw)
        if es is not None:
            es.callback(self.sb_free, name)
        return v

    def sb_free(self, name):
        off, nbytes = self.live.pop(name)
        fl = self.free_list + [(off, nbytes)]
        fl.sort()
        merged = []
        for o, z in fl:
            if merged and merged[-1][0] + merged[-1][1] == o:
                merged[-1] = (merged[-1][0], merged[-1][1] + z)
            else:
                merged.append((o, z))
        self.free_list = merged

    def ps(self, name, shape, dt, es=None):
        return (es or self.es).enter_context(self.nc.psum_tensor("ps_" + name, list(shape), dt))

    def buf(self, name=""):
        self.nbuf += 1
        return Buf(name)

    def _need(self, E, t):
        sem, val, ename = t
        if ename == E.name:
            if E.name == "pe" or not SAME_ENGINE_SYNC:
                return
        if ename is not None:
            assert self.E[ename].count >= val, "waiting on unsignaled ticket of %s" % ename
        key = id(sem)
        if E.seen.get(key, 0) >= val:
            return
        E.h.wait_ge(sem, val)
        E.seen[key] = val

    def _deps(self, E, reads, writes):
        for b in reads:
            for t in b.w:
                self._need(E, t)
        for b in writes:
            for t in b.w:
                self._need(E, t)
            for t in b.r.values():
                self._need(E, t)

    def _record(self, t, reads, writes):
        key = id(t[0])
        for b in reads:
            old = b.r.get(key)
            if old is None or old[1] < t[1]:
                b.r[key] = t
        for b in writes:
            b.w = [t]
            b.r = {}

    def op(self, eng, fn, reads=(), writes=(), sig=True):
        E = self.E[eng]
        self._deps(E, reads, writes)
        ins = fn(E.h)
        if sig:
            E.count += 1
            ins.then_inc(E.sem, 1)
            t = (E.sem, E.count, E.name)
        else:
            t = (E.sem, E.count + 1, E.name)
        self._record(t, reads, writes)
        return t

    def dma(self, queue, out, in_, reads=(), writes=(), is_output=False, add=False, **kw):
        E = self.E[queue]
        if add:
            self._deps(E, reads, ())
        else:
            self._deps(E, reads, writes)
        cls = "sw" if queue == "pool" else "hw"
        NS = N_DMA_SEMS // 2
        i = self.dma_i[cls]
        self.dma_i[cls] += 1
        self.dma_n += 1
        sem = self.dsem[cls][i % NS]
        val = 16 * (i // NS + 1)
        if i >= NS:
            self._need(E, self.dma_tix[cls][i - NS])
        E.h.dma_start(out=out, in_=in_, **kw).then_inc(sem, 16)
        t = (sem, val, None)
        self.dma_tix[cls].append(t)
        for b in reads:
            b.r[("d", self.dma_n)] = t
        for b in writes:
            if add:
                b.w = b.w + [t]
            else:
                b.w = [t]
                b.r = {}
        if is_output:
            self.out_tix.append(t)
        return t

    def barrier(self):
        names = ["pe", "act", "dve", "pool"]
        sc = self._bscr
        self.op("act", lambda e: e.activation(out=sc[0:32, 0:1], in_=sc[0:32, 1:2], func=AF.Copy), writes=[self._bb["act"]])
        self.op("dve", lambda e: e.memset(sc[0:32, 2:3], 0.0), writes=[self._bb["dve"]])
        self.op("pool", lambda e: e.memset(sc[0:32, 3:4], 0.0), writes=[self._bb["pool"]])
        for n in names + ["sp"]:
            E = self.E[n]
            for m in names:
                if m != n:
                    F = self.E[m]
                    self._need(E, (F.sem, F.count, F.name))
            for c in ("sw", "hw"):
                for t in self.dma_tix[c][-(N_DMA_SEMS // 2):]:
                    self._need(E, t)

    def finish(self):
        E = self.E["sp"]
        for t in self.out_tix:
            self._need(E, t)


D = 2048
NKT = 16
S = 4096
NU = 32
NJ = 8
NTOK = 1024
HD = 64
IN_SPLITS = (1024, 256, 256, 256, 256, 256, 256, 48, 1024, 1024, 2048, 2048)
OFF = [0]
for _v in IN_SPLITS:
    OFF.append(OFF[-1] + _v)
(O_Q, O_KC, O_VC, O_KS, O_VS, O_KW, O_VW, O_G, O_LX, O_LY, O_MA, O_MB, O_END) = OFF
EPS = 1e-6


def bc(ap, shape):
    return ap.to_broadcast(list(shape))


class Prog:
    def __init__(self, dbg=None):
        self.dbg = dbg or {}
        nc = bass.Bass("TRN2", target_bir_lowering=False)
        self.nc = nc
        self.din = LazyIn(self)
        self.dout = {}

    def inp(self, name, shape, dt=F32):
        t = self.nc.dram_tensor(name, list(shape), dt, kind="ExternalInput").ap()
        self.din[name] = t
        return t

    def outp(self, name, shape, dt=F32):
        t = self.nc.dram_tensor(name, list(shape), dt, kind="ExternalOutput").ap()
        self.dout[name] = t
        return t


class St:
    pass


def mm_group(k, out_ap, pairs, reads, writes, sig_last=True):
    n = len(pairs)
    for i, (l, r) in enumerate(pairs):
        k.op("pe", lambda e: e.matmul(out_ap, l, r, start=(i == 0), stop=(i == n - 1)),
             reads=reads, writes=writes, sig=(sig_last and i == n - 1))


def mm1(k, out_ap, l, r, start, reads, writes, sig=False):
    k.op("pe", lambda e: e.matmul(out_ap, l, r, start=start, stop=True, skip_group_check=True),
         reads=reads, writes=writes, sig=sig)


IN_SHAPES = {
    "xs": ([S, D], F32), "pown": ([NTOK, 256], F32), "w_in": ([D, O_END], F32),
    "ln_mix": ([D], F32), "ln_ffn": ([D], F32), "ln_ple": ([D], F32), "ln_final": ([D], F32),
    "wa_bd": ([8, 128, 128], F32), "wx_bd": ([8, 128, 128], F32), "lru_small": ([128, 8, 8], F32),
    "cmp_k_pos": ([32, 64], F32), "cmp_k_w1": ([2048, 128], F32), "cmp_k_w2": ([128, 64], F32),
    "cmp_v_pos": ([32, 64], F32), "cmp_v_w1": ([2048, 128], F32), "cmp_v_w2": ([128, 64], F32),
    "w_nsa_up": ([1024, D], F32), "w_lru_up": ([1024, D], F32), "w_out": ([D, D], F32),
    "w_r": ([D, 36], F32), "b_r": ([36], F32),
    "w_gate": ([32, D, 512], F32), "w_up": ([32, D, 512], F32), "w_down": ([32, 512, D], F32),
    "w_ple": ([256, D], F32), "w_ple_gate": ([D, D], F32),
    "ident": ([128, 128], F32),
    "vrow": ([128, 512], F32),
    "vcol": ([128, NU], F32),
    "cmaskT": ([128, NJ, 2, 128], F32),
    "ovm": ([128, 2, 64], F32),
    "scV": ([128, NJ, 64], F32), "scN": ([128, NJ, 64], F32), "scF": ([128, NJ, 64], F32),
    "eall": ([128, S], BF16),
    "trineg": ([128, 2, 512], BF16),
}


class LazyIn(dict):
    def __init__(self, P):
        super().__init__()
        self.P = P

    def __missing__(self, name):
        shape, dt = IN_SHAPES[name]
        t = self.P.nc.dram_tensor(name, list(shape), dt, kind="ExternalInput").ap()
        self[name] = t
        return t


def setup(st):
    k, nc, P = st.k, st.nc, st.P
    din = P.din
    st.identf = k.sb("identf", [128, 128], F32); st.b_identf = k.buf()
    st.identb = k.sb("identb", [128, 128], BF16); st.b_identb = k.buf()
    k.dma("sp", st.identf[:], din["ident"], writes=[st.b_identf])
    k.dma("pool", st.identb[:], din["ident"], writes=[st.b_identb])
    st.gains = {}
    for nm in ("ln_mix", "ln_ffn", "ln_ple"):
        g = k.sb("g_" + nm, [128, NKT], F32)
        b = k.buf()
        k.dma("sp", g[:], din[nm].rearrange("(kt p) -> p kt", p=128), writes=[b],
              allow_slow_non_contiguous=True)
        st.gains[nm] = (g, b)
    st.tp_view = [k.ps("tp%d" % i, [128, 2048], BF16) for i in range(2)]
    st.b_tp = [k.buf("tp%d" % i) for i in range(2)]
    st.pb = [k.ps("pb%d" % i, [128, 512], F32) for i in range(4)]
    st.bpb = [k.buf("pb%d" % i) for i in range(4)]
    st.stat = k.sb("stat", [128, 4, 4], F32)
    st.bstat = [k.buf() for _ in range(4)]
    st.nt_i = 0
    st.cneg = k.sb("cneg", [128, 2], F32); st.b_cneg = k.buf()
    k.op("pool", lambda e: e.memset(st.cneg[:, 0:1], -0.5), writes=[st.b_cneg])
    k.op("pool", lambda e: e.memset(st.cneg[:, 1:2], 0.5), writes=[st.b_cneg])


def norm_front(st, src_ap, src_bufs, xb, junk, mul_eng="dve"):
    k = st.k
    i = st.nt_i % 4
    st.nt_i += 1
    sv = st.stat[:, i, :]
    bs = st.bstat[i]
    jt, bj = junk
    xbt, bxb = xb
    k.op("act", lambda e: e.activation(out=jt[:], in_=src_ap, func=AF.Square, accum_out=sv[:, 0:1]),
         reads=src_bufs, writes=[bj, bs])
    k.op("dve", lambda e: e.tensor_scalar(out=sv[:, 1:2], in0=sv[:, 0:1], scalar1=1.0 / D, scalar2=EPS,
                                           op0=ALU.mult, op1=ALU.add), reads=[bs], writes=[bs])
    k.op("pool", lambda e: e.tensor_tensor(out=sv[:, 3:4], in0=sv[:, 1:2], in1=st.cneg[:, 0:1], op=ALU.pow), reads=[bs, st.b_cneg], writes=[bs])
    k.op(mul_eng, lambda e: e.tensor_scalar(out=xbt[:], in0=src_ap, scalar1=sv[:, 3:4], scalar2=None, op0=ALU.mult),
         reads=list(src_bufs) + [bs], writes=[bxb])


def norm_back(st, xb, gain, dst_ap, dst_buf, tp_banks):
    k = st.k
    g, bg = gain
    xbt, bxb = xb
    tp, btp = tp_banks
    for kk in range(NKT):
        k.op("pe", lambda e: e.transpose(out=tp[:, kk * 128:(kk + 1) * 128], in_=xbt[:, kk * 128:(kk + 1) * 128],
                                         identity=st.identb[:]),
             reads=[bxb, st.b_identb], writes=[btp], sig=(kk == NKT - 1))
    k.op("dve", lambda e: e.tensor_tensor(out=dst_ap, in0=tp[:, :].rearrange("p (k t) -> p k t", t=128),
                                           in1=bc(g[:, :].unsqueeze(2), [128, NKT, 128]), op=ALU.mult),
         reads=[btp, bg], writes=[dst_buf])


def norm_transpose(st, src_ap, src_bufs, gain, dst_ap, dst_buf, xstage, xb, tp_banks, junk):
    norm_front(st, src_ap, src_bufs, xb, junk)
    norm_back(st, xb, gain, dst_ap, dst_buf, tp_banks)


def phase_lru(st):
    k, nc, P = st.k, st.nc, st.P
    din = P.din
    with contextlib.ExitStack() as es:
        Wlx = k.sb("Wlx", [128, NKT, 1024], BF16, es); bWlx = k.buf()
        for q4 in range(4):
            k.dma("pool", Wlx[:, q4 * 4:(q4 + 1) * 4, :],
                  din["w_in"][q4 * 512:(q4 + 1) * 512, O_LX:O_LX + 1024].rearrange("(kt p) n -> p kt n", p=128),
                  writes=[bWlx], add=(q4 > 0))
        WaBD = k.sb("WaBD", [128, 8, 128], F32, es); bWa = k.buf()
        WxBD = k.sb("WxBD", [128, 8, 128], F32, es); bWx = k.buf()
        k.dma("sp", WaBD[:], din["wa_bd"].rearrange("c p n -> p c n"), writes=[bWa])
        k.dma("sp", WxBD[:], din["wx_bd"].rearrange("c p n -> p c n"), writes=[bWx])
        sm = k.sb("lru_sm", [128, 8, 16], F32, es); bsm = k.buf()
        k.dma("sp", sm[:, :, 0:8], din["lru_small"], writes=[bsm])
        k.op("act", lambda e: e.activation(out=sm[:, :, 9:10], in_=sm[:, :, 7:8], func=AF.Exp, scale=-1.0),
             reads=[bsm], writes=[bsm])
        k.op("act", lambda e: e.activation(out=sm[:, :, 10:11], in_=sm[:, :, 9:10], func=AF.Ln, bias=1.0),
             reads=[bsm], writes=[bsm])
        k.op("dve", lambda e: e.tensor_scalar(out=sm[:, :, 8:9], in0=sm[:, :, 10:11], scalar1=-8.0, scalar2=None,
                                               op0=ALU.mult), reads=[bsm], writes=[bsm])
        k.op("dve", lambda e: e.tensor_scalar(out=sm[:, :, 11:13], in0=sm[:, :, 5:7], scalar1=0.5, scalar2=None, op0=ALU.mult),
             reads=[bsm], writes=[bsm])
        k.op("dve", lambda e: e.tensor_scalar(out=sm[:, :, 13:14], in0=sm[:, :, 10:11], scalar1=-4.0, scalar2=None, op0=ALU.mult),
             reads=[bsm], writes=[bsm])
        k.op("dve", lambda e: e.tensor_scalar(out=sm[:, :, 14:15], in0=sm[:, :, 10:11], scalar1=-8.0, scalar2=None, op0=ALU.mult),
             reads=[bsm], writes=[bsm])
        vrow = k.sb("vrow", [128, 512], F32, es); bvrow = k.buf()
        k.dma("sp", vrow[:], din["vrow"], writes=[bvrow])
        xbuf = k.sb("xbuf", [128, 8, 515], F32, es); bxbuf = [k.buf() for _ in range(8)]
        k.op("pool", lambda e: e.memset(xbuf[:, :, 0:3], 0.0), writes=bxbuf)
        carry = k.sb("carry", [128, 8], F32, es); bcarry = [k.buf() for _ in range(8)]
        k.op("pool", lambda e: e.memset(carry[:], 0.0), writes=bcarry)
        xst = [k.sb("xst%d" % i, [128, D], F32, es) for i in range(2)]; bxst = [k.buf() for _ in range(2)]
        xb = [(k.sb("xb%d" % i, [128, D], BF16, es), k.buf()) for i in range(1)]
        junk = (k.sb("junk", [128, D], BF16, es), k.buf())
        hT = [k.sb("hT%d" % i, [128, NKT, 512], BF16, es) for i in range(2)]
        bhT = [[k.buf() for _ in range(4)] for _ in range(2)]
        NT = 4
        tmp = [k.sb("ltmp%d" % i, [128, 5, 512], F32, es) for i in range(NT)]
        tmpb = [k.sb("ltmpb%d" % i, [128, 512], BF16, es) for i in range(NT)]
        btmp = [[k.buf() for _ in range(9)] for _ in range(NT)]
        nti = [0]

        def norms(c):
            hb = c % 2
            for uu in range(4):
                u = 4 * c + uu
                xi = nti[0] % 2
                k.dma("sp", xst[xi][:], din["xs"][u * 128:(u + 1) * 128, :], writes=[bxst[xi]])
                tpi = nti[0] % 2
                norm_front(st, xst[xi][:], [bxst[xi]], xb[0], junk)
                norm_back(st, xb[0], st.gains["ln_mix"], hT[hb][:, :, uu * 128:(uu + 1) * 128], bhT[hb][uu],
                          (st.tp_view[tpi][:, :], st.b_tp[tpi]))
                nti[0] += 1

        def stageA(c, ct):
            hb = c % 2
            s = (c * 8 + ct) % NT
            T = tmp[s]; B = btmp[s]
            pj = st.pb[ct % 2]; bpj = st.bpb[ct % 2]
            mm_group(k, pj[:, :], [(Wlx[:, kk, ct * 128:(ct + 1) * 128], hT[hb][:, kk, :]) for kk in range(NKT)],
                     reads=[bWlx] + bhT[hb], writes=[bpj])
            bx_ = bxbuf[ct]
            k.op("act", lambda e: e.activation(out=xbuf[:, ct, 3:515], in_=pj[:, :], func=AF.Copy),
                 reads=[bpj], writes=[bx_])
            xc = T[:, 0, :]
            k.op("act", lambda e: e.activation(out=xc, in_=pj[:, :], func=AF.Identity, scale=sm[:, ct, 3:4], bias=sm[:, ct, 4:5]),
                 reads=[bpj, bsm], writes=[B[0]])
            for w in (2, 1, 0):
                k.op("dve", lambda e: e.scalar_tensor_tensor(out=xc, in0=xbuf[:, ct, w:w + 512], scalar=sm[:, ct, w:w + 1],
                                                              in1=xc, op0=ALU.mult, op1=ALU.add),
                     reads=[bx_, bsm, B[0]], writes=[B[0]])
            k.op("pool", lambda e: e.tensor_copy(out=xbuf[:, ct, 0:3], in_=xbuf[:, ct, 512:515]),
                 reads=[bx_], writes=[bx_])

        def stageB1(c, ct):
            s = (c * 8 + ct) % NT
            T = tmp[s]; B = btmp[s]
            xc = T[:, 0, :]
            pr = st.pb[2]; pi_ = st.pb[3]
            k.op("pe", lambda e: e.matmul(pr[:, :], WaBD[:, ct, :], xc, start=True, stop=True),
                 reads=[bWa, B[0]], writes=[st.bpb[2]])
            k.op("pe", lambda e: e.matmul(pi_[:, :], WxBD[:, ct, :], xc, start=True, stop=True),
                 reads=[bWx, B[0]], writes=[st.bpb[3]])
            r_ = T[:, 1, :]; i_ = T[:, 2, :]; a_ = T[:, 3, :]; a2 = T[:, 4, :]
            k.op("act", lambda e: e.activation(out=r_, in_=pr[:, :], func=AF.Tanh, scale=0.5, bias=sm[:, ct, 11:12]),
                 reads=[st.bpb[2], bsm], writes=[B[1]])
            k.op("act", lambda e: e.activation(out=i_, in_=pi_[:, :], func=AF.Tanh, scale=0.5, bias=sm[:, ct, 12:13]),
                 reads=[st.bpb[3], bsm], writes=[B[2]])
            k.op("act", lambda e: e.activation(out=a_, in_=r_, func=AF.Exp, scale=sm[:, ct, 13:14], bias=sm[:, ct, 13:14]),
                 reads=[B[1], bsm], writes=[B[3]])
            k.op("act", lambda e: e.activation(out=a2, in_=r_, func=AF.Exp, scale=sm[:, ct, 14:15], bias=sm[:, ct, 14:15]),
                 reads=[B[1], bsm], writes=[B[4]])

        def stageB2(c, ct):
            s = (c * 8 + ct) % NT
            T = tmp[s]; B = btmp[s]
            xc = T[:, 0, :]
            hs = T[:, 1, :]; i_ = T[:, 2, :]; a_ = T[:, 3, :]; a2 = T[:, 4, :]
            mu = a2; u_ = i_
            k.op("act", lambda e: e.activation(out=mu, in_=a2, func=AF.Sqrt, scale=-1.0, bias=1.0 + 2.0 ** -22),
                 reads=[B[4]], writes=[B[4]])
            k.op("dve", lambda e: e.scalar_tensor_tensor(out=u_, in0=i_, scalar=1.0, in1=xc, op0=ALU.add, op1=ALU.mult),
                 reads=[B[2], B[0]], writes=[B[2]])
            k.op("dve", lambda e: e.scalar_tensor_tensor(out=u_, in0=u_, scalar=0.5, in1=mu, op0=ALU.mult, op1=ALU.mult),
                 reads=[B[2], B[4]], writes=[B[2]])
            if c == 0:
                k.op("dve", lambda e: e.tensor_tensor(out=u_, in0=u_, in1=vrow[:, :], op=ALU.mult),
                     reads=[B[2], bvrow], writes=[B[2]])
            k.op("dve", lambda e: e.tensor_tensor_scan(out=hs, data0=a_, data1=u_, initial=carry[:, ct:ct + 1],
                                                        op0=ALU.mult, op1=ALU.add),
                 reads=[B[3], B[2], bcarry[ct]], writes=[B[1]])
            k.op("dve", lambda e: e.tensor_copy(out=carry[:, ct:ct + 1], in_=hs[:, 511:512]),
                 reads=[B[1]], writes=[bcarry[ct]])
            k.op("pool", lambda e: e.tensor_copy(out=st.hstate[:, ct, c * 128:(c + 1) * 128], in_=hs[:, 384:512]),
                 reads=[B[1]], writes=[st.b_hstate])

        norms(0)
        for m in range(33):
            if m < 32:
                for n in (2 * m, 2 * m + 1):
                    stageA(n // 8, n % 8)
            if m >= 1:
                for n in (2 * m - 2, 2 * m - 1):
                    stageB1(n // 8, n % 8)
                for n in (2 * m - 2, 2 * m - 1):
                    stageB2(n // 8, n % 8)
            if m % 4 == 2 and m // 4 + 1 < 8:
                norms(m // 4 + 1)
        k.barrier()


def build(upto="all", dbg=False, n_experts=32):
    P = Prog()
    out = P.outp("out", [NTOK, D])
    nc = P.nc
    with contextlib.ExitStack() as es:
        k = K(nc, es)
        st = St()
        st.k, st.nc, st.P = k, nc, P
        setup(st)
        alloc_mixer(st)
        phase_kv(st)
        if dbg and upto == "kv":
            P.outp("d_KT", [128, 4, S], BF16); P.outp("d_Vs", [128, NU, 4, 65], BF16)
            P.outp("d_Vw", [128, NU, 4, 65], BF16); P.outp("d_kcv", [128, 4, 256], BF16)
            P.outp("d_VC", [128, 2, 4, 129], BF16)
            k.dma("sp", P.dout["d_KT"], st.KT[:, :, :], reads=st.b_KT, is_output=True)
            k.dma("sp", P.dout["d_Vs"], st.Vs[:, :, :, :], reads=st.b_V, is_output=True)
            k.dma("sp", P.dout["d_Vw"], st.Vw[:, :, :, :], reads=st.b_V, is_output=True)
            k.dma("sp", P.dout["d_kcv"], st.kcv[:, :, :], reads=[st.b_kcv], is_output=True)
            k.dma("sp", P.dout["d_VC"], st.VC[:, :, :, :], reads=[st.b_VC], is_output=True)
            k.finish()
            return P
        phase_attn(st)
        if dbg and upto == "attn":
            P.outp("d_nsaT", [128, 8, NTOK], BF16)
            k.dma("sp", P.dout["d_nsaT"], st.nsaT[:, :, :], reads=[st.b_nsaT], is_output=True)
            k.finish()
            return P
        free_mixer(st)
        st.hstate = k.sb("hstate", [128, 8, NTOK], BF16); st.b_hstate = k.buf()
        phase_lru(st)
        if dbg and upto == "lru":
            P.outp("d_hstate", [128, 8, NTOK], BF16)
            k.dma("sp", P.dout["d_hstate"], st.hstate[:], reads=[st.b_hstate], is_output=True)
            k.finish()
            return P
        st.n_experts = n_experts
        phase_post(st, out)
        k.finish()
    return P


def host_consts(q):
    sh = 3 - q
    c = {}
    c["ident"] = np.eye(128, dtype=np.float32)
    pos = np.arange(512)
    c["vrow"] = np.broadcast_to((pos >= 128 * sh).astype(np.float32), (128, 512)).copy()
    c["vcol"] = np.broadcast_to((np.arange(NU) >= sh).astype(np.float32), (128, NU)).copy()
    slot = np.arange(256)
    cp = slot - 1
    cvalid = (cp >= 8 * sh)
    cm = np.zeros((128, NJ, 2, 128), np.float32)
    for j in range(NJ):
        u = 4 * j + 3
        tpos = 128 * u + np.arange(128)
        m = ((16 * cp[:, None] + 31) <= tpos[None, :]) & cvalid[:, None]
        cm[:, j, :, :] = m.reshape(2, 128, 128).transpose(1, 0, 2)
    c["cmaskT"] = cm
    c0 = cp * 16
    s0 = np.arange(64) * 64
    ov = np.minimum(c0[:, None] + 32, s0[None, :] + 64) - np.maximum(c0[:, None], s0[None, :])
    ov = np.clip(ov, 0, None) / 32.0
    ov[~cvalid] = 0.0
    c["ovm"] = ov.reshape(2, 128, 64).transpose(1, 0, 2).astype(np.float32).copy()
    j0 = 2 * sh
    jb = np.arange(64)
    scV = np.zeros((128, NJ, 64), np.float32); scN = np.zeros_like(scV); scF = np.zeros_like(scV)
    for j in range(NJ):
        u = 4 * j + 3
        blk = (128 * u + np.arange(128)) // 64
        V = (jb[None, :] >= j0) & (jb[None, :] <= blk[:, None])
        F = ((jb[None, :] == j0) | (jb[None, :] == blk[:, None]) | (jb[None, :] == blk[:, None] - 1)) & V
        scV[:, j] = V; scN[:, j] = V.astype(np.float32) - 1.0; scF[:, j] = np.where(F, 1e4, -2.0)
    c["scV"], c["scN"], c["scF"] = scV, scN, scF
    import ml_dtypes
    ea = (np.arange(128)[:, None] == (np.arange(S)[None, :] // 64)).astype(np.float32)
    c["eall"] = ea.astype(ml_dtypes.bfloat16)
    kk = np.arange(128)[:, None]; tt = np.arange(128)[None, :]
    tri = np.where(kk <= tt, 0.0, -30000.0)
    atri = np.where(kk > tt, 0.0, -30000.0)
    tn = np.stack([np.tile(tri, (1, 4)), np.tile(atri, (1, 4))], axis=1)
    c["trineg"] = tn.astype(ml_dtypes.bfloat16)
    return c


def prep_inputs(inputs):
    f = lambda a: np.ascontiguousarray(np.asarray(a, dtype=np.float32))
    x = f(inputs["x"]); p = f(inputs["p"])[0]
    sh_w = {}
    sh_w["w_in"] = f(inputs["w_in"])[0]
    for nm in ("ln_mix", "ln_ffn", "ln_ple",
               "cmp_k_pos", "cmp_k_w1", "cmp_k_w2", "cmp_v_pos", "cmp_v_w1", "cmp_v_w2",
               "w_nsa_up", "w_lru_up", "w_out", "w_gate", "w_up", "w_down", "w_ple", "w_ple_gate"):
        sh_w[nm] = f(inputs[nm])[0]
    cols = [f(inputs["conv_w"])[0][w] for w in range(4)] + [f(inputs["conv_b"])[0], f(inputs["lru_ba"])[0].reshape(1024),
                                                            f(inputs["lru_bx"])[0].reshape(1024), f(inputs["lru_lambda"])[0]]
    sh_w["lru_small"] = np.ascontiguousarray(np.stack(cols, axis=-1).reshape(8, 128, 8).transpose(1, 0, 2))
    sh_w["ln_final"] = f(inputs["ln_final"])
    sh_w["w_r"] = np.concatenate([f(inputs["w_grp"])[0], f(inputs["w_exp"])[0]], axis=1)
    sh_w["b_r"] = np.concatenate([f(inputs["b_grp"])[0], f(inputs["b_exp"])[0]], axis=0)
    for nm, src in (("wa_bd", "lru_wa"), ("wx_bd", "lru_wx")):
        w = f(inputs[src])[0]
        bd = np.zeros((8, 128, 128), np.float32)
        for n in range(16):
            t, o = divmod(n, 2)
            bd[t, o * 64:(o + 1) * 64, o * 64:(o + 1) * 64] = w[n]
        sh_w[nm] = bd
    in_maps = []
    for c in range(8):
        b, q = divmod(c, 4)
        sh = 3 - q
        xs = np.zeros((S, D), np.float32)
        xs[128 * sh:] = x[b, :S - 128 * sh]
        rows = np.concatenate([np.arange(128 * (4 * j + q), 128 * (4 * j + q + 1)) for j in range(NJ)])
        m = dict(sh_w)
        m["xs"] = xs
        m["pown"] = np.ascontiguousarray(p[b, rows])
        m.update(host_consts(q))
        in_maps.append(m)
    return in_maps


def assemble(results, key="out"):
    out = np.zeros((2, S, D), np.float32)
    for c in range(8):
        b, q = divmod(c, 4)
        o = np.asarray(results[c][key]).reshape(NJ, 128, D)
        for j in range(NJ):
            r = 4 * j + q
            out[b, 128 * r:128 * (r + 1)] = o[j]
    return out


_CACHE = {}


def kernel(**inputs):
    if "P" not in _CACHE:
        _CACHE["P"] = build()
    P = _CACHE["P"]
    in_maps = prep_inputs(inputs)
    in_maps = [{n: m[n] for n in P.din} for m in in_maps]
    res = run_bass_kernel_spmd(P.nc, in_maps, core_ids=list(range(8)))
    return assemble(res.results)


def alloc_mixer(st):
    k = st.k
    st.KT = k.sb("KT", [128, 4, S], BF16); st.b_KT = [k.buf() for _ in range(8)]
    st.Vs = k.sb("Vs", [128, NU, 4, 65], BF16); st.Vw = k.sb("Vw", [128, NU, 4, 65], BF16)
    st.b_V = [k.buf() for _ in range(NU)]
    st.kcv = k.sb("kcv", [128, 4, 256], BF16); st.b_kcv = k.buf()
    st.VC = k.sb("VC", [128, 2, 4, 129], BF16); st.b_VC = k.buf()


def free_mixer(st):
    for n in ("KT", "Vs", "Vw", "kcv", "VC"):
        st.k.sb_free(n)


def phase_kv(st):
    k, nc, P = st.k, st.nc, st.P
    din = P.din
    with contextlib.ExitStack() as es:
        xst = [k.sb("xst%d" % i, [128, D], F32, es) for i in range(2)]; bxst = [k.buf() for _ in range(2)]
        xbs = [(k.sb("xb%d" % i, [128, D], BF16, es), k.buf()) for i in range(4)]
        hT = k.sb("hT0", [128, NKT, 512], BF16, es); bhT = [k.buf() for _ in range(4)]
        gk = k.sb("gk", [128, 2, 128], BF16, es); bgk = k.buf()
        nti = [0]

        def fronts(c):
            for uu in range(4):
                u = 4 * c + uu
                xi = nti[0] % 2
                nti[0] += 1
                k.dma("sp", xst[xi][:, :], din["xs"][u * 128:(u + 1) * 128, :], writes=[bxst[xi]])
                norm_front(st, xst[xi][:, :], [bxst[xi]], xbs[uu], xbs[uu])

        def backs(c):
            for uu in range(4):
                norm_back(st, xbs[uu], st.gains["ln_mix"], hT[:, :, uu * 128:(uu + 1) * 128], bhT[uu],
                          (st.tp_view[uu % 2][:, :], st.b_tp[uu % 2]))

        fronts(0)
        Wkv = k.sb("Wkv", [128, NKT, 4, 128], BF16, es); bWkv = [k.buf() for _ in range(4)]
        Wcmp = k.sb("Wcmp", [128, NKT, 4, 128], BF16, es); bWcmp = [k.buf() for _ in range(4)]
        Wv = k.sb("Wv", [128, NKT, 512], BF16, es); bWv = k.buf()
        for g in range(4):
            for (W, bW, o1, o2) in ((Wkv, bWkv, O_KS, O_KW), (Wcmp, bWcmp, O_KC, O_VC)):
                for half, o in ((0, o1), (1, o2)):
                    k.dma("pool", W[:, :, g, half * 64:(half + 1) * 64],
                          din["w_in"][:, o + g * 64:o + (g + 1) * 64].rearrange("(kt p) d -> p kt d", p=128),
                          writes=[bW[g]], add=(half == 1))
        k.dma("pool", Wv[:, :, 0:256], din["w_in"][:, O_VS:O_VS + 256].rearrange("(kt p) d -> p kt d", p=128), writes=[bWv])
        k.dma("pool", Wv[:, :, 256:512], din["w_in"][:, O_VW:O_VW + 256].rearrange("(kt p) d -> p kt d", p=128),
              writes=[bWv], add=True)
        w1 = k.sb("w1", [128, 32, 128], BF16, es); bw1 = k.buf()
        k.dma("pool", w1[0:64, :, :], din["cmp_k_w1"].rearrange("(l d) h -> d l h", d=64), writes=[bw1])
        k.dma("pool", w1[64:128, :, :], din["cmp_v_w1"].rearrange("(l d) h -> d l h", d=64), writes=[bw1], add=True)
        w2p = k.sb("w2p", [128, 2, 128], BF16, es); bw2 = k.buf()
        k.op("pool", lambda e: e.memset(w2p[:, :, :], 0.0), writes=[bw2])
        k.dma("pool", w2p[:, 0, 0:64], din["cmp_k_w2"], writes=[bw2])
        k.dma("pool", w2p[:, 1, 64:128], din["cmp_v_w2"], writes=[bw2], add=True)
        posT = k.sb("posT", [128, 32], BF16, es); bpos = k.buf()
        k.dma("pool", posT[0:64, :], din["cmp_k_pos"].rearrange("l d -> d l"), writes=[bpos], allow_slow_non_contiguous=True)
        k.dma("pool", posT[64:128, :], din["cmp_v_pos"].rearrange("l d -> d l"), writes=[bpos], add=True,
              allow_slow_non_contiguous=True)
        vcol = k.sb("vcol", [128, NU], F32, es); bvcol = k.buf()
        k.dma("sp", vcol[:, :], din["vcol"], writes=[bvcol])
        ovm = k.sb("ovm", [128, 2, 64], F32, es); bovm = k.buf()
        k.dma("sp", ovm[:, :, :], din["ovm"], writes=[bovm])
        cbias = k.sb("cbias", [128, 2], F32, es); bcb = k.buf()
        KC = k.sb("KCbuf", [128, 4, 528], BF16, es); bKC = k.buf()
        k.op("pool", lambda e: e.memset(KC[:, :, 0:16], 0.0), writes=[bKC])
        for c in range(8):
            backs(c)
            if c > 0:
                k.op("pool", lambda e: e.tensor_copy(out=KC[:, :, 0:16], in_=KC[:, :, 512:528]), reads=[bKC], writes=[bKC])
            pi = 0
            for g in range(4):
                for which in range(2):
                    W, bW = (Wkv, bWkv) if which == 0 else (Wcmp, bWcmp)
                    pj = st.pb[pi % 2]; bpj = st.bpb[pi % 2]; pi += 1
                    mm_group(k, pj[:, :], [(W[:, kk, g, :], hT[:, kk, :]) for kk in range(NKT)],
                             reads=[bW[g]] + bhT, writes=[bpj])
                    if which == 0:
                        k.op("act", lambda e: e.activation(out=st.KT[:, g, c * 512:(c + 1) * 512], in_=pj[:, :], func=AF.Copy),
                             reads=[bpj], writes=[st.b_KT[c]])
                    else:
                        k.op("act", lambda e: e.activation(out=KC[:, g, 16:528], in_=pj[:, :], func=AF.Copy),
                             reads=[bpj], writes=[bKC])
            if c + 1 < 8:
                fronts(c + 1)
            for uu in range(4):
                u = 4 * c + uu
                pj = st.pb[pi % 2]; bpj = st.bpb[pi % 2]; pi += 1
                mm_group(k, pj[:, :], [(hT[:, kk, uu * 128:(uu + 1) * 128], Wv[:, kk, :]) for kk in range(NKT)],
                         reads=[bWv] + bhT, writes=[bpj])
                k.op("dve", lambda e: e.tensor_copy(out=st.Vs[:, u, :, 0:64], in_=pj[:, 0:256].rearrange("p (g d) -> p g d", d=64)),
                     reads=[bpj], writes=[st.b_V[u]])
                k.op("dve", lambda e: e.tensor_copy(out=st.Vw[:, u, :, 0:64], in_=pj[:, 256:512].rearrange("p (g d) -> p g d", d=64)),
                     reads=[bpj], writes=[st.b_V[u]])
                k.op("pool", lambda e: e.tensor_copy(out=st.Vs[:, u, :, 64:65], in_=bc(vcol[:, u:u + 1].unsqueeze(1), [128, 4, 1])),
                     reads=[bvcol], writes=[st.b_V[u]])
                k.op("pool", lambda e: e.tensor_copy(out=st.Vw[:, u, :, 64:65], in_=bc(vcol[:, u:u + 1].unsqueeze(1), [128, 4, 1])),
                     reads=[bvcol], writes=[st.b_V[u]])
            if c == 0:
                for hv in range(2):
                    lo = hv * 64
                    pc = st.pb[2 + hv]; bpc = st.bpb[2 + hv]
                    mm_group(k, pc[:, 0:1], [(w1[lo:lo + 64, l, :], posT[lo:lo + 64, l:l + 1]) for l in range(32)],
                             reads=[bw1, bpos], writes=[bpc])
                    k.op("dve", lambda e: e.tensor_copy(out=cbias[:, hv:hv + 1], in_=pc[:, 0:1]), reads=[bpc], writes=[bcb])
            ph = st.pb[2]; bph = st.bpb[2]
            ph2 = st.pb[3]; bph2 = st.bpb[3]
            for hv, (pp, bpp) in enumerate(((ph, bph), (ph2, bph2))):
                lo = hv * 64
                mm_group(k, pp[:, 0:128].rearrange("p (g b) -> p g b", b=32),
                         [(w1[lo:lo + 64, l, :], KC[lo:lo + 64, :, l:l + 497:16]) for l in range(32)],
                         reads=[bw1, bKC], writes=[bpp])
                k.op("act", lambda e: e.activation(out=gk[:, hv, :], in_=pp[:, 0:128], func=AF.Gelu_apprx_tanh,
                                                    bias=cbias[:, hv:hv + 1]), reads=[bpp, bcb], writes=[bgk])
            mm_group(k, ph[:, 128:256], [(w2p[:, 0, :], gk[:, 0, :]), (w2p[:, 1, :], gk[:, 1, :])],
                     reads=[bw2, bgk], writes=[bph])
            k.op("dve", lambda e: e.tensor_copy(out=st.kcv[:, :, c * 32:(c + 1) * 32],
                                                 in_=ph[:, 128:256].rearrange("p (g b) -> p g b", b=32)),
                 reads=[bph], writes=[st.b_kcv])
        tpb = st.tp_view[0]; btpb = st.b_tp[0]
        for ct in range(2):
            for g in range(4):
                k.op("pe", lambda e: e.transpose(out=tpb[:, (ct * 4 + g) * 64:(ct * 4 + g + 1) * 64],
                                                 in_=st.kcv[64:128, g, ct * 128:(ct + 1) * 128],
                                                 identity=st.identb[64:128, 64:128]),
                     reads=[st.b_kcv, st.b_identb], writes=[btpb], sig=(ct == 1 and g == 3))
        k.op("dve", lambda e: e.tensor_copy(out=st.VC[:, :, :, 0:64],
                                             in_=tpb[:, 0:512].rearrange("p (c g d) -> p c g d", c=2, g=4)),
             reads=[btpb], writes=[st.b_VC])
        k.op("pool", lambda e: e.memset(st.VC[:, :, :, 64:65], 1.0), writes=[st.b_VC])
        for g in range(4):
            k.op("pool", lambda e: e.tensor_copy(out=st.VC[:, :, g, 65:129], in_=ovm[:, :, :]), reads=[bovm], writes=[st.b_VC])
        k.barrier()


def phase_attn(st):
    k, nc, P = st.k, st.nc, st.P
    din = P.din
    st.nsaT = k.sb("nsaT", [128, 8, NTOK], BF16); st.b_nsaT = k.buf()
    with contextlib.ExitStack() as es:
        eall = k.sb("eall", [128, S], BF16, es); beall = k.buf()
        k.dma("sp", eall[:, :], din["eall"], writes=[beall])
        trineg = k.sb("trineg", [128, 2, 512], BF16, es); btri = k.buf()
        k.dma("sp", trineg[:, :, :], din["trineg"], writes=[btri])
        cmask = k.sb("cmask", [128, NJ, 2, 128], BF16, es); bcm = k.buf()
        k.dma("pool", cmask[:, :, :, :], din["cmaskT"], writes=[bcm])
        scc = k.sb("scc", [128, 3, NJ, 64], F32, es); bscc = k.buf()
        for i_, nm in enumerate(("scV", "scN", "scF")):
            k.dma("sp", scc[:, i_, :, :], din[nm], writes=[bscc], add=(i_ > 0))
        hTo = k.sb("hTo", [128, NKT, NTOK], BF16, es); bhTo = [k.buf() for _ in range(NJ)]
        gsb = k.sb("gsb", [128, NJ, 48], F32, es); bgsb = k.buf()
        Wg48 = k.sb("Wg48", [128, NKT, 48], BF16, es); bWg = k.buf()
        k.dma("pool", Wg48[:, :, :], din["w_in"][:, O_G:O_G + 48].rearrange("(kt p) d -> p kt d", p=128), writes=[bWg])
        with contextlib.ExitStack() as es2:
            xst = [k.sb("xst%d" % i, [128, D], F32, es2) for i in range(2)]; bxst = [k.buf() for _ in range(2)]
            xbs = [(k.sb("xb%d" % i, [128, D], BF16, es2), k.buf()) for i in range(4)]
            for j in range(NJ):
                if j % 4 == 0:
                    for j2 in range(j, j + 4):
                        u = 4 * j2 + 3
                        xi = j2 % 2
                        k.dma("sp", xst[xi][:, :], din["xs"][u * 128:(u + 1) * 128, :], writes=[bxst[xi]])
                        norm_front(st, xst[xi][:, :], [bxst[xi]], xbs[j2 % 4], xbs[j2 % 4])
                norm_back(st, xbs[j % 4], st.gains["ln_mix"], hTo[:, :, j * 128:(j + 1) * 128], bhTo[j],
                          (st.tp_view[j % 2][:, :], st.b_tp[j % 2]))
                pg = st.pb[j % 2]; bpg = st.bpb[j % 2]
                mm_group(k, pg[:, 0:48], [(hTo[:, kk, j * 128:(j + 1) * 128], Wg48[:, kk, :]) for kk in range(NKT)],
                         reads=[bhTo[j], bWg], writes=[bpg])
                k.op("act", lambda e: e.activation(out=gsb[:, j, :], in_=pg[:, 0:48], func=AF.Sigmoid),
                     reads=[bpg], writes=[bgsb])
            k.barrier()
        Wq = k.sb("Wq", [128, NKT, 4, 128], BF16, es); bWq = k.buf()
        qTs = k.sb("qTs", [128, 4, NTOK], BF16, es); qTw = k.sb("qTw", [128, 4, NTOK], BF16, es); bqT = k.buf()
        k.op("pool", lambda e: e.memset(qTs[64:128, :, :], 0.0), writes=[bqT])
        k.op("pool", lambda e: e.memset(qTw[0:64, :, :], 0.0), writes=[bqT])
        EcTA = k.sb("EcT", [128, 2, 2, 512], BF16, es); bEcA = [[k.buf(), k.buf()], [k.buf(), k.buf()]]
        NPT = 3
        PT = [k.sb("PT%d" % i, [128, 512], BF16, es) for i in range(NPT)]; bPT = [k.buf() for _ in range(NPT)]
        smA = k.sb("att_sm", [128, 2, 64], F32, es); bsmA = [k.buf(), k.buf()]
        impA = k.sb("imp", [128, 2, 3, 64], F32, es); bimpA = [k.buf(), k.buf()]
        NEGT = k.sb("NEGT", [128, 4, 128], BF16, es); bNEG = k.buf()
        k.op("pool", lambda e: e.memset(NEGT[:, :, :], 0.0), writes=[bNEG])
        stgA = k.sb("stg", [128, 2, 2, 256], F32, es); bstgA = [k.buf(), k.buf()]
        stgbA = k.sb("stgb", [128, 2, 256], BF16, es); bstgbA = [k.buf(), k.buf()]
        tpf = st.tp_view[1][:, :].bitcast(F32)
        btpf = st.b_tp[1]
        tp0f = st.tp_view[0][:, :].bitcast(F32)
        tpn = st.tp_view[0]; btpn = st.b_tp[0]
        Oc = tpf[:, 512:1024]; bOc = k.buf()
        Ic = tp0f[:, 512:1024]; bIc = k.buf()
        pti = 0
        sbank = 0

        def run_branch(units, Obank, bO, vfirst_start):
            nonlocal pti, sbank
            n = len(units)
            slots = []
            first_pv = [True]

            def emit_S(i):
                nonlocal sbank
                sb_ = sbank % 2
                sbank += 1
                Sb = st.pb[sb_]; bS = st.bpb[sb_]
                sc = units[i]["score"]
                for mi, (cols, l, r, rd) in enumerate(sc):
                    o_ = Sb[:, cols[0]:cols[1]]
                    if len(r.shape) == 3:
                        o_ = o_.rearrange("p (h t) -> p h t", h=r.shape[1])
                    mm1(k, o_, l, r, start=(mi == 0), reads=rd, writes=[bS], sig=(mi == len(sc) - 1))
                return Sb, bS

            def emit_exp(i, Sb, bS):
                nonlocal pti
                p_ = pti % NPT
                pti += 1
                k.op("act", lambda e: e.activation(out=PT[p_][:, :], in_=Sb[:, :], func=AF.Exp, scale=0.125),
                     reads=[bS], writes=[bPT[p_]])
                return p_

            def emit_PV(i, p_):
                vr, vb = units[i]["v"]
                for hh in range(4):
                    mm1(k, Obank[:, hh * 65:(hh + 1) * 65], PT[p_][:, hh * 128:(hh + 1) * 128], vr,
                        start=(first_pv[0]), reads=[bPT[p_]] + vb, writes=[bO], sig=(hh == 3))
                    first_pv[0] = False

            pend = []
            for i in range(n):
                Sb, bS = emit_S(i)
                p_ = emit_exp(i, Sb, bS)
                pend.append((i, p_))
                if len(pend) >= 3:
                    ii, pp = pend.pop(0)
                    emit_PV(ii, pp)
            for ii, pp in pend:
                emit_PV(ii, pp)

        for g in range(4):
            first = True
            for hh in range(4):
                h = 4 * g + hh
                for half in range(2):
                    k.dma("pool", Wq[:, :, hh, half * 64:(half + 1) * 64],
                          din["w_in"][:, O_Q + h * 64:O_Q + (h + 1) * 64].rearrange("(kt p) d -> p kt d", p=128),
                          writes=[bWq], add=not first)
                    first = False
            for hh in range(4):
                for half in range(2):
                    pj = st.pb[(hh * 2 + half) % 2]; bpj = st.bpb[(hh * 2 + half) % 2]
                    mm_group(k, pj[:, :], [(Wq[:, kk, hh, :], hTo[:, kk, half * 512:(half + 1) * 512]) for kk in range(NKT)],
                             reads=[bWq] + bhTo, writes=[bpj])
                    k.op("act", lambda e: e.activation(out=qTs[0:64, hh, half * 512:(half + 1) * 512], in_=pj[0:64, :], func=AF.Copy),
                         reads=[bpj], writes=[bqT])
                    k.op("dve", lambda e: e.tensor_copy(out=qTw[64:128, hh, half * 512:(half + 1) * 512], in_=pj[64:128, :]),
                         reads=[bpj], writes=[bqT])
            def ctx(j):
                p = j % 2
                return dict(u=4 * j + 3, tok=slice(j * 128, (j + 1) * 128), sm=smA[:, p, :], bsm=bsmA[p], imp=impA[:, p, :, :], bimp=bimpA[p],
                            stg=stgA[:, p, :, :], bstg=bstgA[p], EcT=EcTA[:, p, :, :], bEc=bEcA[p],
                            gv=gsb[:, j, :].rearrange("p (h b) -> p h b", b=3))

            def cmp_a1(j):
                nonlocal sbank
                c_ = ctx(j); u = c_["u"]; tok = c_["tok"]; sm = c_["sm"]; bsm = c_["bsm"]; imp = c_["imp"]; bimp = c_["bimp"]
                stg = c_["stg"]; bstg = c_["bstg"]; EcT = c_["EcT"]; bEc = c_["bEc"]; gv = c_["gv"]
                Oc3 = Oc[:, 0:260].rearrange("p (h d) -> p h d", d=65)
                for ct in range(2):
                    Sb = st.pb[sbank % 2]; bS = st.bpb[sbank % 2]; sbank += 1
                    mm1(k, Sb[:, :].rearrange("p (h t) -> p h t", h=4), st.kcv[:, g, ct * 128:(ct + 1) * 128], qTs[:, :, tok],
                        start=True, reads=[st.b_kcv, bqT], writes=[bS], sig=True)
                    k.op("act", lambda e: e.activation(out=EcT[:, ct, :], in_=Sb[:, :], func=AF.Exp, scale=0.125),
                         reads=[bS], writes=[bEc[ct]])
                    k.op("dve", lambda e: e.tensor_tensor(out=EcT[:, ct, :].rearrange("p (h t) -> p h t", h=4),
                                                           in0=EcT[:, ct, :].rearrange("p (h t) -> p h t", h=4),
                                                           in1=bc(cmask[:, j, ct, :].unsqueeze(1), [128, 4, 128]), op=ALU.mult),
                         reads=[bEc[ct], bcm], writes=[bEc[ct]])

            def cmp_a2(j):
                c_ = ctx(j); u = c_["u"]; tok = c_["tok"]; sm = c_["sm"]; bsm = c_["bsm"]; imp = c_["imp"]; bimp = c_["bimp"]
                stg = c_["stg"]; bstg = c_["bstg"]; EcT = c_["EcT"]; bEc = c_["bEc"]; gv = c_["gv"]
                Oc3 = Oc[:, 0:260].rearrange("p (h d) -> p h d", d=65)
                fo = True
                for hh in range(4):
                    for ct in range(2):
                        mm1(k, Oc[:, hh * 65:(hh + 1) * 65], EcT[:, ct, hh * 128:(hh + 1) * 128], st.VC[:, ct, g, 0:65],
                            start=fo, reads=[bEc[ct], st.b_VC], writes=[bOc], sig=(hh == 3 and ct == 1))
                        mm1(k, Ic[:, hh * 64:(hh + 1) * 64], EcT[:, ct, hh * 128:(hh + 1) * 128], st.VC[:, ct, g, 65:129],
                            start=fo, reads=[bEc[ct], st.b_VC], writes=[bIc], sig=(hh == 3 and ct == 1))
                        fo = False
                k.op("dve", lambda e: e.tensor_scalar(out=sm[:, 0:4], in0=Oc3[:, :, 64], scalar1=1e-30, scalar2=None, op0=ALU.max),
                     reads=[bOc], writes=[bsm])
                k.op("dve", lambda e: e.reciprocal(out=sm[:, 0:4], in_=sm[:, 0:4]), reads=[bsm], writes=[bsm])
                k.op("dve", lambda e: e.tensor_scalar(out=imp[:, 0, :], in0=Ic[:, 0:64], scalar1=sm[:, 0:1], scalar2=None, op0=ALU.mult),
                     reads=[bIc, bsm], writes=[bimp])
                for hh in range(1, 4):
                    k.op("dve", lambda e: e.scalar_tensor_tensor(out=imp[:, 0, :], in0=Ic[:, hh * 64:(hh + 1) * 64], scalar=sm[:, hh:hh + 1],
                                                                  in1=imp[:, 0, :], op0=ALU.mult, op1=ALU.add),
                         reads=[bIc, bsm, bimp], writes=[bimp])
                k.op("dve", lambda e: e.tensor_tensor(out=imp[:, 0, :], in0=imp[:, 0, :], in1=scc[:, 0, j, :], op=ALU.mult),
                     reads=[bimp, bscc], writes=[bimp])
                k.op("dve", lambda e: e.tensor_tensor(out=imp[:, 0, :], in0=imp[:, 0, :], in1=scc[:, 1, j, :], op=ALU.add),
                     reads=[bimp, bscc], writes=[bimp])
                k.op("dve", lambda e: e.tensor_tensor(out=imp[:, 0, :], in0=imp[:, 0, :], in1=scc[:, 2, j, :], op=ALU.max),
                     reads=[bimp, bscc], writes=[bimp])
                k.op("dve", lambda e: e.max(out=sm[:, 16:24], in_=imp[:, 0, :]), reads=[bimp], writes=[bsm])
                k.op("dve", lambda e: e.match_replace(out=imp[:, 1, :], in_to_replace=sm[:, 16:24], in_values=imp[:, 0, :], imm_value=-1e30),
                     reads=[bimp, bsm], writes=[bimp])
                k.op("dve", lambda e: e.max(out=sm[:, 24:32], in_=imp[:, 1, :]), reads=[bimp], writes=[bsm])
                k.op("dve", lambda e: e.tensor_scalar(out=sm[:, 32:33], in0=sm[:, 31:32], scalar1=0.0, scalar2=None, op0=ALU.max),
                     reads=[bsm], writes=[bsm])
                k.op("dve", lambda e: e.tensor_scalar(out=imp[:, 2, :], in0=imp[:, 0, :], scalar1=sm[:, 32:33], scalar2=30000.0,
                                                       op0=ALU.is_ge, op1=ALU.mult), reads=[bimp, bsm], writes=[bimp])
                k.op("dve", lambda e: e.tensor_scalar(out=imp[:, 2, :], in0=imp[:, 2, :], scalar1=-30000.0, scalar2=None, op0=ALU.add),
                     reads=[bimp], writes=[bimp])
                k.op("dve", lambda e: e.tensor_tensor(out=sm[:, 12:16], in0=sm[:, 0:4], in1=gv[:, 4 * g:4 * g + 4, 0], op=ALU.mult),
                     reads=[bsm, bgsb], writes=[bsm])
                k.op("dve", lambda e: e.tensor_tensor(out=stg[:, 0, :].rearrange("p (h d) -> p h d", d=64), in0=Oc3[:, :, 0:64],
                                                       in1=bc(sm[:, 12:16].unsqueeze(2), [128, 4, 64]), op=ALU.mult),
                     reads=[bOc, bsm], writes=[bstg])

            def cmp_b(j):
                c_ = ctx(j); imp = c_["imp"]; bimp = c_["bimp"]
                k.op("pe", lambda e: e.transpose(out=tpf[0:64, 0:128], in_=imp[:, 2, :], identity=st.identf[:, :]),
                     reads=[bimp, st.b_identf], writes=[btpf])
                k.op("dve", lambda e: e.tensor_copy(out=NEGT[0:64, :, :], in_=bc(tpf[0:64, 0:128].unsqueeze(1), [64, 4, 128])),
                     reads=[btpf], writes=[bNEG])

            def slc_(j):
                c_ = ctx(j); u = c_["u"]; tok = c_["tok"]; sm = c_["sm"]; bsm = c_["bsm"]; stg = c_["stg"]; bstg = c_["bstg"]; gv = c_["gv"]
                NEG2 = NEGT[:, :, :].rearrange("p h t -> p (h t)")
                units = []
                for kt in range(u + 1):
                    ksl = slice(kt * 128, (kt + 1) * 128)
                    sc = [((0, 512), eall[:, ksl], NEG2, [beall, bNEG])]
                    if kt == u:
                        sc.append(((0, 512), st.identb[:, :], trineg[:, 0, :], [st.b_identb, btri]))
                    sc.append(((0, 512), st.KT[:, g, ksl], qTs[:, :, tok], [st.b_KT[kt // 4], bqT]))
                    units.append(dict(score=sc, v=(st.Vs[:, kt, g, :], [st.b_V[kt]])))
                Os = st.pb[2]; bOs = st.bpb[2]
                run_branch(units, Os, bOs, True)
                Os3 = Os[:, 0:260].rearrange("p (h d) -> p h d", d=65)
                k.op("dve", lambda e: e.tensor_scalar(out=sm[:, 4:8], in0=Os3[:, :, 64], scalar1=1e-30, scalar2=None, op0=ALU.max),
                     reads=[bOs], writes=[bsm])
                k.op("dve", lambda e: e.reciprocal(out=sm[:, 4:8], in_=sm[:, 4:8]), reads=[bsm], writes=[bsm])
                k.op("dve", lambda e: e.tensor_tensor(out=sm[:, 12:16], in0=sm[:, 4:8], in1=gv[:, 4 * g:4 * g + 4, 1], op=ALU.mult),
                     reads=[bsm, bgsb], writes=[bsm])
                k.op("dve", lambda e: e.tensor_tensor(out=stg[:, 1, :].rearrange("p (h d) -> p h d", d=64), in0=Os3[:, :, 0:64],
                                                       in1=bc(sm[:, 12:16].unsqueeze(2), [128, 4, 64]), op=ALU.mult),
                     reads=[bOs, bsm], writes=[bstg])
                k.op("dve", lambda e: e.tensor_tensor(out=stg[:, 0, :], in0=stg[:, 0, :], in1=stg[:, 1, :], op=ALU.add),
                     reads=[bstg], writes=[bstg])

            def win_(j):
                c_ = ctx(j); u = c_["u"]; tok = c_["tok"]; sm = c_["sm"]; bsm = c_["bsm"]; stg = c_["stg"]; bstg = c_["bstg"]; gv = c_["gv"]
                units = []
                for kt in range(max(u - 4, 0), u + 1):
                    ksl = slice(kt * 128, (kt + 1) * 128)
                    sc = []
                    if kt == u:
                        sc.append(((0, 512), st.identb[:, :], trineg[:, 0, :], [st.b_identb, btri]))
                    if kt == u - 4:
                        sc.append(((0, 512), st.identb[:, :], trineg[:, 1, :], [st.b_identb, btri]))
                    sc.append(((0, 512), st.KT[:, g, ksl], qTw[:, :, tok], [st.b_KT[kt // 4], bqT]))
                    units.append(dict(score=sc, v=(st.Vw[:, kt, g, :], [st.b_V[kt]])))
                Ow = st.pb[3]; bOw = st.bpb[3]
                run_branch(units, Ow, bOw, True)
                Ow3 = Ow[:, 0:260].rearrange("p (h d) -> p h d", d=65)
                k.op("dve", lambda e: e.tensor_scalar(out=sm[:, 8:12], in0=Ow3[:, :, 64], scalar1=1e-30, scalar2=None, op0=ALU.max),
                     reads=[bOw], writes=[bsm])
                k.op("dve", lambda e: e.reciprocal(out=sm[:, 8:12], in_=sm[:, 8:12]), reads=[bsm], writes=[bsm])
                k.op("dve", lambda e: e.tensor_tensor(out=sm[:, 12:16], in0=sm[:, 8:12], in1=gv[:, 4 * g:4 * g + 4, 2], op=ALU.mult),
                     reads=[bsm, bgsb], writes=[bsm])
                k.op("dve", lambda e: e.tensor_tensor(out=stg[:, 1, :].rearrange("p (h d) -> p h d", d=64), in0=Ow3[:, :, 0:64],
                                                       in1=bc(sm[:, 12:16].unsqueeze(2), [128, 4, 64]), op=ALU.mult),
                     reads=[bOw, bsm], writes=[bstg])
                stgb = stgbA[:, j % 2, :]; bstgb = bstgbA[j % 2]
                k.op("dve", lambda e: e.tensor_tensor(out=stgb[:, :], in0=stg[:, 0, :], in1=stg[:, 1, :], op=ALU.add),
                     reads=[bstg], writes=[bstgb])

            def fin_(j):
                tok = slice(j * 128, (j + 1) * 128)
                stgb = stgbA[:, j % 2, :]; bstgb = bstgbA[j % 2]
                for t2 in range(2):
                    k.op("pe", lambda e: e.transpose(out=tpn[:, t2 * 128:(t2 + 1) * 128], in_=stgb[:, t2 * 128:(t2 + 1) * 128],
                                                     identity=st.identb[:, :]),
                         reads=[bstgb, st.b_identb], writes=[btpn], sig=(t2 == 1))
                k.op("act", lambda e: e.activation(out=st.nsaT[:, 2 * g:2 * g + 2, tok],
                                                    in_=tpn[:, 0:256].rearrange("p (a t) -> p a t", a=2), func=AF.Copy),
                     reads=[btpn], writes=[st.b_nsaT])

            cmp_a1(0)
            cmp_a2(0)
            cmp_b(0)
            for j in range(NJ):
                if j + 1 < NJ:
                    cmp_a1(j + 1)
                slc_(j)
                if j + 1 < NJ:
                    cmp_a2(j + 1)
                win_(j)
                if j + 1 < NJ:
                    cmp_b(j + 1)
                if j >= 1:
                    fin_(j - 1)
            fin_(NJ - 1)
        k.barrier()


def phase_post(st, out_ap):
    k, nc, P = st.k, st.nc, st.P
    din = P.din
    WS = [k.sb("wslot%d" % i, [128, NKT, 512], BF16) for i in range(4)]
    bWS = [k.buf() for _ in range(4)]
    wsi = [0]

    def wslot():
        i = wsi[0] % 4
        wsi[0] += 1
        return WS[i], bWS[i]

    mergedT = k.sb("mergedT", [128, NKT, NTOK], BF16); bmT = [k.buf() for _ in range(NKT)]
    with contextlib.ExitStack() as es:
        hTo = k.sb("hTo", [128, NKT, NTOK], BF16, es); bhTo = [k.buf() for _ in range(NJ)]
        xst = [k.sb("xst%d" % i, [128, D], F32, es) for i in range(2)]; bxst = [k.buf() for _ in range(2)]
        xbs = [(k.sb("xb%d" % i, [128, D], BF16, es), k.buf()) for i in range(2)]
        for j in range(NJ):
            if j % 2 == 0:
                for j2 in range(j, j + 2):
                    u = 4 * j2 + 3
                    xi = j2 % 2
                    k.dma("sp", xst[xi][:, :], din["xs"][u * 128:(u + 1) * 128, :], writes=[bxst[xi]])
                    norm_front(st, xst[xi][:, :], [bxst[xi]], xbs[j2 % 2], xbs[j2 % 2])
            norm_back(st, xbs[j % 2], st.gains["ln_mix"], hTo[:, :, j * 128:(j + 1) * 128], bhTo[j],
                      (st.tp_view[j % 2][:, :], st.b_tp[j % 2]))
        tmp = [k.sb("b1tmp%d" % i, [128, 2, 512], F32, es) for i in range(2)]; btmp = [k.buf() for _ in range(2)]
        ti = 0
        pbi = 0
        for cc in range(2):
            W, bW = wslot()
            k.dma("pool", W[:, :, :], din["w_in"][:, O_LY + cc * 512:O_LY + (cc + 1) * 512].rearrange("(kt p) n -> p kt n", p=128),
                  writes=[bW])
            for ct in range(4):
                for half in range(2):
                    hs = slice(half * 512, (half + 1) * 512)
                    pj = st.pb[pbi % 4]; bpj = st.bpb[pbi % 4]; pbi += 1
                    mm_group(k, pj[:, :], [(W[:, kk, ct * 128:(ct + 1) * 128], hTo[:, kk, hs]) for kk in range(NKT)],
                             reads=[bW] + bhTo, writes=[bpj])
                    T = tmp[ti % 2]; bT = btmp[ti % 2]; ti += 1
                    k.op("act", lambda e: e.activation(out=T[:, 0, :], in_=pj[:, :], func=AF.Gelu_apprx_tanh), reads=[bpj], writes=[bT])
                    k.op("dve", lambda e: e.tensor_tensor(out=st.hstate[:, cc * 4 + ct, hs], in0=st.hstate[:, cc * 4 + ct, hs],
                                                           in1=T[:, 0, :], op=ALU.mult), reads=[bT, st.b_hstate], writes=[st.b_hstate])
        lruT = st.hstate
        tpfB = [st.tp_view[i][:, :].bitcast(F32) for i in range(2)]
        banksets = [([st.pb[i][:, :] for i in range(4)], [st.bpb[i] for i in range(4)]),
                    ([tpfB[0][:, 0:512], tpfB[0][:, 512:1024], tpfB[1][:, 0:512], tpfB[1][:, 512:1024]], [k.buf() for _ in range(4)])]
        mi_ = [0]
        k.barrier()
        for dc in range(4):
            cs = slice(dc * 512, (dc + 1) * 512)
            Wn, bWn = wslot(); Wl, bWl = wslot(); Wa, bWa = wslot(); Wb, bWb = wslot()
            Wn3 = Wn[:, 0:8, :]; Wl3 = Wl[:, 0:8, :]
            k.dma("pool", Wn3, din["w_nsa_up"][:, cs].rearrange("(kt p) n -> p kt n", p=128), writes=[bWn])
            k.dma("pool", Wl3, din["w_lru_up"][:, cs].rearrange("(kt p) n -> p kt n", p=128), writes=[bWl])
            k.dma("pool", Wa[:, :, :], din["w_in"][:, O_MA + dc * 512:O_MA + (dc + 1) * 512].rearrange("(kt p) n -> p kt n", p=128),
                  writes=[bWa])
            k.dma("pool", Wb[:, :, :], din["w_in"][:, O_MB + dc * 512:O_MB + (dc + 1) * 512].rearrange("(kt p) n -> p kt n", p=128),
                  writes=[bWb])
            for dt in range(4):
                ds_ = slice(dt * 128, (dt + 1) * 128)
                for half in range(2):
                    hs = slice(half * 512, (half + 1) * 512)
                    PB, BPB = banksets[mi_[0] % 2]
                    mi_[0] += 1
                    mm_group(k, PB[0], [(Wn3[:, kk, ds_], st.nsaT[:, kk, hs]) for kk in range(8)],
                             reads=[bWn, st.b_nsaT], writes=[BPB[0]])
                    mm_group(k, PB[1], [(Wl3[:, kk, ds_], lruT[:, kk, hs]) for kk in range(8)],
                             reads=[bWl, st.b_hstate], writes=[BPB[1]])
                    mm_group(k, PB[2], [(Wa[:, kk, ds_], hTo[:, kk, hs]) for kk in range(NKT)],
                             reads=[bWa] + bhTo, writes=[BPB[2]])
                    mm_group(k, PB[3], [(Wb[:, kk, ds_], hTo[:, kk, hs]) for kk in range(NKT)],
                             reads=[bWb] + bhTo, writes=[BPB[3]])
                    T = tmp[ti % 2]; bT = btmp[ti % 2]; ti += 1
                    k.op("act", lambda e: e.activation(out=T[:, 0, :], in_=PB[2], func=AF.Sigmoid), reads=[BPB[2]], writes=[bT])
                    k.op("act", lambda e: e.activation(out=T[:, 1, :], in_=PB[3], func=AF.Sigmoid), reads=[BPB[3]], writes=[bT])
                    k.op("dve", lambda e: e.tensor_tensor(out=T[:, 0, :], in0=PB[0], in1=T[:, 0, :], op=ALU.mult),
                         reads=[BPB[0], bT], writes=[bT])
                    k.op("dve", lambda e: e.tensor_tensor(out=T[:, 1, :], in0=PB[1], in1=T[:, 1, :], op=ALU.mult),
                         reads=[BPB[1], bT], writes=[bT])
                    k.op("dve", lambda e: e.tensor_tensor(out=mergedT[:, dc * 4 + dt, hs], in0=T[:, 0, :], in1=T[:, 1, :], op=ALU.add),
                         reads=[bT], writes=[bmT[dc * 4 + dt]])
        k.barrier()
    k.sb_free("hstate"); k.sb_free("nsaT")
    acc = k.sb("acc", [128, NJ, D], F32); bacc = [k.buf() for _ in range(NJ)]
    for j in range(NJ):
        u = 4 * j + 3
        k.dma("sp", acc[:, j, :], din["xs"][u * 128:(u + 1) * 128, :], writes=[bacc[j]])
    pbi = 0
    for dc in range(4):
        cs = slice(dc * 512, (dc + 1) * 512)
        W, bW = wslot()
        k.dma("pool", W[:, :, :], din["w_out"][:, cs].rearrange("(kt p) n -> p kt n", p=128), writes=[bW])
        for j in range(NJ):
            pj = st.pb[pbi % 4]; bpj = st.bpb[pbi % 4]; pbi += 1
            mm_group(k, pj[:, :], [(mergedT[:, kk, j * 128:(j + 1) * 128], W[:, kk, :]) for kk in range(NKT)],
                     reads=[bW] + bmT, writes=[bpj])
            k.op("dve", lambda e: e.tensor_tensor(out=acc[:, j, cs], in0=pj[:, :], in1=acc[:, j, cs], op=ALU.add),
                 reads=[bpj, bacc[j]], writes=[bacc[j]])
    k.barrier()
    k.sb_free("mergedT")
    xnT = k.sb("xnT", [128, NKT, NTOK], BF16); bxnT = [k.buf() for _ in range(NJ)]
    comb = k.sb("comb", [128, NJ, 32], F32); bcomb = k.buf()
    tpf = [st.tp_view[i][:, :].bitcast(F32) for i in range(2)]
    slots = {}

    def load(kind, e):
        W, bW = wslot()
        if kind == "g":
            src = din["w_gate"][e].rearrange("(kt p) n -> p kt n", p=128); dst = W[:, :, :]
        elif kind == "u":
            src = din["w_up"][e].rearrange("(kt p) n -> p kt n", p=128); dst = W[:, :, :]
        else:
            src = din["w_down"][e].rearrange("(ft p) n -> p ft n", p=128)
            dst = W[:, :, :].rearrange("p a b -> p (a b)").rearrange("p (f n) -> p f n", f=4)
        k.dma("pool", dst, src, writes=[bW])
        slots[(kind, e)] = (dst, bW)

    load("g", 0); load("u", 0); load("d", 0)
    with contextlib.ExitStack() as es:
        Wr = k.sb("Wr", [128, NKT, 36], F32, es); bWr = k.buf()
        k.dma("sp", Wr[:, :, :], din["w_r"].rearrange("(kt p) n -> p kt n", p=128), writes=[bWr])
        br = k.sb("br", [128, 36], F32, es); bbr = k.buf()
        k.dma("sp", br[:, :], din["b_r"].partition_broadcast(128), writes=[bbr])
        xs32 = k.sb("xs32", [128, D], F32, es); bxs32 = k.buf()
        xT32 = k.sb("xT32", [128, NKT, 128], F32, es); bxT32 = k.buf()
        junk = (k.sb("junkb", [128, D], BF16, es), k.buf())
        rsA = k.sb("rsm", [128, NJ, 80], F32, es); brs = k.buf()
        rtmp = k.sb("rtmp", [128, NJ, 4, 8], F32, es); brtmp = k.buf()
        gffn, bgffn = st.gains["ln_ffn"]
        for j in range(NJ):
            i = st.nt_i % 4; st.nt_i += 1
            sv = st.stat[:, i, :]; bs = st.bstat[i]
            k.op("act", lambda e: e.activation(out=junk[0][:, :], in_=acc[:, j, :], func=AF.Square, accum_out=sv[:, 0:1]),
                 reads=[bacc[j]], writes=[junk[1], bs])
            k.op("dve", lambda e: e.tensor_scalar(out=sv[:, 1:2], in0=sv[:, 0:1], scalar1=1.0 / D, scalar2=EPS, op0=ALU.mult, op1=ALU.add),
                 reads=[bs], writes=[bs])
            k.op("pool", lambda e: e.tensor_tensor(out=sv[:, 3:4], in0=sv[:, 1:2], in1=st.cneg[:, 0:1], op=ALU.pow), reads=[bs, st.b_cneg], writes=[bs])
            k.op("dve", lambda e: e.tensor_scalar(out=xs32[:, :], in0=acc[:, j, :], scalar1=sv[:, 3:4], scalar2=None, op0=ALU.mult),
                 reads=[bacc[j], bs], writes=[bxs32])
            for hf in range(2):
                tp_ = tpf[hf]; btp_ = st.b_tp[hf]
                for kk in range(8):
                    kq = hf * 8 + kk
                    k.op("pe", lambda e: e.transpose(out=tp_[:, kk * 128:(kk + 1) * 128], in_=xs32[:, kq * 128:(kq + 1) * 128],
                                                     identity=st.identf[:, :]),
                         reads=[bxs32, st.b_identf], writes=[btp_], sig=(kk == 7))
                k.op("dve", lambda e: e.tensor_tensor(out=xT32[:, hf * 8:(hf + 1) * 8, :], in0=tp_[:, :].rearrange("p (k t) -> p k t", t=128),
                                                       in1=bc(gffn[:, hf * 8:(hf + 1) * 8].unsqueeze(2), [128, 8, 128]), op=ALU.mult),
                     reads=[btp_, bgffn], writes=[bxT32])
            k.op("pool", lambda e: e.tensor_copy(out=xnT[:, :, j * 128:(j + 1) * 128], in_=xT32[:, :, :]), reads=[bxT32], writes=[bxnT[j]])
            pr = st.pb[j % 2]; bpr = st.bpb[j % 2]
            mm_group(k, pr[:, 0:36], [(xT32[:, kk, :], Wr[:, kk, :]) for kk in range(NKT)], reads=[bxT32, bWr], writes=[bpr])
            k.op("dve", lambda e: e.tensor_tensor(out=rsA[:, j, 0:36], in0=pr[:, 0:36], in1=br[:, :], op=ALU.add), reads=[bpr, bbr], writes=[brs])
        V = lambda a_, b_: rsA[:, :, a_:b_]
        R = [brs]
        k.op("dve", lambda e: e.reduce_max(out=V(36, 37), in_=V(0, 4), axis=AX.X), reads=R, writes=R)
        k.op("dve", lambda e: e.tensor_tensor(out=V(40, 44), in0=V(0, 4), in1=bc(V(36, 37), [128, NJ, 4]), op=ALU.subtract), reads=R, writes=R)
        k.op("act", lambda e: e.activation(out=V(40, 44), in_=V(40, 44), func=AF.Exp), reads=R, writes=R)
        k.op("dve", lambda e: e.reduce_sum(out=V(38, 39), in_=V(40, 44), axis=AX.X), reads=R, writes=R)
        k.op("dve", lambda e: e.reciprocal(out=V(39, 40), in_=V(38, 39)), reads=R, writes=R)
        k.op("dve", lambda e: e.tensor_tensor(out=V(44, 48), in0=V(0, 4), in1=bc(V(36, 37), [128, NJ, 4]), op=ALU.is_ge), reads=R, writes=R)
        k.op("dve", lambda e: e.tensor_tensor(out=rtmp[:, :, :, :], in0=V(4, 36).rearrange("p j (g x) -> p j g x", g=4),
                                               in1=bc(V(44, 48).unsqueeze(3), [128, NJ, 4, 8]), op=ALU.mult), reads=R, writes=[brtmp])
        k.op("dve", lambda e: e.reduce_sum(out=V(48, 56), in_=rtmp[:, :, :, :].rearrange("p j g x -> p j x g"), axis=AX.X),
             reads=[brtmp], writes=R)
        k.op("dve", lambda e: e.reduce_max(out=V(56, 57), in_=V(48, 56), axis=AX.X), reads=R, writes=R)
        k.op("dve", lambda e: e.tensor_tensor(out=V(48, 56), in0=V(48, 56), in1=bc(V(56, 57), [128, NJ, 8]), op=ALU.subtract), reads=R, writes=R)
        k.op("act", lambda e: e.activation(out=V(48, 56), in_=V(48, 56), func=AF.Exp), reads=R, writes=R)
        k.op("dve", lambda e: e.reduce_max(out=V(59, 60), in_=V(48, 56), axis=AX.X), reads=R, writes=R)
        k.op("dve", lambda e: e.tensor_tensor(out=V(64, 72), in0=V(48, 56), in1=bc(V(59, 60), [128, NJ, 8]), op=ALU.is_ge), reads=R, writes=R)
        k.op("dve", lambda e: e.tensor_scalar(out=V(64, 72), in0=V(64, 72), scalar1=-2.0, scalar2=None, op0=ALU.mult), reads=R, writes=R)
        k.op("dve", lambda e: e.tensor_tensor(out=V(64, 72), in0=V(64, 72), in1=V(48, 56), op=ALU.add), reads=R, writes=R)
        k.op("dve", lambda e: e.reduce_max(out=V(57, 58), in_=V(64, 72), axis=AX.X), reads=R, writes=R)
        k.op("dve", lambda e: e.tensor_tensor(out=V(58, 59), in0=V(57, 58), in1=V(59, 60), op=ALU.add), reads=R, writes=R)
        k.op("dve", lambda e: e.reciprocal(out=V(58, 59), in_=V(58, 59)), reads=R, writes=R)
        k.op("dve", lambda e: e.tensor_tensor(out=V(58, 59), in0=V(58, 59), in1=V(39, 40), op=ALU.mult), reads=R, writes=R)
        k.op("dve", lambda e: e.tensor_tensor(out=V(72, 80), in0=V(48, 56), in1=bc(V(57, 58), [128, NJ, 8]), op=ALU.is_ge), reads=R, writes=R)
        k.op("dve", lambda e: e.tensor_tensor(out=V(72, 80), in0=V(72, 80), in1=V(48, 56), op=ALU.mult), reads=R, writes=R)
        k.op("dve", lambda e: e.tensor_tensor(out=V(72, 80), in0=V(72, 80), in1=bc(V(58, 59), [128, NJ, 8]), op=ALU.mult), reads=R, writes=R)
        k.op("dve", lambda e: e.tensor_tensor(out=comb[:, :, :].rearrange("p j (g x) -> p j g x", g=4),
                                               in0=bc(V(44, 48).unsqueeze(3), [128, NJ, 4, 8]),
                                               in1=bc(V(72, 80).unsqueeze(2), [128, NJ, 4, 8]), op=ALU.mult),
             reads=R, writes=[bcomb])
        k.barrier()
    with contextlib.ExitStack() as es:
        hidT = k.sb("hidT", [128, 4, NTOK], BF16, es); bhid = [k.buf() for _ in range(4)]
        sgt = [k.sb("sgt%d" % i, [128, 512], F32, es) for i in range(2)]; bsgt = [k.buf() for _ in range(2)]
        ob = [tpf[0][:, 0:512], tpf[0][:, 512:1024], tpf[1][:, 0:512], tpf[1][:, 512:1024]]
        bob = [k.buf() for _ in range(4)]
        NE = st.n_experts
        gi = 0
        oi = 0
        for e in range(NE):
            if e + 1 < NE:
                load("g", e + 1)
            Wg, bWg = slots.pop(("g", e)); Wu, bWu = slots.pop(("u", e))
            for f in range(4):
                fs = slice(f * 128, (f + 1) * 128)
                for half in range(2):
                    hs = slice(half * 512, (half + 1) * 512)
                    pg = st.pb[(gi % 2) * 2]; bpg = st.bpb[(gi % 2) * 2]
                    pu = st.pb[(gi % 2) * 2 + 1]; bpu = st.bpb[(gi % 2) * 2 + 1]
                    S_ = sgt[gi % 2]; bS_ = bsgt[gi % 2]
                    gi += 1
                    mm_group(k, pg[:, :], [(Wg[:, kk, fs], xnT[:, kk, hs]) for kk in range(NKT)], reads=[bWg] + bxnT, writes=[bpg])
                    mm_group(k, pu[:, :], [(Wu[:, kk, fs], xnT[:, kk, hs]) for kk in range(NKT)], reads=[bWu] + bxnT, writes=[bpu])
                    k.op("act", lambda e_: e_.activation(out=S_[:, :], in_=pg[:, :], func=AF.Silu), reads=[bpg], writes=[bS_])
                    k.op("dve", lambda e_: e_.tensor_tensor(out=hidT[:, f, hs], in0=pu[:, :], in1=S_[:, :], op=ALU.mult),
                         reads=[bpu, bS_], writes=[bhid[f]])
            if e + 1 < NE:
                load("u", e + 1); load("d", e + 1)
            Wd, bWd = slots.pop(("d", e))
            for j in range(NJ):
                for dc in range(4):
                    cs = slice(dc * 512, (dc + 1) * 512)
                    O_ = ob[oi % 4]; bO_ = bob[oi % 4]; oi += 1
                    mm_group(k, O_, [(hidT[:, f, j * 128:(j + 1) * 128], Wd[:, f, cs]) for f in range(4)],
                             reads=[bWd] + bhid, writes=[bO_])
                    k.op("dve", lambda e_: e_.scalar_tensor_tensor(out=acc[:, j, cs], in0=O_, scalar=comb[:, j, e:e + 1], in1=acc[:, j, cs],
                                                                   op0=ALU.mult, op1=ALU.add),
                         reads=[bO_, bcomb, bacc[j]], writes=[bacc[j]])
        k.barrier()
    with contextlib.ExitStack() as es:
        xb = (k.sb("xb0", [128, D], BF16, es), k.buf())
        for j in range(NJ):
            norm_transpose(st, acc[:, j, :], [bacc[j]], st.gains["ln_ple"], xnT[:, :, j * 128:(j + 1) * 128], bxnT[j],
                           None, xb, (st.tp_view[j % 2][:, :], st.b_tp[j % 2]), xb)
        pT = k.sb("pT", [128, 2, NTOK], BF16, es); bpT = k.buf()
        pst = k.sb("pst", [128, 256], F32, es); bpst = k.buf()
        pstb = k.sb("pstb", [128, 256], BF16, es); bpstb = k.buf()
        for j in range(NJ):
            k.dma("sp", pst[:, :], din["pown"][j * 128:(j + 1) * 128, :], writes=[bpst])
            k.op("dve", lambda e: e.tensor_copy(out=pstb[:, :], in_=pst[:, :]), reads=[bpst], writes=[bpstb])
            tp_ = st.tp_view[j % 2]; btp_ = st.b_tp[j % 2]
            for t2 in range(2):
                k.op("pe", lambda e: e.transpose(out=tp_[:, t2 * 128:(t2 + 1) * 128], in_=pstb[:, t2 * 128:(t2 + 1) * 128], identity=st.identb[:, :]),
                     reads=[bpstb, st.b_identb], writes=[btp_], sig=(t2 == 1))
            k.op("act", lambda e: e.activation(out=pT[:, :, j * 128:(j + 1) * 128], in_=tp_[:, 0:256].rearrange("p (a t) -> p a t", a=2),
                                                func=AF.Copy), reads=[btp_], writes=[bpT])
        Wp = k.sb("Wp", [128, 2, D], BF16, es); bWp = k.buf()
        k.dma("pool", Wp[:, :, :], din["w_ple"].rearrange("(kt p) n -> p kt n", p=128), writes=[bWp])
        sg2 = [k.sb("sg2_%d" % i, [128, 512], F32, es) for i in range(2)]; bsg2 = [k.buf() for _ in range(2)]
        gi = 0
        for dc in range(4):
            cs = slice(dc * 512, (dc + 1) * 512)
            W, bW = wslot()
            k.dma("pool", W[:, :, :], din["w_ple_gate"][:, cs].rearrange("(kt p) n -> p kt n", p=128), writes=[bW])
            for j in range(NJ):
                ts_ = slice(j * 128, (j + 1) * 128)
                pg = st.pb[(gi % 2) * 2]; bpg = st.bpb[(gi % 2) * 2]
                pp = st.pb[(gi % 2) * 2 + 1]; bpp = st.bpb[(gi % 2) * 2 + 1]
                S_ = sg2[gi % 2]; bS_ = bsg2[gi % 2]
                gi += 1
                mm_group(k, pg[:, :], [(xnT[:, kk, ts_], W[:, kk, :]) for kk in range(NKT)], reads=[bW, bxnT[j]], writes=[bpg])
                mm_group(k, pp[:, :], [(pT[:, kk, ts_], Wp[:, kk, cs]) for kk in range(2)], reads=[bWp, bpT], writes=[bpp])
                k.op("act", lambda e: e.activation(out=S_[:, :], in_=pg[:, :], func=AF.Sigmoid), reads=[bpg], writes=[bS_])
                k.op("dve", lambda e: e.tensor_tensor(out=S_[:, :], in0=pp[:, :], in1=S_[:, :], op=ALU.mult), reads=[bpp, bS_], writes=[bS_])
                k.op("dve", lambda e: e.tensor_tensor(out=acc[:, j, cs], in0=acc[:, j, cs], in1=S_[:, :], op=ALU.add),
                     reads=[bS_, bacc[j]], writes=[bacc[j]])
        k.barrier()
    k.sb_free("xnT")
    with contextlib.ExitStack() as es:
        gfin = k.sb("gfin", [128, D], F32, es); bgfin = k.buf()
        k.dma("sp", gfin[:, :], din["ln_final"].partition_broadcast(128), writes=[bgfin])
        ost = [k.sb("ost%d" % i, [128, D], F32, es) for i in range(2)]; bost = [k.buf() for _ in range(2)]
        junk = (k.sb("junkc", [128, D], BF16, es), k.buf())
        for j in range(NJ):
            i = st.nt_i % 4; st.nt_i += 1
            sv = st.stat[:, i, :]; bs = st.bstat[i]
            k.op("act", lambda e: e.activation(out=junk[0][:, :], in_=acc[:, j, :], func=AF.Square, accum_out=sv[:, 0:1]),
                 reads=[bacc[j]], writes=[junk[1], bs])
            k.op("dve", lambda e: e.tensor_scalar(out=sv[:, 1:2], in0=sv[:, 0:1], scalar1=1.0 / D, scalar2=EPS, op0=ALU.mult, op1=ALU.add),
                 reads=[bs], writes=[bs])
            k.op("pool", lambda e: e.tensor_tensor(out=sv[:, 3:4], in0=sv[:, 1:2], in1=st.cneg[:, 0:1], op=ALU.pow), reads=[bs, st.b_cneg], writes=[bs])
            O_ = ost[j % 2]; bO_ = bost[j % 2]
            k.op("dve", lambda e: e.scalar_tensor_tensor(out=O_[:, :], in0=acc[:, j, :], scalar=sv[:, 3:4], in1=gfin[:, :],
                                                          op0=ALU.mult, op1=ALU.mult), reads=[bacc[j], bs, bgfin], writes=[bO_])
            k.dma("sp", out_ap[j * 128:(j + 1) * 128, :], O_[:, :], reads=[bO_], is_output=True)
    for i in range(4):
        k.sb_free("wslot%d" % i)
    k.sb_free("acc"); k.sb_free("comb")
```

```python
import contextlib
import numpy as np
import concourse.bass as bass
import concourse.mybir as mybir
from concourse.bass_utils import run_bass_kernel_spmd

F32 = mybir.dt.float32
BF16 = mybir.dt.bfloat16
I32 = mybir.dt.int32
AF = mybir.ActivationFunctionType
ALU = mybir.AluOpType
AX = mybir.AxisListType

SAME_ENGINE_SYNC = True
N_DMA_SEMS = 48


class Buf:
    __slots__ = ("name", "w", "r")

    def __init__(self, name=""):
        self.name = name
        self.w = []
        self.r = {}


class Eng:
    def __init__(self, name, h, sem):
        self.name = name
        self.h = h
        self.sem = sem
        self.count = 0
        self.seen = {}


class K:
    def __init__(self, nc, es):
        self.nc = nc
        self.es = es
        self.E = {}
        for name, h in (("pe", nc.tensor), ("act", nc.scalar), ("dve", nc.vector),
                        ("pool", nc.gpsimd), ("sp", nc.sync)):
            sem = es.enter_context(nc.semaphore("sem_" + name))
            self.E[name] = Eng(name, h, sem)
        self.dsem = {c: [es.enter_context(nc.semaphore("dsem_%s%d" % (c, i))) for i in range(N_DMA_SEMS // 2)] for c in ("sw", "hw")}
        self.dma_i = {"sw": 0, "hw": 0}
        self.dma_tix = {"sw": [], "hw": []}
        self.dma_n = 0
        self.out_tix = []
        self.nbuf = 0
        self._bscr = self.sb("bar_scr", [128, 8], F32)
        self._bb = {n: self.buf("bar_" + n) for n in ("pe", "act", "dve", "pool")}
        self.op("dve", lambda e: e.memset(self._bscr[:, :], 0.0), writes=list(self._bb.values()))

    ARENA = 204 * 1024

    def _arena_init(self):
        self.arena = self.es.enter_context(self.nc.sbuf_tensor("arena", [128, self.ARENA // 2], BF16))
        self.free_list = [(0, self.ARENA)]
        self.live = {}

    def sb(self, name, shape, dt, es=None):
        if not hasattr(self, "arena"):
            self._arena_init()
        esz = {F32: 4, BF16: 2, I32: 4}[dt]
        n = 1
        for d in shape[1:]:
            n *= d
        nbytes = (n * esz + 63) // 64 * 64
        for idx, (off, sz) in enumerate(self.free_list):
            if sz >= nbytes:
                break
        else:
            raise RuntimeError("arena full allocating %s (%d B); live=%s" % (name, nbytes, sorted((v[1], k_) for k_, v in self.live.items())))
        if sz == nbytes:
            self.free_list.pop(idx)
        else:
            self.free_list[idx] = (off + nbytes, sz - nbytes)
        assert name not in self.live, name
        self.live[name] = (off, nbytes)
        v = self.arena[0:shape[0], off // 2:(off + n * esz) // 2]
        if dt != BF16:
            v = v.bitcast(dt)
        if len(shape) > 2:
            names = " ".join("d%d" % i for i in range(1, len(shape)))
            kw = {"d%d" % i: shape[i] for i in range(2, len(shape))}
            v = v.rearrange("p (%s) -> p %s" % (names, names), **kw)
        if es is not None:
            es.callback(self.sb_free, name)
        return v

    def sb_free(self, name):
        off, nbytes = self.live.pop(name)
        fl = self.free_list + [(off, nbytes)]
        fl.sort()
        merged = []
        for o, z in fl:
            if merged and merged[-1][0] + merged[-1][1] == o:
                merged[-1] = (merged[-1][0], merged[-1][1] + z)
            else:
                merged.append((o, z))
        self.free_list = merged

    def ps(self, name, shape, dt, es=None):
        return (es or self.es).enter_context(self.nc.psum_tensor("ps_" + name, list(shape), dt))

    def buf(self, name=""):
        self.nbuf += 1
        return Buf(name)

    def _need(self, E, t):
        sem, val, ename = t
        if ename == E.name:
            if E.name == "pe" or not SAME_ENGINE_SYNC:
                return
        if ename is not None:
            assert self.E[ename].count >= val, "waiting on unsignaled ticket of %s" % ename
        key = id(sem)
        if E.seen.get(key, 0) >= val:
            return
        E.h.wait_ge(sem, val)
        E.seen[key] = val

    def _deps(self, E, reads, writes):
        for b in reads:
            for t in b.w:
                self._need(E, t)
        for b in writes:
            for t in b.w:
                self._need(E, t)
            for t in b.r.values():
                self._need(E, t)

    def _record(self, t, reads, writes):
        key = id(t[0])
        for b in reads:
            old = b.r.get(key)
            if old is None or old[1] < t[1]:
                b.r[key] = t
        for b in writes:
            b.w = [t]
            b.r = {}

    def op(self, eng, fn, reads=(), writes=(), sig=True):
        E = self.E[eng]
        self._deps(E, reads, writes)
        ins = fn(E.h)
        if sig:
            E.count += 1
            ins.then_inc(E.sem, 1)
            t = (E.sem, E.count, E.name)
        else:
            t = (E.sem, E.count + 1, E.name)
        self._record(t, reads, writes)
        return t

    def dma(self, queue, out, in_, reads=(), writes=(), is_output=False, add=False, **kw):
        E = self.E[queue]
        if add:
            self._deps(E, reads, ())
        else:
            self._deps(E, reads, writes)
        cls = "sw" if queue == "pool" else "hw"
        NS = N_DMA_SEMS // 2
        i = self.dma_i[cls]
        self.dma_i[cls] += 1
        self.dma_n += 1
        sem = self.dsem[cls][i % NS]
        val = 16 * (i // NS + 1)
        if i >= NS:
            self._need(E, self.dma_tix[cls][i - NS])
        E.h.dma_start(out=out, in_=in_, **kw).then_inc(sem, 16)
        t = (sem, val, None)
        self.dma_tix[cls].append(t)
        for b in reads:
            b.r[("d", self.dma_n)] = t
        for b in writes:
            if add:
                b.w = b.w + [t]
            else:
                b.w = [t]
                b.r = {}
        if is_output:
            self.out_tix.append(t)
        return t

    def barrier(self):
        names = ["pe", "act", "dve", "pool"]
        sc = self._bscr
        self.op("act", lambda e: e.activation(out=sc[0:32, 0:1], in_=sc[0:32, 1:2], func=AF.Copy), writes=[self._bb["act"]])
        self.op("dve", lambda e: e.memset(sc[0:32, 2:3], 0.0), writes=[self._bb["dve"]])
        self.op("pool", lambda e: e.memset(sc[0:32, 3:4], 0.0), writes=[self._bb["pool"]])
        for n in names + ["sp"]:
            E = self.E[n]
            for m in names:
                if m != n:
                    F = self.E[m]
                    self._need(E, (F.sem, F.count, F.name))
            for c in ("sw", "hw"):
                for t in self.dma_tix[c][-(N_DMA_SEMS // 2):]:
                    self._need(E, t)

    def finish(self):
        E = self.E["sp"]
        for t in self.out_tix:
            self._need(E, t)


D = 2048
NKT = 16
S = 4096
NU = 32
NJ = 8
NTOK = 1024
HD = 64
IN_SPLITS = (1024, 256, 256, 256, 256, 256, 256, 48, 1024, 1024, 2048, 2048)
OFF = [0]
for _v in IN_SPLITS:
    OFF.append(OFF[-1] + _v)
(O_Q, O_KC, O_VC, O_KS, O_VS, O_KW, O_VW, O_G, O_LX, O_LY, O_MA, O_MB, O_END) = OFF
EPS = 1e-6


def bc(ap, shape):
    return ap.to_broadcast(list(shape))


class Prog:
    def __init__(self, dbg=None):
        self.dbg = dbg or {}
        nc = bass.Bass("TRN2", target_bir_lowering=False)
        self.nc = nc
        self.din = LazyIn(self)
        self.dout = {}

    def inp(self, name, shape, dt=F32):
        t = self.nc.dram_tensor(name, list(shape), dt, kind="ExternalInput").ap()
        self.din[name] = t
        return t

    def outp(self, name, shape, dt=F32):
        t = self.nc.dram_tensor(name, list(shape), dt, kind="ExternalOutput").ap()
        self.dout[name] = t
        return t


class St:
    pass


def mm_group(k, out_ap, pairs, reads, writes, sig_last=True):
    n = len(pairs)
    for i, (l, r) in enumerate(pairs):
        k.op("pe", lambda e: e.matmul(out_ap, l, r, start=(i == 0), stop=(i == n - 1)),
             reads=reads, writes=writes, sig=(sig_last and i == n - 1))


def mm1(k, out_ap, l, r, start, reads, writes, sig=False):
    k.op("pe", lambda e: e.matmul(out_ap, l, r, start=start, stop=True, skip_group_check=True),
         reads=reads, writes=writes, sig=sig)


IN_SHAPES = {
    "xs": ([S, D], F32), "pown": ([NTOK, 256], F32), "w_in": ([D, O_END], F32),
    "ln_mix": ([D], F32), "ln_ffn": ([D], F32), "ln_ple": ([D], F32), "ln_final": ([D], F32),
    "wa_bd": ([8, 128, 128], F32), "wx_bd": ([8, 128, 128], F32), "lru_small": ([128, 8, 8], F32),
    "cmp_k_pos": ([32, 64], F32), "cmp_k_w1": ([2048, 128], F32), "cmp_k_w2": ([128, 64], F32),
    "cmp_v_pos": ([32, 64], F32), "cmp_v_w1": ([2048, 128], F32), "cmp_v_w2": ([128, 64], F32),
    "w_nsa_up": ([1024, D], F32), "w_lru_up": ([1024, D], F32), "w_out": ([D, D], F32),
    "w_r": ([D, 36], F32), "b_r": ([36], F32),
    "w_gate": ([32, D, 512], F32), "w_up": ([32, D, 512], F32), "w_down": ([32, 512, D], F32),
    "w_ple": ([256, D], F32), "w_ple_gate": ([D, D], F32),
    "ident": ([128, 128], F32),
    "vrow": ([128, 512], F32),
    "vcol": ([128, NU], F32),
    "cmaskT": ([128, NJ, 2, 128], F32),
    "ovm": ([128, 2, 64], F32),
    "scV": ([128, NJ, 64], F32), "scN": ([128, NJ, 64], F32), "scF": ([128, NJ, 64], F32),
    "eall": ([128, S], BF16),
    "trineg": ([128, 2, 512], BF16),
}


class LazyIn(dict):
    def __init__(self, P):
        super().__init__()
        self.P = P

    def __missing__(self, name):
        shape, dt = IN_SHAPES[name]
        t = self.P.nc.dram_tensor(name, list(shape), dt, kind="ExternalInput").ap()
        self[name] = t
        return t


def setup(st):
    k, nc, P = st.k, st.nc, st.P
    din = P.din
    st.identf = k.sb("identf", [128, 128], F32); st.b_identf = k.buf()
    st.identb = k.sb("identb", [128, 128], BF16); st.b_identb = k.buf()
    k.dma("sp", st.identf[:], din["ident"], writes=[st.b_identf])
    k.dma("pool", st.identb[:], din["ident"], writes=[st.b_identb])
    st.gains = {}
    for nm in ("ln_mix", "ln_ffn", "ln_ple"):
        g = k.sb("g_" + nm, [128, NKT], F32)
        b = k.buf()
        k.dma("sp", g[:], din[nm].rearrange("(kt p) -> p kt", p=128), writes=[b],
              allow_slow_non_contiguous=True)
        st.gains[nm] = (g, b)
    st.tp_view = [k.ps("tp%d" % i, [128, 2048], BF16) for i in range(2)]
    st.b_tp = [k.buf("tp%d" % i) for i in range(2)]
    st.pb = [k.ps("pb%d" % i, [128, 512], F32) for i in range(4)]
    st.bpb = [k.buf("pb%d" % i) for i in range(4)]
    st.stat = k.sb("stat", [128, 4, 4], F32)
    st.bstat = [k.buf() for _ in range(4)]
    st.nt_i = 0
    st.cneg = k.sb("cneg", [128, 2], F32); st.b_cneg = k.buf()
    k.op("pool", lambda e: e.memset(st.cneg[:, 0:1], -0.5), writes=[st.b_cneg])
    k.op("pool", lambda e: e.memset(st.cneg[:, 1:2], 0.5), writes=[st.b_cneg])


def norm_front(st, src_ap, src_bufs, xb, junk, mul_eng="dve"):
    k = st.k
    i = st.nt_i % 4
    st.nt_i += 1
    sv = st.stat[:, i, :]
    bs = st.bstat[i]
    jt, bj = junk
    xbt, bxb = xb
    k.op("act", lambda e: e.activation(out=jt[:], in_=src_ap, func=AF.Square, accum_out=sv[:, 0:1]),
         reads=src_bufs, writes=[bj, bs])
    k.op("dve", lambda e: e.tensor_scalar(out=sv[:, 1:2], in0=sv[:, 0:1], scalar1=1.0 / D, scalar2=EPS,
                                           op0=ALU.mult, op1=ALU.add), reads=[bs], writes=[bs])
    k.op("pool", lambda e: e.tensor_tensor(out=sv[:, 3:4], in0=sv[:, 1:2], in1=st.cneg[:, 0:1], op=ALU.pow), reads=[bs, st.b_cneg], writes=[bs])
    k.op(mul_eng, lambda e: e.tensor_scalar(out=xbt[:], in0=src_ap, scalar1=sv[:, 3:4], scalar2=None, op0=ALU.mult),
         reads=list(src_bufs) + [bs], writes=[bxb])


def norm_back(st, xb, gain, dst_ap, dst_buf, tp_banks):
    k = st.k
    g, bg = gain
    xbt, bxb = xb
    tp, btp = tp_banks
    for kk in range(NKT):
        k.op("pe", lambda e: e.transpose(out=tp[:, kk * 128:(kk + 1) * 128], in_=xbt[:, kk * 128:(kk + 1) * 128],
                                         identity=st.identb[:]),
             reads=[bxb, st.b_identb], writes=[btp], sig=(kk == NKT - 1))
    k.op("dve", lambda e: e.tensor_tensor(out=dst_ap, in0=tp[:, :].rearrange("p (k t) -> p k t", t=128),
                                           in1=bc(g[:, :].unsqueeze(2), [128, NKT, 128]), op=ALU.mult),
         reads=[btp, bg], writes=[dst_buf])


def norm_transpose(st, src_ap, src_bufs, gain, dst_ap, dst_buf, xstage, xb, tp_banks, junk):
    norm_front(st, src_ap, src_bufs, xb, junk)
    norm_back(st, xb, gain, dst_ap, dst_buf, tp_banks)


def phase_lru(st):
    k, nc, P = st.k, st.nc, st.P
    din = P.din
    with contextlib.ExitStack() as es:
        Wlx = k.sb("Wlx", [128, NKT, 1024], BF16, es); bWlx = k.buf()
        for q4 in range(4):
            k.dma("pool", Wlx[:, q4 * 4:(q4 + 1) * 4, :],
                  din["w_in"][q4 * 512:(q4 + 1) * 512, O_LX:O_LX + 1024].rearrange("(kt p) n -> p kt n", p=128),
                  writes=[bWlx], add=(q4 > 0))
        WaBD = k.sb("WaBD", [128, 8, 128], F32, es); bWa = k.buf()
        WxBD = k.sb("WxBD", [128, 8, 128], F32, es); bWx = k.buf()
        k.dma("sp", WaBD[:], din["wa_bd"].rearrange("c p n -> p c n"), writes=[bWa])
        k.dma("sp", WxBD[:], din["wx_bd"].rearrange("c p n -> p c n"), writes=[bWx])
        sm = k.sb("lru_sm", [128, 8, 16], F32, es); bsm = k.buf()
        k.dma("sp", sm[:, :, 0:8], din["lru_small"], writes=[bsm])
        k.op("act", lambda e: e.activation(out=sm[:, :, 9:10], in_=sm[:, :, 7:8], func=AF.Exp, scale=-1.0),
             reads=[bsm], writes=[bsm])
        k.op("act", lambda e: e.activation(out=sm[:, :, 10:11], in_=sm[:, :, 9:10], func=AF.Ln, bias=1.0),
             reads=[bsm], writes=[bsm])
        k.op("dve", lambda e: e.tensor_scalar(out=sm[:, :, 8:9], in0=sm[:, :, 10:11], scalar1=-8.0, scalar2=None,
                                               op0=ALU.mult), reads=[bsm], writes=[bsm])
        k.op("dve", lambda e: e.tensor_scalar(out=sm[:, :, 11:13], in0=sm[:, :, 5:7], scalar1=0.5, scalar2=None, op0=ALU.mult),
             reads=[bsm], writes=[bsm])
        k.op("dve", lambda e: e.tensor_scalar(out=sm[:, :, 13:14], in0=sm[:, :, 10:11], scalar1=-4.0, scalar2=None, op0=ALU.mult),
             reads=[bsm], writes=[bsm])
        k.op("dve", lambda e: e.tensor_scalar(out=sm[:, :, 14:15], in0=sm[:, :, 10:11], scalar1=-8.0, scalar2=None, op0=ALU.mult),
             reads=[bsm], writes=[bsm])
        vrow = k.sb("vrow", [128, 512], F32, es); bvrow = k.buf()
        k.dma("sp", vrow[:], din["vrow"], writes=[bvrow])
        xbuf = k.sb("xbuf", [128, 8, 515], F32, es); bxbuf = [k.buf() for _ in range(8)]
        k.op("pool", lambda e: e.memset(xbuf[:, :, 0:3], 0.0), writes=bxbuf)
        carry = k.sb("carry", [128, 8], F32, es); bcarry = [k.buf() for _ in range(8)]
        k.op("pool", lambda e: e.memset(carry[:], 0.0), writes=bcarry)
        xst = [k.sb("xst%d" % i, [128, D], F32, es) for i in range(2)]; bxst = [k.buf() for _ in range(2)]
        xb = [(k.sb("xb%d" % i, [128, D], BF16, es), k.buf()) for i in range(1)]
        junk = (k.sb("junk", [128, D], BF16, es), k.buf())
        hT = [k.sb("hT%d" % i, [128, NKT, 512], BF16, es) for i in range(2)]
        bhT = [[k.buf() for _ in range(4)] for _ in range(2)]
        NT = 4
        tmp = [k.sb("ltmp%d" % i, [128, 5, 512], F32, es) for i in range(NT)]
        tmpb = [k.sb("ltmpb%d" % i, [128, 512], BF16, es) for i in range(NT)]
        btmp = [[k.buf() for _ in range(9)] for _ in range(NT)]
        nti = [0]

        def norms(c):
            hb = c % 2
            for uu in range(4):
                u = 4 * c + uu
                xi = nti[0] % 2
                k.dma("sp", xst[xi][:], din["xs"][u * 128:(u + 1) * 128, :], writes=[bxst[xi]])
                tpi = nti[0] % 2
                norm_front(st, xst[xi][:], [bxst[xi]], xb[0], junk)
                norm_back(st, xb[0], st.gains["ln_mix"], hT[hb][:, :, uu * 128:(uu + 1) * 128], bhT[hb][uu],
                          (st.tp_view[tpi][:, :], st.b_tp[tpi]))
                nti[0] += 1

        def stageA(c, ct):
            hb = c % 2
            s = (c * 8 + ct) % NT
            T = tmp[s]; B = btmp[s]
            pj = st.pb[ct % 2]; bpj = st.bpb[ct % 2]
            mm_group(k, pj[:, :], [(Wlx[:, kk, ct * 128:(ct + 1) * 128], hT[hb][:, kk, :]) for kk in range(NKT)],
                     reads=[bWlx] + bhT[hb], writes=[bpj])
            bx_ = bxbuf[ct]
            k.op("act", lambda e: e.activation(out=xbuf[:, ct, 3:515], in_=pj[:, :], func=AF.Copy),
                 reads=[bpj], writes=[bx_])
            xc = T[:, 0, :]
            k.op("act", lambda e: e.activation(out=xc, in_=pj[:, :], func=AF.Identity, scale=sm[:, ct, 3:4], bias=sm[:, ct, 4:5]),
                 reads=[bpj, bsm], writes=[B[0]])
            for w in (2, 1, 0):
                k.op("dve", lambda e: e.scalar_tensor_tensor(out=xc, in0=xbuf[:, ct, w:w + 512], scalar=sm[:, ct, w:w + 1],
                                                              in1=xc, op0=ALU.mult, op1=ALU.add),
                     reads=[bx_, bsm, B[0]], writes=[B[0]])
            k.op("pool", lambda e: e.tensor_copy(out=xbuf[:, ct, 0:3], in_=xbuf[:, ct, 512:515]),
                 reads=[bx_], writes=[bx_])

        def stageB1(c, ct):
            s = (c * 8 + ct) % NT
            T = tmp[s]; B = btmp[s]
            xc = T[:, 0, :]
            pr = st.pb[2]; pi_ = st.pb[3]
            k.op("pe", lambda e: e.matmul(pr[:, :], WaBD[:, ct, :], xc, start=True, stop=True),
                 reads=[bWa, B[0]], writes=[st.bpb[2]])
            k.op("pe", lambda e: e.matmul(pi_[:, :], WxBD[:, ct, :], xc, start=True, stop=True),
                 reads=[bWx, B[0]], writes=[st.bpb[3]])
            r_ = T[:, 1, :]; i_ = T[:, 2, :]; a_ = T[:, 3, :]; a2 = T[:, 4, :]
            k.op("act", lambda e: e.activation(out=r_, in_=pr[:, :], func=AF.Tanh, scale=0.5, bias=sm[:, ct, 11:12]),
                 reads=[st.bpb[2], bsm], writes=[B[1]])
            k.op("act", lambda e: e.activation(out=i_, in_=pi_[:, :], func=AF.Tanh, scale=0.5, bias=sm[:, ct, 12:13]),
                 reads=[st.bpb[3], bsm], writes=[B[2]])
            k.op("act", lambda e: e.activation(out=a_, in_=r_, func=AF.Exp, scale=sm[:, ct, 13:14], bias=sm[:, ct, 13:14]),
                 reads=[B[1], bsm], writes=[B[3]])
            k.op("act", lambda e: e.activation(out=a2, in_=r_, func=AF.Exp, scale=sm[:, ct, 14:15], bias=sm[:, ct, 14:15]),
                 reads=[B[1], bsm], writes=[B[4]])

        def stageB2(c, ct):
            s = (c * 8 + ct) % NT
            T = tmp[s]; B = btmp[s]
            xc = T[:, 0, :]
            hs = T[:, 1, :]; i_ = T[:, 2, :]; a_ = T[:, 3, :]; a2 = T[:, 4, :]
            mu = a2; u_ = i_
            k.op("act", lambda e: e.activation(out=mu, in_=a2, func=AF.Sqrt, scale=-1.0, bias=1.0 + 2.0 ** -22),
                 reads=[B[4]], writes=[B[4]])
            k.op("dve", lambda e: e.scalar_tensor_tensor(out=u_, in0=i_, scalar=1.0, in1=xc, op0=ALU.add, op1=ALU.mult),
                 reads=[B[2], B[0]], writes=[B[2]])
            k.op("dve", lambda e: e.scalar_tensor_tensor(out=u_, in0=u_, scalar=0.5, in1=mu, op0=ALU.mult, op1=ALU.mult),
                 reads=[B[2], B[4]], writes=[B[2]])
            if c == 0:
                k.op("dve", lambda e: e.tensor_tensor(out=u_, in0=u_, in1=vrow[:, :], op=ALU.mult),
                     reads=[B[2], bvrow], writes=[B[2]])
            k.op("dve", lambda e: e.tensor_tensor_scan(out=hs, data0=a_, data1=u_, initial=carry[:, ct:ct + 1],
                                                        op0=ALU.mult, op1=ALU.add),
                 reads=[B[3], B[2], bcarry[ct]], writes=[B[1]])
            k.op("dve", lambda e: e.tensor_copy(out=carry[:, ct:ct + 1], in_=hs[:, 511:512]),
                 reads=[B[1]], writes=[bcarry[ct]])
            k.op("pool", lambda e: e.tensor_copy(out=st.hstate[:, ct, c * 128:(c + 1) * 128], in_=hs[:, 384:512]),
                 reads=[B[1]], writes=[st.b_hstate])

        norms(0)
        for m in range(33):
            if m < 32:
                for n in (2 * m, 2 * m + 1):
                    stageA(n // 8, n % 8)
            if m >= 1:
                for n in (2 * m - 2, 2 * m - 1):
                    stageB1(n // 8, n % 8)
                for n in (2 * m - 2, 2 * m - 1):
                    stageB2(n // 8, n % 8)
            if m % 4 == 2 and m // 4 + 1 < 8:
                norms(m // 4 + 1)
        k.barrier()


def build(upto="all", dbg=False, n_experts=32):
    P = Prog()
    out = P.outp("out", [NTOK, D])
    nc = P.nc
    with contextlib.ExitStack() as es:
        k = K(nc, es)
        st = St()
        st.k, st.nc, st.P = k, nc, P
        setup(st)
        alloc_mixer(st)
        phase_kv(st)
        if dbg and upto == "kv":
            P.outp("d_KT", [128, 4, S], BF16); P.outp("d_Vs", [128, NU, 4, 65], BF16)
            P.outp("d_Vw", [128, NU, 4, 65], BF16); P.outp("d_kcv", [128, 4, 256], BF16)
            P.outp("d_VC", [128, 2, 4, 129], BF16)
            k.dma("sp", P.dout["d_KT"], st.KT[:, :, :], reads=st.b_KT, is_output=True)
            k.dma("sp", P.dout["d_Vs"], st.Vs[:, :, :, :], reads=st.b_V, is_output=True)
            k.dma("sp", P.dout["d_Vw"], st.Vw[:, :, :, :], reads=st.b_V, is_output=True)
            k.dma("sp", P.dout["d_kcv"], st.kcv[:, :, :], reads=[st.b_kcv], is_output=True)
            k.dma("sp", P.dout["d_VC"], st.VC[:, :, :, :], reads=[st.b_VC], is_output=True)
            k.finish()
            return P
        phase_attn(st)
        if dbg and upto == "attn":
            P.outp("d_nsaT", [128, 8, NTOK], BF16)
            k.dma("sp", P.dout["d_nsaT"], st.nsaT[:, :, :], reads=[st.b_nsaT], is_output=True)
            k.finish()
            return P
        free_mixer(st)
        st.hstate = k.sb("hstate", [128, 8, NTOK], BF16); st.b_hstate = k.buf()
        phase_lru(st)
        if dbg and upto == "lru":
            P.outp("d_hstate", [128, 8, NTOK], BF16)
            k.dma("sp", P.dout["d_hstate"], st.hstate[:], reads=[st.b_hstate], is_output=True)
            k.finish()
            return P
        st.n_experts = n_experts
        phase_post(st, out)
        k.finish()
    return P


def host_consts(q):
    sh = 3 - q
    c = {}
    c["ident"] = np.eye(128, dtype=np.float32)
    pos = np.arange(512)
    c["vrow"] = np.broadcast_to((pos >= 128 * sh).astype(np.float32), (128, 512)).copy()
    c["vcol"] = np.broadcast_to((np.arange(NU) >= sh).astype(np.float32), (128, NU)).copy()
    slot = np.arange(256)
    cp = slot - 1
    cvalid = (cp >= 8 * sh)
    cm = np.zeros((128, NJ, 2, 128), np.float32)
    for j in range(NJ):
        u = 4 * j + 3
        tpos = 128 * u + np.arange(128)
        m = ((16 * cp[:, None] + 31) <= tpos[None, :]) & cvalid[:, None]
        cm[:, j, :, :] = m.reshape(2, 128, 128).transpose(1, 0, 2)
    c["cmaskT"] = cm
    c0 = cp * 16
    s0 = np.arange(64) * 64
    ov = np.minimum(c0[:, None] + 32, s0[None, :] + 64) - np.maximum(c0[:, None], s0[None, :])
    ov = np.clip(ov, 0, None) / 32.0
    ov[~cvalid] = 0.0
    c["ovm"] = ov.reshape(2, 128, 64).transpose(1, 0, 2).astype(np.float32).copy()
    j0 = 2 * sh
    jb = np.arange(64)
    scV = np.zeros((128, NJ, 64), np.float32); scN = np.zeros_like(scV); scF = np.zeros_like(scV)
    for j in range(NJ):
        u = 4 * j + 3
        blk = (128 * u + np.arange(128)) // 64
        V = (jb[None, :] >= j0) & (jb[None, :] <= blk[:, None])
        F = ((jb[None, :] == j0) | (jb[None, :] == blk[:, None]) | (jb[None, :] == blk[:, None] - 1)) & V
        scV[:, j] = V; scN[:, j] = V.astype(np.float32) - 1.0; scF[:, j] = np.where(F, 1e4, -2.0)
    c["scV"], c["scN"], c["scF"] = scV, scN, scF
    import ml_dtypes
    ea = (np.arange(128)[:, None] == (np.arange(S)[None, :] // 64)).astype(np.float32)
    c["eall"] = ea.astype(ml_dtypes.bfloat16)
    kk = np.arange(128)[:, None]; tt = np.arange(128)[None, :]
    tri = np.where(kk <= tt, 0.0, -30000.0)
    atri = np.where(kk > tt, 0.0, -30000.0)
    tn = np.stack([np.tile(tri, (1, 4)), np.tile(atri, (1, 4))], axis=1)
    c["trineg"] = tn.astype(ml_dtypes.bfloat16)
    return c


def prep_inputs(inputs):
    f = lambda a: np.ascontiguousarray(np.asarray(a, dtype=np.float32))
    x = f(inputs["x"]); p = f(inputs["p"])[0]
    sh_w = {}
    sh_w["w_in"] = f(inputs["w_in"])[0]
    for nm in ("ln_mix", "ln_ffn", "ln_ple",
               "cmp_k_pos", "cmp_k_w1", "cmp_k_w2", "cmp_v_pos", "cmp_v_w1", "cmp_v_w2",
               "w_nsa_up", "w_lru_up", "w_out", "w_gate", "w_up", "w_down", "w_ple", "w_ple_gate"):
        sh_w[nm] = f(inputs[nm])[0]
    cols = [f(inputs["conv_w"])[0][w] for w in range(4)] + [f(inputs["conv_b"])[0], f(inputs["lru_ba"])[0].reshape(1024),
                                                            f(inputs["lru_bx"])[0].reshape(1024), f(inputs["lru_lambda"])[0]]
    sh_w["lru_small"] = np.ascontiguousarray(np.stack(cols, axis=-1).reshape(8, 128, 8).transpose(1, 0, 2))
    sh_w["ln_final"] = f(inputs["ln_final"])
    sh_w["w_r"] = np.concatenate([f(inputs["w_grp"])[0], f(inputs["w_exp"])[0]], axis=1)
    sh_w["b_r"] = np.concatenate([f(inputs["b_grp"])[0], f(inputs["b_exp"])[0]], axis=0)
    for nm, src in (("wa_bd", "lru_wa"), ("wx_bd", "lru_wx")):
        w = f(inputs[src])[0]
        bd = np.zeros((8, 128, 128), np.float32)
        for n in range(16):
            t, o = divmod(n, 2)
            bd[t, o * 64:(o + 1) * 64, o * 64:(o + 1) * 64] = w[n]
        sh_w[nm] = bd
    in_maps = []
    for c in range(8):
        b, q = divmod(c, 4)
        sh = 3 - q
        xs = np.zeros((S, D), np.float32)
        xs[128 * sh:] = x[b, :S - 128 * sh]
        rows = np.concatenate([np.arange(128 * (4 * j + q), 128 * (4 * j + q + 1)) for j in range(NJ)])
        m = dict(sh_w)
        m["xs"] = xs
        m["pown"] = np.ascontiguousarray(p[b, rows])
        m.update(host_consts(q))
        in_maps.append(m)
    return in_maps


def assemble(results, key="out"):
    out = np.zeros((2, S, D), np.float32)
    for c in range(8):
        b, q = divmod(c, 4)
        o = np.asarray(results[c][key]).reshape(NJ, 128, D)
        for j in range(NJ):
            r = 4 * j + q
            out[b, 128 * r:128 * (r + 1)] = o[j]
    return out


_CACHE = {}


def kernel(**inputs):
    if "P" not in _CACHE:
        _CACHE["P"] = build()
    P = _CACHE["P"]
    in_maps = prep_inputs(inputs)
    in_maps = [{n: m[n] for n in P.din} for m in in_maps]
    res = run_bass_kernel_spmd(P.nc, in_maps, core_ids=list(range(8)))
    return assemble(res.results)


def alloc_mixer(st):
    k = st.k
    st.KT = k.sb("KT", [128, 4, S], BF16); st.b_KT = [k.buf() for _ in range(8)]
    st.Vs = k.sb("Vs", [128, NU, 4, 65], BF16); st.Vw = k.sb("Vw", [128, NU, 4, 65], BF16)
    st.b_V = [k.buf() for _ in range(NU)]
    st.kcv = k.sb("kcv", [128, 4, 256], BF16); st.b_kcv = k.buf()
    st.VC = k.sb("VC", [128, 2, 4, 129], BF16); st.b_VC = k.buf()


def free_mixer(st):
    for n in ("KT", "Vs", "Vw", "kcv", "VC"):
        st.k.sb_free(n)


def phase_kv(st):
    k, nc, P = st.k, st.nc, st.P
    din = P.din
    with contextlib.ExitStack() as es:
        xst = [k.sb("xst%d" % i, [128, D], F32, es) for i in range(2)]; bxst = [k.buf() for _ in range(2)]
        xbs = [(k.sb("xb%d" % i, [128, D], BF16, es), k.buf()) for i in range(4)]
        hT = k.sb("hT0", [128, NKT, 512], BF16, es); bhT = [k.buf() for _ in range(4)]
        gk = k.sb("gk", [128, 2, 128], BF16, es); bgk = k.buf()
        nti = [0]

        def fronts(c):
            for uu in range(4):
                u = 4 * c + uu
                xi = nti[0] % 2
                nti[0] += 1
                k.dma("sp", xst[xi][:, :], din["xs"][u * 128:(u + 1) * 128, :], writes=[bxst[xi]])
                norm_front(st, xst[xi][:, :], [bxst[xi]], xbs[uu], xbs[uu])

        def backs(c):
            for uu in range(4):
                norm_back(st, xbs[uu], st.gains["ln_mix"], hT[:, :, uu * 128:(uu + 1) * 128], bhT[uu],
                          (st.tp_view[uu % 2][:, :], st.b_tp[uu % 2]))

        fronts(0)
        Wkv = k.sb("Wkv", [128, NKT, 4, 128], BF16, es); bWkv = [k.buf() for _ in range(4)]
        Wcmp = k.sb("Wcmp", [128, NKT, 4, 128], BF16, es); bWcmp = [k.buf() for _ in range(4)]
        Wv = k.sb("Wv", [128, NKT, 512], BF16, es); bWv = k.buf()
        for g in range(4):
            for (W, bW, o1, o2) in ((Wkv, bWkv, O_KS, O_KW), (Wcmp, bWcmp, O_KC, O_VC)):
                for half, o in ((0, o1), (1, o2)):
                    k.dma("pool", W[:, :, g, half * 64:(half + 1) * 64],
                          din["w_in"][:, o + g * 64:o + (g + 1) * 64].rearrange("(kt p) d -> p kt d", p=128),
                          writes=[bW[g]], add=(half == 1))
        k.dma("pool", Wv[:, :, 0:256], din["w_in"][:, O_VS:O_VS + 256].rearrange("(kt p) d -> p kt d", p=128), writes=[bWv])
        k.dma("pool", Wv[:, :, 256:512], din["w_in"][:, O_VW:O_VW + 256].rearrange("(kt p) d -> p kt d", p=128),
              writes=[bWv], add=True)
        w1 = k.sb("w1", [128, 32, 128], BF16, es); bw1 = k.buf()
        k.dma("pool", w1[0:64, :, :], din["cmp_k_w1"].rearrange("(l d) h -> d l h", d=64), writes=[bw1])
        k.dma("pool", w1[64:128, :, :], din["cmp_v_w1"].rearrange("(l d) h -> d l h", d=64), writes=[bw1], add=True)
        w2p = k.sb("w2p", [128, 2, 128], BF16, es); bw2 = k.buf()
        k.op("pool", lambda e: e.memset(w2p[:, :, :], 0.0), writes=[bw2])
        k.dma("pool", w2p[:, 0, 0:64], din["cmp_k_w2"], writes=[bw2])
        k.dma("pool", w2p[:, 1, 64:128], din["cmp_v_w2"], writes=[bw2], add=True)
        posT = k.sb("posT", [128, 32], BF16, es); bpos = k.buf()
        k.dma("pool", posT[0:64, :], din["cmp_k_pos"].rearrange("l d -> d l"), writes=[bpos], allow_slow_non_contiguous=True)
        k.dma("pool", posT[64:128, :], din["cmp_v_pos"].rearrange("l d -> d l"), writes=[bpos], add=True,
              allow_slow_non_contiguous=True)
        vcol = k.sb("vcol", [128, NU], F32, es); bvcol = k.buf()
        k.dma("sp", vcol[:, :], din["vcol"], writes=[bvcol])
        ovm = k.sb("ovm", [128, 2, 64], F32, es); bovm = k.buf()
        k.dma("sp", ovm[:, :, :], din["ovm"], writes=[bovm])
        cbias = k.sb("cbias", [128, 2], F32, es); bcb = k.buf()
        KC = k.sb("KCbuf", [128, 4, 528], BF16, es); bKC = k.buf()
        k.op("pool", lambda e: e.memset(KC[:, :, 0:16], 0.0), writes=[bKC])
        for c in range(8):
            backs(c)
            if c > 0:
                k.op("pool", lambda e: e.tensor_copy(out=KC[:, :, 0:16], in_=KC[:, :, 512:528]), reads=[bKC], writes=[bKC])
            pi = 0
            for g in range(4):
                for which in range(2):
                    W, bW = (Wkv, bWkv) if which == 0 else (Wcmp, bWcmp)
                    pj = st.pb[pi % 2]; bpj = st.bpb[pi % 2]; pi += 1
                    mm_group(k, pj[:, :], [(W[:, kk, g, :], hT[:, kk, :]) for kk in range(NKT)],
                             reads=[bW[g]] + bhT, writes=[bpj])
                    if which == 0:
                        k.op("act", lambda e: e.activation(out=st.KT[:, g, c * 512:(c + 1) * 512], in_=pj[:, :], func=AF.Copy),
                             reads=[bpj], writes=[st.b_KT[c]])
                    else:
                        k.op("act", lambda e: e.activation(out=KC[:, g, 16:528], in_=pj[:, :], func=AF.Copy),
                             reads=[bpj], writes=[bKC])
            if c + 1 < 8:
                fronts(c + 1)
            for uu in range(4):
                u = 4 * c + uu
                pj = st.pb[pi % 2]; bpj = st.bpb[pi % 2]; pi += 1
                mm_group(k, pj[:, :], [(hT[:, kk, uu * 128:(uu + 1) * 128], Wv[:, kk, :]) for kk in range(NKT)],
                         reads=[bWv] + bhT, writes=[bpj])
                k.op("dve", lambda e: e.tensor_copy(out=st.Vs[:, u, :, 0:64], in_=pj[:, 0:256].rearrange("p (g d) -> p g d", d=64)),
                     reads=[bpj], writes=[st.b_V[u]])
                k.op("dve", lambda e: e.tensor_copy(out=st.Vw[:, u, :, 0:64], in_=pj[:, 256:512].rearrange("p (g d) -> p g d", d=64)),
                     reads=[bpj], writes=[st.b_V[u]])
                k.op("pool", lambda e: e.tensor_copy(out=st.Vs[:, u, :, 64:65], in_=bc(vcol[:, u:u + 1].unsqueeze(1), [128, 4, 1])),
                     reads=[bvcol], writes=[st.b_V[u]])
                k.op("pool", lambda e: e.tensor_copy(out=st.Vw[:, u, :, 64:65], in_=bc(vcol[:, u:u + 1].unsqueeze(1), [128, 4, 1])),
                     reads=[bvcol], writes=[st.b_V[u]])
            if c == 0:
                for hv in range(2):
                    lo = hv * 64
                    pc = st.pb[2 + hv]; bpc = st.bpb[2 + hv]
                    mm_group(k, pc[:, 0:1], [(w1[lo:lo + 64, l, :], posT[lo:lo + 64, l:l + 1]) for l in range(32)],
                             reads=[bw1, bpos], writes=[bpc])
                    k.op("dve", lambda e: e.tensor_copy(out=cbias[:, hv:hv + 1], in_=pc[:, 0:1]), reads=[bpc], writes=[bcb])
            ph = st.pb[2]; bph = st.bpb[2]
            ph2 = st.pb[3]; bph2 = st.bpb[3]
            for hv, (pp, bpp) in enumerate(((ph, bph), (ph2, bph2))):
                lo = hv * 64
                mm_group(k, pp[:, 0:128].rearrange("p (g b) -> p g b", b=32),
                         [(w1[lo:lo + 64, l, :], KC[lo:lo + 64, :, l:l + 497:16]) for l in range(32)],
                         reads=[bw1, bKC], writes=[bpp])
                k.op("act", lambda e: e.activation(out=gk[:, hv, :], in_=pp[:, 0:128], func=AF.Gelu_apprx_tanh,
                                                    bias=cbias[:, hv:hv + 1]), reads=[bpp, bcb], writes=[bgk])
            mm_group(k, ph[:, 128:256], [(w2p[:, 0, :], gk[:, 0, :]), (w2p[:, 1, :], gk[:, 1, :])],
                     reads=[bw2, bgk], writes=[bph])
            k.op("dve", lambda e: e.tensor_copy(out=st.kcv[:, :, c * 32:(c + 1) * 32],
                                                 in_=ph[:, 128:256].rearrange("p (g b) -> p g b", b=32)),
                 reads=[bph], writes=[st.b_kcv])
        tpb = st.tp_view[0]; btpb = st.b_tp[0]
        for ct in range(2):
            for g in range(4):
                k.op("pe", lambda e: e.transpose(out=tpb[:, (ct * 4 + g) * 64:(ct * 4 + g + 1) * 64],
                                                 in_=st.kcv[64:128, g, ct * 128:(ct + 1) * 128],
                                                 identity=st.identb[64:128, 64:128]),
                     reads=[st.b_kcv, st.b_identb], writes=[btpb], sig=(ct == 1 and g == 3))
        k.op("dve", lambda e: e.tensor_copy(out=st.VC[:, :, :, 0:64],
                                             in_=tpb[:, 0:512].rearrange("p (c g d) -> p c g d", c=2, g=4)),
             reads=[btpb], writes=[st.b_VC])
        k.op("pool", lambda e: e.memset(st.VC[:, :, :, 64:65], 1.0), writes=[st.b_VC])
        for g in range(4):
            k.op("pool", lambda e: e.tensor_copy(out=st.VC[:, :, g, 65:129], in_=ovm[:, :, :]), reads=[bovm], writes=[st.b_VC])
        k.barrier()


def phase_attn(st):
    k, nc, P = st.k, st.nc, st.P
    din = P.din
    st.nsaT = k.sb("nsaT", [128, 8, NTOK], BF16); st.b_nsaT = k.buf()
    with contextlib.ExitStack() as es:
        eall = k.sb("eall", [128, S], BF16, es); beall = k.buf()
        k.dma("sp", eall[:, :], din["eall"], writes=[beall])
        trineg = k.sb("trineg", [128, 2, 512], BF16, es); btri = k.buf()
        k.dma("sp", trineg[:, :, :], din["trineg"], writes=[btri])
        cmask = k.sb("cmask", [128, NJ, 2, 128], BF16, es); bcm = k.buf()
        k.dma("pool", cmask[:, :, :, :], din["cmaskT"], writes=[bcm])
        scc = k.sb("scc", [128, 3, NJ, 64], F32, es); bscc = k.buf()
        for i_, nm in enumerate(("scV", "scN", "scF")):
            k.dma("sp", scc[:, i_, :, :], din[nm], writes=[bscc], add=(i_ > 0))
        hTo = k.sb("hTo", [128, NKT, NTOK], BF16, es); bhTo = [k.buf() for _ in range(NJ)]
        gsb = k.sb("gsb", [128, NJ, 48], F32, es); bgsb = k.buf()
        Wg48 = k.sb("Wg48", [128, NKT, 48], BF16, es); bWg = k.buf()
        k.dma("pool", Wg48[:, :, :], din["w_in"][:, O_G:O_G + 48].rearrange("(kt p) d -> p kt d", p=128), writes=[bWg])
        with contextlib.ExitStack() as es2:
            xst = [k.sb("xst%d" % i, [128, D], F32, es2) for i in range(2)]; bxst = [k.buf() for _ in range(2)]
            xbs = [(k.sb("xb%d" % i, [128, D], BF16, es2), k.buf()) for i in range(4)]
            for j in range(NJ):
                if j % 4 == 0:
                    for j2 in range(j, j + 4):
                        u = 4 * j2 + 3
                        xi = j2 % 2
                        k.dma("sp", xst[xi][:, :], din["xs"][u * 128:(u + 1) * 128, :], writes=[bxst[xi]])
                        norm_front(st, xst[xi][:, :], [bxst[xi]], xbs[j2 % 4], xbs[j2 % 4])
                norm_back(st, xbs[j % 4], st.gains["ln_mix"], hTo[:, :, j * 128:(j + 1) * 128], bhTo[j],
                          (st.tp_view[j % 2][:, :], st.b_tp[j % 2]))
                pg = st.pb[j % 2]; bpg = st.bpb[j % 2]
                mm_group(k, pg[:, 0:48], [(hTo[:, kk, j * 128:(j + 1) * 128], Wg48[:, kk, :]) for kk in range(NKT)],
                         reads=[bhTo[j], bWg], writes=[bpg])
                k.op("act", lambda e: e.activation(out=gsb[:, j, :], in_=pg[:, 0:48], func=AF.Sigmoid),
                     reads=[bpg], writes=[bgsb])
            k.barrier()
        Wq = k.sb("Wq", [128, NKT, 4, 128], BF16, es); bWq = k.buf()
        qTs = k.sb("qTs", [128, 4, NTOK], BF16, es); qTw = k.sb("qTw", [128, 4, NTOK], BF16, es); bqT = k.buf()
        k.op("pool", lambda e: e.memset(qTs[64:128, :, :], 0.0), writes=[bqT])
        k.op("pool", lambda e: e.memset(qTw[0:64, :, :], 0.0), writes=[bqT])
        EcTA = k.sb("EcT", [128, 2, 2, 512], BF16, es); bEcA = [[k.buf(), k.buf()], [k.buf(), k.buf()]]
        NPT = 3
        PT = [k.sb("PT%d" % i, [128, 512], BF16, es) for i in range(NPT)]; bPT = [k.buf() for _ in range(NPT)]
        smA = k.sb("att_sm", [128, 2, 64], F32, es); bsmA = [k.buf(), k.buf()]
        impA = k.sb("imp", [128, 2, 3, 64], F32, es); bimpA = [k.buf(), k.buf()]
        NEGT = k.sb("NEGT", [128, 4, 128], BF16, es); bNEG = k.buf()
        k.op("pool", lambda e: e.memset(NEGT[:, :, :], 0.0), writes=[bNEG])
        stgA = k.sb("stg", [128, 2, 2, 256], F32, es); bstgA = [k.buf(), k.buf()]
        stgbA = k.sb("stgb", [128, 2, 256], BF16, es); bstgbA = [k.buf(), k.buf()]
        tpf = st.tp_view[1][:, :].bitcast(F32)
        btpf = st.b_tp[1]
        tp0f = st.tp_view[0][:, :].bitcast(F32)
        tpn = st.tp_view[0]; btpn = st.b_tp[0]
        Oc = tpf[:, 512:1024]; bOc = k.buf()
        Ic = tp0f[:, 512:1024]; bIc = k.buf()
        pti = 0
        sbank = 0

        def run_branch(units, Obank, bO, vfirst_start):
            nonlocal pti, sbank
            n = len(units)
            slots = []
            first_pv = [True]

            def emit_S(i):
                nonlocal sbank
                sb_ = sbank % 2
                sbank += 1
                Sb = st.pb[sb_]; bS = st.bpb[sb_]
                sc = units[i]["score"]
                for mi, (cols, l, r, rd) in enumerate(sc):
                    o_ = Sb[:, cols[0]:cols[1]]
                    if len(r.shape) == 3:
                        o_ = o_.rearrange("p (h t) -> p h t", h=r.shape[1])
                    mm1(k, o_, l, r, start=(mi == 0), reads=rd, writes=[bS], sig=(mi == len(sc) - 1))
                return Sb, bS

            def emit_exp(i, Sb, bS):
                nonlocal pti
                p_ = pti % NPT
                pti += 1
                k.op("act", lambda e: e.activation(out=PT[p_][:, :], in_=Sb[:, :], func=AF.Exp, scale=0.125),
                     reads=[bS], writes=[bPT[p_]])
                return p_

            def emit_PV(i, p_):
                vr, vb = units[i]["v"]
                for hh in range(4):
                    mm1(k, Obank[:, hh * 65:(hh + 1) * 65], PT[p_][:, hh * 128:(hh + 1) * 128], vr,
                        start=(first_pv[0]), reads=[bPT[p_]] + vb, writes=[bO], sig=(hh == 3))
                    first_pv[0] = False

            pend = []
            for i in range(n):
                Sb, bS = emit_S(i)
                p_ = emit_exp(i, Sb, bS)
                pend.append((i, p_))
                if len(pend) >= 3:
                    ii, pp = pend.pop(0)
                    emit_PV(ii, pp)
            for ii, pp in pend:
                emit_PV(ii, pp)

        for g in range(4):
            first = True
            for hh in range(4):
                h = 4 * g + hh
                for half in range(2):
                    k.dma("pool", Wq[:, :, hh, half * 64:(half + 1) * 64],
                          din["w_in"][:, O_Q + h * 64:O_Q + (h + 1) * 64].rearrange("(kt p) d -> p kt d", p=128),
                          writes=[bWq], add=not first)
                    first = False
            for hh in range(4):
                for half in range(2):
                    pj = st.pb[(hh * 2 + half) % 2]; bpj = st.bpb[(hh * 2 + half) % 2]
                    mm_group(k, pj[:, :], [(Wq[:, kk, hh, :], hTo[:, kk, half * 512:(half + 1) * 512]) for kk in range(NKT)],
                             reads=[bWq] + bhTo, writes=[bpj])
                    k.op("act", lambda e: e.activation(out=qTs[0:64, hh, half * 512:(half + 1) * 512], in_=pj[0:64, :], func=AF.Copy),
                         reads=[bpj], writes=[bqT])
                    k.op("dve", lambda e: e.tensor_copy(out=qTw[64:128, hh, half * 512:(half + 1) * 512], in_=pj[64:128, :]),
                         reads=[bpj], writes=[bqT])
            def ctx(j):
                p = j % 2
                return dict(u=4 * j + 3, tok=slice(j * 128, (j + 1) * 128), sm=smA[:, p, :], bsm=bsmA[p], imp=impA[:, p, :, :], bimp=bimpA[p],
                            stg=stgA[:, p, :, :], bstg=bstgA[p], EcT=EcTA[:, p, :, :], bEc=bEcA[p],
                            gv=gsb[:, j, :].rearrange("p (h b) -> p h b", b=3))

            def cmp_a1(j):
                nonlocal sbank
                c_ = ctx(j); u = c_["u"]; tok = c_["tok"]; sm = c_["sm"]; bsm = c_["bsm"]; imp = c_["imp"]; bimp = c_["bimp"]
                stg = c_["stg"]; bstg = c_["bstg"]; EcT = c_["EcT"]; bEc = c_["bEc"]; gv = c_["gv"]
                Oc3 = Oc[:, 0:260].rearrange("p (h d) -> p h d", d=65)
                for ct in range(2):
                    Sb = st.pb[sbank % 2]; bS = st.bpb[sbank % 2]; sbank += 1
                    mm1(k, Sb[:, :].rearrange("p (h t) -> p h t", h=4), st.kcv[:, g, ct * 128:(ct + 1) * 128], qTs[:, :, tok],
                        start=True, reads=[st.b_kcv, bqT], writes=[bS], sig=True)
                    k.op("act", lambda e: e.activation(out=EcT[:, ct, :], in_=Sb[:, :], func=AF.Exp, scale=0.125),
                         reads=[bS], writes=[bEc[ct]])
                    k.op("dve", lambda e: e.tensor_tensor(out=EcT[:, ct, :].rearrange("p (h t) -> p h t", h=4),
                                                           in0=EcT[:, ct, :].rearrange("p (h t) -> p h t", h=4),
                                                           in1=bc(cmask[:, j, ct, :].unsqueeze(1), [128, 4, 128]), op=ALU.mult),
                         reads=[bEc[ct], bcm], writes=[bEc[ct]])

            def cmp_a2(j):
                c_ = ctx(j); u = c_["u"]; tok = c_["tok"]; sm = c_["sm"]; bsm = c_["bsm"]; imp = c_["imp"]; bimp = c_["bimp"]
                stg = c_["stg"]; bstg = c_["bstg"]; EcT = c_["EcT"]; bEc = c_["bEc"]; gv = c_["gv"]
                Oc3 = Oc[:, 0:260].rearrange("p (h d) -> p h d", d=65)
                fo = True
                for hh in range(4):
                    for ct in range(2):
                        mm1(k, Oc[:, hh * 65:(hh + 1) * 65], EcT[:, ct, hh * 128:(hh + 1) * 128], st.VC[:, ct, g, 0:65],
                            start=fo, reads=[bEc[ct], st.b_VC], writes=[bOc], sig=(hh == 3 and ct == 1))
                        mm1(k, Ic[:, hh * 64:(hh + 1) * 64], EcT[:, ct, hh * 128:(hh + 1) * 128], st.VC[:, ct, g, 65:129],
                            start=fo, reads=[bEc[ct], st.b_VC], writes=[bIc], sig=(hh == 3 and ct == 1))
                        fo = False
                k.op("dve", lambda e: e.tensor_scalar(out=sm[:, 0:4], in0=Oc3[:, :, 64], scalar1=1e-30, scalar2=None, op0=ALU.max),
                     reads=[bOc], writes=[bsm])
                k.op("dve", lambda e: e.reciprocal(out=sm[:, 0:4], in_=sm[:, 0:4]), reads=[bsm], writes=[bsm])
                k.op("dve", lambda e: e.tensor_scalar(out=imp[:, 0, :], in0=Ic[:, 0:64], scalar1=sm[:, 0:1], scalar2=None, op0=ALU.mult),
                     reads=[bIc, bsm], writes=[bimp])
                for hh in range(1, 4):
                    k.op("dve", lambda e: e.scalar_tensor_tensor(out=imp[:, 0, :], in0=Ic[:, hh * 64:(hh + 1) * 64], scalar=sm[:, hh:hh + 1],
                                                                  in1=imp[:, 0, :], op0=ALU.mult, op1=ALU.add),
                         reads=[bIc, bsm, bimp], writes=[bimp])
                k.op("dve", lambda e: e.tensor_tensor(out=imp[:, 0, :], in0=imp[:, 0, :], in1=scc[:, 0, j, :], op=ALU.mult),
                     reads=[bimp, bscc], writes=[bimp])
                k.op("dve", lambda e: e.tensor_tensor(out=imp[:, 0, :], in0=imp[:, 0, :], in1=scc[:, 1, j, :], op=ALU.add),
                     reads=[bimp, bscc], writes=[bimp])
                k.op("dve", lambda e: e.tensor_tensor(out=imp[:, 0, :], in0=imp[:, 0, :], in1=scc[:, 2, j, :], op=ALU.max),
                     reads=[bimp, bscc], writes=[bimp])
                k.op("dve", lambda e: e.max(out=sm[:, 16:24], in_=imp[:, 0, :]), reads=[bimp], writes=[bsm])
                k.op("dve", lambda e: e.match_replace(out=imp[:, 1, :], in_to_replace=sm[:, 16:24], in_values=imp[:, 0, :], imm_value=-1e30),
                     reads=[bimp, bsm], writes=[bimp])
                k.op("dve", lambda e: e.max(out=sm[:, 24:32], in_=imp[:, 1, :]), reads=[bimp], writes=[bsm])
                k.op("dve", lambda e: e.tensor_scalar(out=sm[:, 32:33], in0=sm[:, 31:32], scalar1=0.0, scalar2=None, op0=ALU.max),
                     reads=[bsm], writes=[bsm])
                k.op("dve", lambda e: e.tensor_scalar(out=imp[:, 2, :], in0=imp[:, 0, :], scalar1=sm[:, 32:33], scalar2=30000.0,
                                                       op0=ALU.is_ge, op1=ALU.mult), reads=[bimp, bsm], writes=[bimp])
                k.op("dve", lambda e: e.tensor_scalar(out=imp[:, 2, :], in0=imp[:, 2, :], scalar1=-30000.0, scalar2=None, op0=ALU.add),
                     reads=[bimp], writes=[bimp])
                k.op("dve", lambda e: e.tensor_tensor(out=sm[:, 12:16], in0=sm[:, 0:4], in1=gv[:, 4 * g:4 * g + 4, 0], op=ALU.mult),
                     reads=[bsm, bgsb], writes=[bsm])
                k.op("dve", lambda e: e.tensor_tensor(out=stg[:, 0, :].rearrange("p (h d) -> p h d", d=64), in0=Oc3[:, :, 0:64],
                                                       in1=bc(sm[:, 12:16].unsqueeze(2), [128, 4, 64]), op=ALU.mult),
                     reads=[bOc, bsm], writes=[bstg])

            def cmp_b(j):
                c_ = ctx(j); imp = c_["imp"]; bimp = c_["bimp"]
                k.op("pe", lambda e: e.transpose(out=tpf[0:64, 0:128], in_=imp[:, 2, :], identity=st.identf[:, :]),
                     reads=[bimp, st.b_identf], writes=[btpf])
                k.op("dve", lambda e: e.tensor_copy(out=NEGT[0:64, :, :], in_=bc(tpf[0:64, 0:128].unsqueeze(1), [64, 4, 128])),
                     reads=[btpf], writes=[bNEG])

            def slc_(j):
                c_ = ctx(j); u = c_["u"]; tok = c_["tok"]; sm = c_["sm"]; bsm = c_["bsm"]; stg = c_["stg"]; bstg = c_["bstg"]; gv = c_["gv"]
                NEG2 = NEGT[:, :, :].rearrange("p h t -> p (h t)")
                units = []
                for kt in range(u + 1):
                    ksl = slice(kt * 128, (kt + 1) * 128)
                    sc = [((0, 512), eall[:, ksl], NEG2, [beall, bNEG])]
                    if kt == u:
                        sc.append(((0, 512), st.identb[:, :], trineg[:, 0, :], [st.b_identb, btri]))
                    sc.append(((0, 512), st.KT[:, g, ksl], qTs[:, :, tok], [st.b_KT[kt // 4], bqT]))
                    units.append(dict(score=sc, v=(st.Vs[:, kt, g, :], [st.b_V[kt]])))
                Os = st.pb[2]; bOs = st.bpb[2]
                run_branch(units, Os, bOs, True)
                Os3 = Os[:, 0:260].rearrange("p (h d) -> p h d", d=65)
                k.op("dve", lambda e: e.tensor_scalar(out=sm[:, 4:8], in0=Os3[:, :, 64], scalar1=1e-30, scalar2=None, op0=ALU.max),
                     reads=[bOs], writes=[bsm])
                k.op("dve", lambda e: e.reciprocal(out=sm[:, 4:8], in_=sm[:, 4:8]), reads=[bsm], writes=[bsm])
                k.op("dve", lambda e: e.tensor_tensor(out=sm[:, 12:16], in0=sm[:, 4:8], in1=gv[:, 4 * g:4 * g + 4, 1], op=ALU.mult),
                     reads=[bsm, bgsb], writes=[bsm])
                k.op("dve", lambda e: e.tensor_tensor(out=stg[:, 1, :].rearrange("p (h d) -> p h d", d=64), in0=Os3[:, :, 0:64],
                                                       in1=bc(sm[:, 12:16].unsqueeze(2), [128, 4, 64]), op=ALU.mult),
                     reads=[bOs, bsm], writes=[bstg])
                k.op("dve", lambda e: e.tensor_tensor(out=stg[:, 0, :], in0=stg[:, 0, :], in1=stg[:, 1, :], op=ALU.add),
                     reads=[bstg], writes=[bstg])

            def win_(j):
                c_ = ctx(j); u = c_["u"]; tok = c_["tok"]; sm = c_["sm"]; bsm = c_["bsm"]; stg = c_["stg"]; bstg = c_["bstg"]; gv = c_["gv"]
                units = []
                for kt in range(max(u - 4, 0), u + 1):
                    ksl = slice(kt * 128, (kt + 1) * 128)
                    sc = []
                    if kt == u:
                        sc.append(((0, 512), st.identb[:, :], trineg[:, 0, :], [st.b_identb, btri]))
                    if kt == u - 4:
                        sc.append(((0, 512), st.identb[:, :], trineg[:, 1, :], [st.b_identb, btri]))
                    sc.append(((0, 512), st.KT[:, g, ksl], qTw[:, :, tok], [st.b_KT[kt // 4], bqT]))
                    units.append(dict(score=sc, v=(st.Vw[:, kt, g, :], [st.b_V[kt]])))
                Ow = st.pb[3]; bOw = st.bpb[3]
                run_branch(units, Ow, bOw, True)
                Ow3 = Ow[:, 0:260].rearrange("p (h d) -> p h d", d=65)
                k.op("dve", lambda e: e.tensor_scalar(out=sm[:, 8:12], in0=Ow3[:, :, 64], scalar1=1e-30, scalar2=None, op0=ALU.max),
                     reads=[bOw], writes=[bsm])
                k.op("dve", lambda e: e.reciprocal(out=sm[:, 8:12], in_=sm[:, 8:12]), reads=[bsm], writes=[bsm])
                k.op("dve", lambda e: e.tensor_tensor(out=sm[:, 12:16], in0=sm[:, 8:12], in1=gv[:, 4 * g:4 * g + 4, 2], op=ALU.mult),
                     reads=[bsm, bgsb], writes=[bsm])
                k.op("dve", lambda e: e.tensor_tensor(out=stg[:, 1, :].rearrange("p (h d) -> p h d", d=64), in0=Ow3[:, :, 0:64],
                                                       in1=bc(sm[:, 12:16].unsqueeze(2), [128, 4, 64]), op=ALU.mult),
                     reads=[bOw, bsm], writes=[bstg])
                stgb = stgbA[:, j % 2, :]; bstgb = bstgbA[j % 2]
                k.op("dve", lambda e: e.tensor_tensor(out=stgb[:, :], in0=stg[:, 0, :], in1=stg[:, 1, :], op=ALU.add),
                     reads=[bstg], writes=[bstgb])

            def fin_(j):
                tok = slice(j * 128, (j + 1) * 128)
                stgb = stgbA[:, j % 2, :]; bstgb = bstgbA[j % 2]
                for t2 in range(2):
                    k.op("pe", lambda e: e.transpose(out=tpn[:, t2 * 128:(t2 + 1) * 128], in_=stgb[:, t2 * 128:(t2 + 1) * 128],
                                                     identity=st.identb[:, :]),
                         reads=[bstgb, st.b_identb], writes=[btpn], sig=(t2 == 1))
                k.op("act", lambda e: e.activation(out=st.nsaT[:, 2 * g:2 * g + 2, tok],
                                                    in_=tpn[:, 0:256].rearrange("p (a t) -> p a t", a=2), func=AF.Copy),
                     reads=[btpn], writes=[st.b_nsaT])

            cmp_a1(0)
            cmp_a2(0)
            cmp_b(0)
            for j in range(NJ):
                if j + 1 < NJ:
                    cmp_a1(j + 1)
                slc_(j)
                if j + 1 < NJ:
                    cmp_a2(j + 1)
                win_(j)
                if j + 1 < NJ:
                    cmp_b(j + 1)
                if j >= 1:
                    fin_(j - 1)
            fin_(NJ - 1)
        k.barrier()


def phase_post(st, out_ap):
    k, nc, P = st.k, st.nc, st.P
    din = P.din
    WS = [k.sb("wslot%d" % i, [128, NKT, 512], BF16) for i in range(4)]
    bWS = [k.buf() for _ in range(4)]
    wsi = [0]

    def wslot():
        i = wsi[0] % 4
        wsi[0] += 1
        return WS[i], bWS[i]

    mergedT = k.sb("mergedT", [128, NKT, NTOK], BF16); bmT = [k.buf() for _ in range(NKT)]
    with contextlib.ExitStack() as es:
        hTo = k.sb("hTo", [128, NKT, NTOK], BF16, es); bhTo = [k.buf() for _ in range(NJ)]
        xst = [k.sb("xst%d" % i, [128, D], F32, es) for i in range(2)]; bxst = [k.buf() for _ in range(2)]
        xbs = [(k.sb("xb%d" % i, [128, D], BF16, es), k.buf()) for i in range(2)]
        for j in range(NJ):
            if j % 2 == 0:
                for j2 in range(j, j + 2):
                    u = 4 * j2 + 3
                    xi = j2 % 2
                    k.dma("sp", xst[xi][:, :], din["xs"][u * 128:(u + 1) * 128, :], writes=[bxst[xi]])
                    norm_front(st, xst[xi][:, :], [bxst[xi]], xbs[j2 % 2], xbs[j2 % 2])
            norm_back(st, xbs[j % 2], st.gains["ln_mix"], hTo[:, :, j * 128:(j + 1) * 128], bhTo[j],
                      (st.tp_view[j % 2][:, :], st.b_tp[j % 2]))
        tmp = [k.sb("b1tmp%d" % i, [128, 2, 512], F32, es) for i in range(2)]; btmp = [k.buf() for _ in range(2)]
        ti = 0
        pbi = 0
        for cc in range(2):
            W, bW = wslot()
            k.dma("pool", W[:, :, :], din["w_in"][:, O_LY + cc * 512:O_LY + (cc + 1) * 512].rearrange("(kt p) n -> p kt n", p=128),
                  writes=[bW])
            for ct in range(4):
                for half in range(2):
                    hs = slice(half * 512, (half + 1) * 512)
                    pj = st.pb[pbi % 4]; bpj = st.bpb[pbi % 4]; pbi += 1
                    mm_group(k, pj[:, :], [(W[:, kk, ct * 128:(ct + 1) * 128], hTo[:, kk, hs]) for kk in range(NKT)],
                             reads=[bW] + bhTo, writes=[bpj])
                    T = tmp[ti % 2]; bT = btmp[ti % 2]; ti += 1
                    k.op("act", lambda e: e.activation(out=T[:, 0, :], in_=pj[:, :], func=AF.Gelu_apprx_tanh), reads=[bpj], writes=[bT])
                    k.op("dve", lambda e: e.tensor_tensor(out=st.hstate[:, cc * 4 + ct, hs], in0=st.hstate[:, cc * 4 + ct, hs],
                                                           in1=T[:, 0, :], op=ALU.mult), reads=[bT, st.b_hstate], writes=[st.b_hstate])
        lruT = st.hstate
        tpfB = [st.tp_view[i][:, :].bitcast(F32) for i in range(2)]
        banksets = [([st.pb[i][:, :] for i in range(4)], [st.bpb[i] for i in range(4)]),
                    ([tpfB[0][:, 0:512], tpfB[0][:, 512:1024], tpfB[1][:, 0:512], tpfB[1][:, 512:1024]], [k.buf() for _ in range(4)])]
        mi_ = [0]
        k.barrier()
        for dc in range(4):
            cs = slice(dc * 512, (dc + 1) * 512)
            Wn, bWn = wslot(); Wl, bWl = wslot(); Wa, bWa = wslot(); Wb, bWb = wslot()
            Wn3 = Wn[:, 0:8, :]; Wl3 = Wl[:, 0:8, :]
            k.dma("pool", Wn3, din["w_nsa_up"][:, cs].rearrange("(kt p) n -> p kt n", p=128), writes=[bWn])
            k.dma("pool", Wl3, din["w_lru_up"][:, cs].rearrange("(kt p) n -> p kt n", p=128), writes=[bWl])
            k.dma("pool", Wa[:, :, :], din["w_in"][:, O_MA + dc * 512:O_MA + (dc + 1) * 512].rearrange("(kt p) n -> p kt n", p=128),
                  writes=[bWa])
            k.dma("pool", Wb[:, :, :], din["w_in"][:, O_MB + dc * 512:O_MB + (dc + 1) * 512].rearrange("(kt p) n -> p kt n", p=128),
                  writes=[bWb])
            for dt in range(4):
                ds_ = slice(dt * 128, (dt + 1) * 128)
                for half in range(2):
                    hs = slice(half * 512, (half + 1) * 512)
                    PB, BPB = banksets[mi_[0] % 2]
                    mi_[0] += 1
                    mm_group(k, PB[0], [(Wn3[:, kk, ds_], st.nsaT[:, kk, hs]) for kk in range(8)],
                             reads=[bWn, st.b_nsaT], writes=[BPB[0]])
                    mm_group(k, PB[1], [(Wl3[:, kk, ds_], lruT[:, kk, hs]) for kk in range(8)],
                             reads=[bWl, st.b_hstate], writes=[BPB[1]])
                    mm_group(k, PB[2], [(Wa[:, kk, ds_], hTo[:, kk, hs]) for kk in range(NKT)],
                             reads=[bWa] + bhTo, writes=[BPB[2]])
                    mm_group(k, PB[3], [(Wb[:, kk, ds_], hTo[:, kk, hs]) for kk in range(NKT)],
                             reads=[bWb] + bhTo, writes=[BPB[3]])
                    T = tmp[ti % 2]; bT = btmp[ti % 2]; ti += 1
                    k.op("act", lambda e: e.activation(out=T[:, 0, :], in_=PB[2], func=AF.Sigmoid), reads=[BPB[2]], writes=[bT])
                    k.op("act", lambda e: e.activation(out=T[:, 1, :], in_=PB[3], func=AF.Sigmoid), reads=[BPB[3]], writes=[bT])
                    k.op("dve", lambda e: e.tensor_tensor(out=T[:, 0, :], in0=PB[0], in1=T[:, 0, :], op=ALU.mult),
                         reads=[BPB[0], bT], writes=[bT])
                    k.op("dve", lambda e: e.tensor_tensor(out=T[:, 1, :], in0=PB[1], in1=T[:, 1, :], op=ALU.mult),
                         reads=[BPB[1], bT], writes=[bT])
                    k.op("dve", lambda e: e.tensor_tensor(out=mergedT[:, dc * 4 + dt, hs], in0=T[:, 0, :], in1=T[:, 1, :], op=ALU.add),
                         reads=[bT], writes=[bmT[dc * 4 + dt]])
        k.barrier()
    k.sb_free("hstate"); k.sb_free("nsaT")
    acc = k.sb("acc", [128, NJ, D], F32); bacc = [k.buf() for _ in range(NJ)]
    for j in range(NJ):
        u = 4 * j + 3
        k.dma("sp", acc[:, j, :], din["xs"][u * 128:(u + 1) * 128, :], writes=[bacc[j]])
    pbi = 0
    for dc in range(4):
        cs = slice(dc * 512, (dc + 1) * 512)
        W, bW = wslot()
        k.dma("pool", W[:, :, :], din["w_out"][:, cs].rearrange("(kt p) n -> p kt n", p=128), writes=[bW])
        for j in range(NJ):
            pj = st.pb[pbi % 4]; bpj = st.bpb[pbi % 4]; pbi += 1
            mm_group(k, pj[:, :], [(mergedT[:, kk, j * 128:(j + 1) * 128], W[:, kk, :]) for kk in range(NKT)],
                     reads=[bW] + bmT, writes=[bpj])
            k.op("dve", lambda e: e.tensor_tensor(out=acc[:, j, cs], in0=pj[:, :], in1=acc[:, j, cs], op=ALU.add),
                 reads=[bpj, bacc[j]], writes=[bacc[j]])
    k.barrier()
    k.sb_free("mergedT")
    xnT = k.sb("xnT", [128, NKT, NTOK], BF16); bxnT = [k.buf() for _ in range(NJ)]
    comb = k.sb("comb", [128, NJ, 32], F32); bcomb = k.buf()
    tpf = [st.tp_view[i][:, :].bitcast(F32) for i in range(2)]
    slots = {}

    def load(kind, e):
        W, bW = wslot()
        if kind == "g":
            src = din["w_gate"][e].rearrange("(kt p) n -> p kt n", p=128); dst = W[:, :, :]
        elif kind == "u":
            src = din["w_up"][e].rearrange("(kt p) n -> p kt n", p=128); dst = W[:, :, :]
        else:
            src = din["w_down"][e].rearrange("(ft p) n -> p ft n", p=128)
            dst = W[:, :, :].rearrange("p a b -> p (a b)").rearrange("p (f n) -> p f n", f=4)
        k.dma("pool", dst, src, writes=[bW])
        slots[(kind, e)] = (dst, bW)

    load("g", 0); load("u", 0); load("d", 0)
    with contextlib.ExitStack() as es:
        Wr = k.sb("Wr", [128, NKT, 36], F32, es); bWr = k.buf()
        k.dma("sp", Wr[:, :, :], din["w_r"].rearrange("(kt p) n -> p kt n", p=128), writes=[bWr])
        br = k.sb("br", [128, 36], F32, es); bbr = k.buf()
        k.dma("sp", br[:, :], din["b_r"].partition_broadcast(128), writes=[bbr])
        xs32L = [k.sb("xs32_0", [128, D], F32, es)] * 2; bxs32L = [k.buf()] * 2
        xT32L = [k.sb("xT32_%d" % i, [128, NKT, 128], F32, es) for i in range(2)]; bxT32L = [k.buf(), k.buf()]
        junk = (k.sb("junkb", [128, D], BF16, es), k.buf())
        rsA = k.sb("rsm", [128, NJ, 80], F32, es); brs = k.buf()
        rtmp = k.sb("rtmp", [128, NJ, 4, 8], F32, es); brtmp = k.buf()
        gffn, bgffn = st.gains["ln_ffn"]
        for j in range(NJ):
            xs32 = xs32L[j % 2]; bxs32 = bxs32L[j % 2]; xT32 = xT32L[j % 2]; bxT32 = bxT32L[j % 2]
            i = st.nt_i % 4; st.nt_i += 1
            sv = st.stat[:, i, :]; bs = st.bstat[i]
            k.op("act", lambda e: e.activation(out=junk[0][:, :], in_=acc[:, j, :], func=AF.Square, accum_out=sv[:, 0:1]),
                 reads=[bacc[j]], writes=[junk[1], bs])
            k.op("dve", lambda e: e.tensor_scalar(out=sv[:, 1:2], in0=sv[:, 0:1], scalar1=1.0 / D, scalar2=EPS, op0=ALU.mult, op1=ALU.add),
                 reads=[bs], writes=[bs])
            k.op("pool", lambda e: e.tensor_tensor(out=sv[:, 3:4], in0=sv[:, 1:2], in1=st.cneg[:, 0:1], op=ALU.pow), reads=[bs, st.b_cneg], writes=[bs])
            k.op("dve", lambda e: e.tensor_scalar(out=xs32[:, :], in0=acc[:, j, :], scalar1=sv[:, 3:4], scalar2=None, op0=ALU.mult),
                 reads=[bacc[j], bs], writes=[bxs32])
            for hf in range(2):
                tp_ = tpf[hf]; btp_ = st.b_tp[hf]
                for kk in range(8):
                    kq = hf * 8 + kk
                    k.op("pe", lambda e: e.transpose(out=tp_[:, kk * 128:(kk + 1) * 128], in_=xs32[:, kq * 128:(kq + 1) * 128],
                                                     identity=st.identf[:, :]),
                         reads=[bxs32, st.b_identf], writes=[btp_], sig=(kk == 7))
                k.op("dve", lambda e: e.tensor_tensor(out=xT32[:, hf * 8:(hf + 1) * 8, :], in0=tp_[:, :].rearrange("p (k t) -> p k t", t=128),
                                                       in1=bc(gffn[:, hf * 8:(hf + 1) * 8].unsqueeze(2), [128, 8, 128]), op=ALU.mult),
                     reads=[btp_, bgffn], writes=[bxT32])
            k.op("act", lambda e: e.activation(out=xnT[:, :, j * 128:(j + 1) * 128], in_=xT32[:, :, :], func=AF.Copy), reads=[bxT32], writes=[bxnT[j]])
            pr = st.pb[j % 2]; bpr = st.bpb[j % 2]
            mm_group(k, pr[:, 0:36], [(xT32[:, kk, :], Wr[:, kk, :]) for kk in range(NKT)], reads=[bxT32, bWr], writes=[bpr])
            k.op("dve", lambda e: e.tensor_tensor(out=rsA[:, j, 0:36], in0=pr[:, 0:36], in1=br[:, :], op=ALU.add), reads=[bpr, bbr], writes=[brs])
        V = lambda a_, b_: rsA[:, :, a_:b_]
        R = [brs]
        k.op("dve", lambda e: e.reduce_max(out=V(36, 37), in_=V(0, 4), axis=AX.X), reads=R, writes=R)
        k.op("dve", lambda e: e.tensor_tensor(out=V(40, 44), in0=V(0, 4), in1=bc(V(36, 37), [128, NJ, 4]), op=ALU.subtract), reads=R, writes=R)
        k.op("act", lambda e: e.activation(out=V(40, 44), in_=V(40, 44), func=AF.Exp), reads=R, writes=R)
        k.op("dve", lambda e: e.reduce_sum(out=V(38, 39), in_=V(40, 44), axis=AX.X), reads=R, writes=R)
        k.op("dve", lambda e: e.reciprocal(out=V(39, 40), in_=V(38, 39)), reads=R, writes=R)
        k.op("dve", lambda e: e.tensor_tensor(out=V(44, 48), in0=V(0, 4), in1=bc(V(36, 37), [128, NJ, 4]), op=ALU.is_ge), reads=R, writes=R)
        k.op("dve", lambda e: e.tensor_tensor(out=rtmp[:, :, :, :], in0=V(4, 36).rearrange("p j (g x) -> p j g x", g=4),
                                               in1=bc(V(44, 48).unsqueeze(3), [128, NJ, 4, 8]), op=ALU.mult), reads=R, writes=[brtmp])
        k.op("dve", lambda e: e.reduce_sum(out=V(48, 56), in_=rtmp[:, :, :, :].rearrange("p j g x -> p j x g"), axis=AX.X),
             reads=[brtmp], writes=R)
        k.op("dve", lambda e: e.reduce_max(out=V(56, 57), in_=V(48, 56), axis=AX.X), reads=R, writes=R)
        k.op("dve", lambda e: e.tensor_tensor(out=V(48, 56), in0=V(48, 56), in1=bc(V(56, 57), [128, NJ, 8]), op=ALU.subtract), reads=R, writes=R)
        k.op("act", lambda e: e.activation(out=V(48, 56), in_=V(48, 56), func=AF.Exp), reads=R, writes=R)
        k.op("dve", lambda e: e.reduce_max(out=V(59, 60), in_=V(48, 56), axis=AX.X), reads=R, writes=R)
        k.op("dve", lambda e: e.tensor_tensor(out=V(64, 72), in0=V(48, 56), in1=bc(V(59, 60), [128, NJ, 8]), op=ALU.is_ge), reads=R, writes=R)
        k.op("dve", lambda e: e.tensor_scalar(out=V(64, 72), in0=V(64, 72), scalar1=-2.0, scalar2=None, op0=ALU.mult), reads=R, writes=R)
        k.op("dve", lambda e: e.tensor_tensor(out=V(64, 72), in0=V(64, 72), in1=V(48, 56), op=ALU.add), reads=R, writes=R)
        k.op("dve", lambda e: e.reduce_max(out=V(57, 58), in_=V(64, 72), axis=AX.X), reads=R, writes=R)
        k.op("dve", lambda e: e.tensor_tensor(out=V(58, 59), in0=V(57, 58), in1=V(59, 60), op=ALU.add), reads=R, writes=R)
        k.op("dve", lambda e: e.reciprocal(out=V(58, 59), in_=V(58, 59)), reads=R, writes=R)
        k.op("dve", lambda e: e.tensor_tensor(out=V(58, 59), in0=V(58, 59), in1=V(39, 40), op=ALU.mult), reads=R, writes=R)
        k.op("dve", lambda e: e.tensor_tensor(out=V(72, 80), in0=V(48, 56), in1=bc(V(57, 58), [128, NJ, 8]), op=ALU.is_ge), reads=R, writes=R)
        k.op("dve", lambda e: e.tensor_tensor(out=V(72, 80), in0=V(72, 80), in1=V(48, 56), op=ALU.mult), reads=R, writes=R)
        k.op("dve", lambda e: e.tensor_tensor(out=V(72, 80), in0=V(72, 80), in1=bc(V(58, 59), [128, NJ, 8]), op=ALU.mult), reads=R, writes=R)
        k.op("dve", lambda e: e.tensor_tensor(out=comb[:, :, :].rearrange("p j (g x) -> p j g x", g=4),
                                               in0=bc(V(44, 48).unsqueeze(3), [128, NJ, 4, 8]),
                                               in1=bc(V(72, 80).unsqueeze(2), [128, NJ, 4, 8]), op=ALU.mult),
             reads=R, writes=[bcomb])
        k.barrier()
    with contextlib.ExitStack() as es:
        hidT = k.sb("hidT", [128, 4, NTOK], BF16, es); bhid = [k.buf() for _ in range(4)]
        sgt = [k.sb("sgt%d" % i, [128, 512], F32, es) for i in range(2)]; bsgt = [k.buf() for _ in range(2)]
        ob = [tpf[0][:, 0:512], tpf[0][:, 512:1024], tpf[1][:, 0:512], tpf[1][:, 512:1024]]
        bob = [k.buf() for _ in range(4)]
        NE = st.n_experts
        gi = 0
        oi = 0
        for e in range(NE):
            if e + 1 < NE:
                load("g", e + 1)
            Wg, bWg = slots.pop(("g", e)); Wu, bWu = slots.pop(("u", e))
            for f in range(4):
                fs = slice(f * 128, (f + 1) * 128)
                for half in range(2):
                    hs = slice(half * 512, (half + 1) * 512)
                    pg = st.pb[(gi % 2) * 2]; bpg = st.bpb[(gi % 2) * 2]
                    pu = st.pb[(gi % 2) * 2 + 1]; bpu = st.bpb[(gi % 2) * 2 + 1]
                    S_ = sgt[gi % 2]; bS_ = bsgt[gi % 2]
                    gi += 1
                    mm_group(k, pg[:, :], [(Wg[:, kk, fs], xnT[:, kk, hs]) for kk in range(NKT)], reads=[bWg] + bxnT, writes=[bpg])
                    mm_group(k, pu[:, :], [(Wu[:, kk, fs], xnT[:, kk, hs]) for kk in range(NKT)], reads=[bWu] + bxnT, writes=[bpu])
                    k.op("act", lambda e_: e_.activation(out=S_[:, :], in_=pg[:, :], func=AF.Silu), reads=[bpg], writes=[bS_])
                    k.op("dve", lambda e_: e_.tensor_tensor(out=hidT[:, f, hs], in0=pu[:, :], in1=S_[:, :], op=ALU.mult),
                         reads=[bpu, bS_], writes=[bhid[f]])
            if e + 1 < NE:
                load("u", e + 1); load("d", e + 1)
            Wd, bWd = slots.pop(("d", e))
            for j in range(NJ):
                for dc in range(4):
                    cs = slice(dc * 512, (dc + 1) * 512)
                    O_ = ob[oi % 4]; bO_ = bob[oi % 4]; oi += 1
                    mm_group(k, O_, [(hidT[:, f, j * 128:(j + 1) * 128], Wd[:, f, cs]) for f in range(4)],
                             reads=[bWd] + bhid, writes=[bO_])
                    k.op("dve", lambda e_: e_.scalar_tensor_tensor(out=acc[:, j, cs], in0=O_, scalar=comb[:, j, e:e + 1], in1=acc[:, j, cs],
                                                                   op0=ALU.mult, op1=ALU.add),
                         reads=[bO_, bcomb, bacc[j]], writes=[bacc[j]])
        k.barrier()
    with contextlib.ExitStack() as es:
        xb = (k.sb("xb0", [128, D], BF16, es), k.buf())
        for j in range(NJ):
            norm_transpose(st, acc[:, j, :], [bacc[j]], st.gains["ln_ple"], xnT[:, :, j * 128:(j + 1) * 128], bxnT[j],
                           None, xb, (st.tp_view[j % 2][:, :], st.b_tp[j % 2]), xb)
        pT = k.sb("pT", [128, 2, NTOK], BF16, es); bpT = k.buf()
        pst = k.sb("pst", [128, 256], F32, es); bpst = k.buf()
        pstb = k.sb("pstb", [128, 256], BF16, es); bpstb = k.buf()
        for j in range(NJ):
            k.dma("sp", pst[:, :], din["pown"][j * 128:(j + 1) * 128, :], writes=[bpst])
            k.op("dve", lambda e: e.tensor_copy(out=pstb[:, :], in_=pst[:, :]), reads=[bpst], writes=[bpstb])
            tp_ = st.tp_view[j % 2]; btp_ = st.b_tp[j % 2]
            for t2 in range(2):
                k.op("pe", lambda e: e.transpose(out=tp_[:, t2 * 128:(t2 + 1) * 128], in_=pstb[:, t2 * 128:(t2 + 1) * 128], identity=st.identb[:, :]),
                     reads=[bpstb, st.b_identb], writes=[btp_], sig=(t2 == 1))
            k.op("act", lambda e: e.activation(out=pT[:, :, j * 128:(j + 1) * 128], in_=tp_[:, 0:256].rearrange("p (a t) -> p a t", a=2),
                                                func=AF.Copy), reads=[btp_], writes=[bpT])
        Wp = k.sb("Wp", [128, 2, D], BF16, es); bWp = k.buf()
        k.dma("pool", Wp[:, :, :], din["w_ple"].rearrange("(kt p) n -> p kt n", p=128), writes=[bWp])
        sg2 = [k.sb("sg2_%d" % i, [128, 512], F32, es) for i in range(2)]; bsg2 = [k.buf() for _ in range(2)]
        gi = 0
        for dc in range(4):
            cs = slice(dc * 512, (dc + 1) * 512)
            W, bW = wslot()
            k.dma("pool", W[:, :, :], din["w_ple_gate"][:, cs].rearrange("(kt p) n -> p kt n", p=128), writes=[bW])
            for j in range(NJ):
                ts_ = slice(j * 128, (j + 1) * 128)
                pg = st.pb[(gi % 2) * 2]; bpg = st.bpb[(gi % 2) * 2]
                pp = st.pb[(gi % 2) * 2 + 1]; bpp = st.bpb[(gi % 2) * 2 + 1]
                S_ = sg2[gi % 2]; bS_ = bsg2[gi % 2]
                gi += 1
                mm_group(k, pg[:, :], [(xnT[:, kk, ts_], W[:, kk, :]) for kk in range(NKT)], reads=[bW, bxnT[j]], writes=[bpg])
                mm_group(k, pp[:, :], [(pT[:, kk, ts_], Wp[:, kk, cs]) for kk in range(2)], reads=[bWp, bpT], writes=[bpp])
                k.op("act", lambda e: e.activation(out=S_[:, :], in_=pg[:, :], func=AF.Sigmoid), reads=[bpg], writes=[bS_])
                k.op("dve", lambda e: e.tensor_tensor(out=S_[:, :], in0=pp[:, :], in1=S_[:, :], op=ALU.mult), reads=[bpp, bS_], writes=[bS_])
                k.op("dve", lambda e: e.tensor_tensor(out=acc[:, j, cs], in0=acc[:, j, cs], in1=S_[:, :], op=ALU.add),
                     reads=[bS_, bacc[j]], writes=[bacc[j]])
        k.barrier()
    k.sb_free("xnT")
    with contextlib.ExitStack() as es:
        gfin = k.sb("gfin", [128, D], F32, es); bgfin = k.buf()
        k.dma("sp", gfin[:, :], din["ln_final"].partition_broadcast(128), writes=[bgfin])
        ost = [k.sb("ost%d" % i, [128, D], F32, es) for i in range(2)]; bost = [k.buf() for _ in range(2)]
        junk = (k.sb("junkc", [128, D], BF16, es), k.buf())
        for j in range(NJ):
            i = st.nt_i % 4; st.nt_i += 1
            sv = st.stat[:, i, :]; bs = st.bstat[i]
            k.op("act", lambda e: e.activation(out=junk[0][:, :], in_=acc[:, j, :], func=AF.Square, accum_out=sv[:, 0:1]),
                 reads=[bacc[j]], writes=[junk[1], bs])
            k.op("dve", lambda e: e.tensor_scalar(out=sv[:, 1:2], in0=sv[:, 0:1], scalar1=1.0 / D, scalar2=EPS, op0=ALU.mult, op1=ALU.add),
                 reads=[bs], writes=[bs])
            k.op("pool", lambda e: e.tensor_tensor(out=sv[:, 3:4], in0=sv[:, 1:2], in1=st.cneg[:, 0:1], op=ALU.pow), reads=[bs, st.b_cneg], writes=[bs])
            O_ = ost[j % 2]; bO_ = bost[j % 2]
            k.op("dve", lambda e: e.scalar_tensor_tensor(out=O_[:, :], in0=acc[:, j, :], scalar=sv[:, 3:4], in1=gfin[:, :],
                                                          op0=ALU.mult, op1=ALU.mult), reads=[bacc[j], bs, bgfin], writes=[bO_])
            k.dma("sp", out_ap[j * 128:(j + 1) * 128, :], O_[:, :], reads=[bO_], is_output=True)
    for i in range(4):
        k.sb_free("wslot%d" % i)
    k.sb_free("acc"); k.sb_free("comb")
```
